# Optimizing a Trainium2 kernel written in Bass

```python
import jax, jax.numpy as jnp
from jax import lax
import numpy as np

D_MODEL = 1024
BATCH = 4
SEQ = 8192
DEPTH = 1

CHUNK = 64
D_MIX = D_MODEL
M_WIDTH = D_MIX // 2
M_HEADS = 4
M_HEAD_DIM = M_WIDTH // M_HEADS
SB_WIDTH = D_MIX - M_WIDTH
SB_HEADS = 8
SB_HEAD_DIM = SB_WIDTH // SB_HEADS
SB_Q_BLOCK = 128
CONV_WIDTH = 4
N_GROUPS = 4
EXPERTS_PER_GROUP = 8
N_EXPERTS = N_GROUPS * EXPERTS_PER_GROUP
TOP_K_IN_GROUP = 2
D_EXPERT = D_MODEL // 2
EXPERT_BLOCK = 128
PLE_DIM = 256
EPS = 1e-6
SPLITS = [M_WIDTH, 2 * M_WIDTH, 3 * M_WIDTH, 4 * M_WIDTH,
          4 * M_WIDTH + M_HEADS, 4 * M_WIDTH + 2 * M_HEADS,
          4 * M_WIDTH + 2 * M_HEADS + SB_WIDTH, 4 * M_WIDTH + 2 * M_HEADS + 2 * SB_WIDTH]
IN_COLS = 4 * M_WIDTH + 2 * M_HEADS + 3 * SB_WIDTH

kernel_name = 'hymba_mlstm_stickbreak_hiermoe_ple'


def rmsnorm(x, g):
    xf = x.astype(jnp.float32)
    y = xf * lax.rsqrt(jnp.mean(xf * xf, axis=-1, keepdims=True) + EPS)
    return (y * g.astype(jnp.float32)).astype(x.dtype)


def causal_conv(x, w):
    K = w.shape[0]
    S = x.shape[1]
    xp = jnp.pad(x, ((0, 0), (K - 1, 0), (0, 0)))
    out = xp[:, 0:S] * w[0]
    for j in range(1, K):
        out = out + xp[:, j:j + S] * w[j]
    return out


def mlstm_chunkwise(q, k, v, i_pre, f_pre):
    B, S, H, D = q.shape
    L = CHUNK
    NC = S // L
    f32 = jnp.float32

    def chunks(t):
        return t.astype(f32).reshape(B, NC, L, H, -1).transpose(0, 1, 3, 2, 4)

    q = chunks(q)
    k = chunks(k) * (D ** -0.5)
    v = chunks(v)
    log_i = i_pre.astype(f32).reshape(B, NC, L, H).transpose(0, 1, 3, 2)
    log_f = jax.nn.log_sigmoid(f_pre.astype(f32)).reshape(B, NC, L, H).transpose(0, 1, 3, 2)
    b = jnp.cumsum(log_f, axis=-1)
    b_last = b[..., -1]

    w_end = b_last[..., None] - b + log_i
    m_loc = jnp.max(w_end, axis=-1)
    e_end = jnp.exp(w_end - m_loc[..., None])
    dC = jnp.einsum('bchs,bchsd,bchse->bchde', e_end, k, v)
    dn = jnp.einsum('bchs,bchsd->bchd', e_end, k)

    def step(carry, xs):
        C, n, m = carry
        dC_c, dn_c, mloc_c, blast_c = xs
        m_new = jnp.maximum(blast_c + m, mloc_c)
        decay = jnp.exp(blast_c + m - m_new)
        scale = jnp.exp(mloc_c - m_new)
        C_new = decay[..., None, None] * C + scale[..., None, None] * dC_c
        n_new = decay[..., None] * n + scale[..., None] * dn_c
        return (C_new, n_new, m_new), (C, n, m)

    init = (jnp.zeros((B, H, D, D), f32), jnp.zeros((B, H, D), f32), jnp.zeros((B, H), f32))
    xs = (jnp.moveaxis(dC, 1, 0), jnp.moveaxis(dn, 1, 0),
          jnp.moveaxis(m_loc, 1, 0), jnp.moveaxis(b_last, 1, 0))
    _, (C_prev, n_prev, m_prev) = lax.scan(step, init, xs)
    C_prev = jnp.moveaxis(C_prev, 0, 1)
    n_prev = jnp.moveaxis(n_prev, 0, 1)
    m_prev = jnp.moveaxis(m_prev, 0, 1)

    causal = jnp.tril(jnp.ones((L, L), dtype=bool))
    log_intra = b[..., :, None] - b[..., None, :] + log_i[..., None, :]
    log_intra = jnp.where(causal, log_intra, -jnp.inf)
    log_inter = b + m_prev[..., None]
    m_t = jnp.maximum(log_inter, jnp.max(log_intra, axis=-1))
    w_qk = jnp.exp(log_intra - m_t[..., None]) * jnp.einsum('bchtd,bchsd->bchts', q, k)
    a_int = jnp.exp(log_inter - m_t)
    num = (jnp.einsum('bchts,bchse->bchte', w_qk, v)
           + a_int[..., None] * jnp.einsum('bchtd,bchde->bchte', q, C_prev))
    den = jnp.sum(w_qk, axis=-1) + a_int * jnp.einsum('bchtd,bchd->bcht', q, n_prev)
    h = num / jnp.maximum(jnp.abs(den), jnp.exp(-m_t))[..., None]
    return h.transpose(0, 1, 3, 2, 4).reshape(B, S, H, D)


def stick_breaking(q, k, v):
    B, S, H, D = q.shape
    f32 = jnp.float32
    q = q.astype(f32).transpose(0, 2, 1, 3) * (D ** -0.5)
    k = k.astype(f32).transpose(0, 2, 1, 3)
    v = v.astype(f32).transpose(0, 2, 1, 3)
    s_idx = jnp.arange(S)

    def block(qb):
        start = qb * SB_Q_BLOCK
        qblk = lax.dynamic_slice_in_dim(q, start, SB_Q_BLOCK, axis=2)
        z = jnp.einsum('bhtd,bhsd->bhts', qblk, k)
        t_idx = start + jnp.arange(SB_Q_BLOCK)
        mask = s_idx[None, :] < t_idx[:, None]
        log_keep = jnp.where(mask, jax.nn.log_sigmoid(-z), 0.0)
        later = lax.cumsum(log_keep, axis=3, reverse=True) - log_keep
        log_a = jnp.where(mask, jax.nn.log_sigmoid(z) + later, -jnp.inf)
        return jnp.einsum('bhts,bhsd->bhtd', jnp.exp(log_a), v)

    o = lax.map(block, jnp.arange(S // SB_Q_BLOCK))
    return o.transpose(1, 0, 3, 2, 4).reshape(B, S, H, D)


def hier_moe(c, w_rg, b_rg, w_re, b_re, w_gate, w_up, w_down):
    B, S, D = c.shape
    T = B * S
    xf = c.reshape(T, D)
    x32 = xf.astype(jnp.float32)
    g_logits = x32 @ w_rg.astype(jnp.float32) + b_rg.astype(jnp.float32)
    g_prob = jax.nn.softmax(g_logits, axis=-1)
    g_sel = jnp.argmax(g_logits, axis=-1).astype(jnp.int32)
    p_g = jnp.take_along_axis(g_prob, g_sel[:, None], axis=1)[:, 0]
    e_logits = (x32 @ w_re.astype(jnp.float32) + b_re.astype(jnp.float32)).reshape(T, N_GROUPS, EXPERTS_PER_GROUP)
    e_sel_logits = jnp.take_along_axis(e_logits, g_sel[:, None, None], axis=1)[:, 0]
    top_v, top_i = lax.top_k(e_sel_logits, TOP_K_IN_GROUP)
    weights = p_g[:, None] * jax.nn.softmax(top_v, axis=-1)
    eids = g_sel[:, None] * EXPERTS_PER_GROUP + top_i.astype(jnp.int32)

    flat_e = eids.reshape(-1)
    flat_w = weights.reshape(-1)
    flat_tok = jnp.repeat(jnp.arange(T, dtype=jnp.int32), TOP_K_IN_GROUP)
    order = jnp.argsort(flat_e, stable=True)
    sorted_e = flat_e[order]
    counts = jnp.bincount(flat_e, length=N_EXPERTS).astype(jnp.int32)
    padded = ((counts + EXPERT_BLOCK - 1) // EXPERT_BLOCK) * EXPERT_BLOCK
    offs = jnp.cumsum(counts) - counts
    pend = jnp.cumsum(padded)
    poffs = pend - padded
    n_assign = T * TOP_K_IN_GROUP
    rank = jnp.arange(n_assign, dtype=jnp.int32) - offs[sorted_e]
    dest = poffs[sorted_e] + rank
    R = n_assign + N_EXPERTS * EXPERT_BLOCK
    NB = R // EXPERT_BLOCK
    buf_tok = jnp.zeros((R,), jnp.int32).at[dest].set(flat_tok[order])
    buf_w = jnp.zeros((R,), jnp.float32).at[dest].set(flat_w[order])
    block_start = jnp.arange(NB, dtype=jnp.int32) * EXPERT_BLOCK
    block_e = jnp.minimum(jnp.searchsorted(pend, block_start, side='right'), N_EXPERTS - 1)
    xin = xf[buf_tok].reshape(NB, EXPERT_BLOCK, D)

    def expert_block(args):
        xb, e = args
        hid = jax.nn.silu(xb @ w_gate[e]) * (xb @ w_up[e])
        return hid @ w_down[e]

    y = lax.map(expert_block, (xin, block_e)).reshape(R, D)
    out = jax.ops.segment_sum(y.astype(jnp.float32) * buf_w[:, None], buf_tok, num_segments=T)
    return out.reshape(B, S, D).astype(c.dtype)


def setup_inputs(seed: int = 0) -> dict:
    key = jax.random.key(seed)
    ks = jax.random.split(key, 24)
    f32 = jnp.float32

    def nrm(k, shape, scale):
        return jax.random.normal(k, shape, f32) * scale

    def gain(k, shape):
        return 1.0 + 0.02 * jax.random.normal(k, shape, f32)

    b_gates = jnp.concatenate([
        0.1 * jax.random.normal(ks[3], (DEPTH, M_HEADS), f32),
        jnp.linspace(3.0, 6.0, M_HEADS, dtype=f32)[None, :] + 0.1 * jax.random.normal(ks[4], (DEPTH, M_HEADS), f32),
    ], axis=-1)
    return {
        'x': nrm(ks[0], (BATCH, SEQ, D_MODEL), 1.0),
        'p': nrm(ks[1], (DEPTH, BATCH, SEQ, PLE_DIM), 1.0),
        'g_mix': gain(ks[2], (DEPTH, D_MODEL)),
        'w_in': nrm(ks[5], (DEPTH, D_MODEL, IN_COLS), D_MODEL ** -0.5),
        'b_gates': b_gates,
        'conv_q': nrm(ks[6], (DEPTH, CONV_WIDTH, M_WIDTH), CONV_WIDTH ** -0.5),
        'conv_k': nrm(ks[7], (DEPTH, CONV_WIDTH, M_WIDTH), CONV_WIDTH ** -0.5),
        'g_mhead': gain(ks[8], (DEPTH, M_WIDTH)),
        'w_out': nrm(ks[9], (DEPTH, D_MIX, D_MODEL), D_MIX ** -0.5),
        'g_ffn': gain(ks[10], (DEPTH, D_MODEL)),
        'w_router_group': nrm(ks[11], (DEPTH, D_MODEL, N_GROUPS), D_MODEL ** -0.5),
        'b_router_group': nrm(ks[12], (DEPTH, N_GROUPS), 0.01),
        'w_router_expert': nrm(ks[13], (DEPTH, D_MODEL, N_EXPERTS), D_MODEL ** -0.5),
        'b_router_expert': nrm(ks[14], (DEPTH, N_EXPERTS), 0.01),
        'w_exp_gate': nrm(ks[15], (DEPTH, N_EXPERTS, D_MODEL, D_EXPERT), D_MODEL ** -0.5),
        'w_exp_up': nrm(ks[16], (DEPTH, N_EXPERTS, D_MODEL, D_EXPERT), D_MODEL ** -0.5),
        'w_exp_down': nrm(ks[17], (DEPTH, N_EXPERTS, D_EXPERT, D_MODEL), D_EXPERT ** -0.5),
        'g_ple': gain(ks[18], (DEPTH, D_MODEL)),
        'w_ple_gate': nrm(ks[19], (DEPTH, D_MODEL, D_MODEL), D_MODEL ** -0.5),
        'w_ple_proj': nrm(ks[20], (DEPTH, PLE_DIM, D_MODEL), PLE_DIM ** -0.5),
        'g_ple_post': gain(ks[21], (DEPTH, D_MODEL)),
        'g_final': gain(ks[22], (D_MODEL,)),
    }


def reference(x, p, g_mix, w_in, b_gates, conv_q, conv_k, g_mhead, w_out, g_ffn,
              w_router_group, b_router_group, w_router_expert, b_router_expert,
              w_exp_gate, w_exp_up, w_exp_down, g_ple, w_ple_gate, w_ple_proj,
              g_ple_post, g_final):
    h = x
    B, S, _ = x.shape
    for l in range(DEPTH):
        a = rmsnorm(h, g_mix[l])
        u = a @ w_in[l]
        mq, mk, mv, mo, mi, mf, sq, sk, sv = jnp.split(u, SPLITS, axis=-1)
        mq = jax.nn.silu(causal_conv(mq, conv_q[l]))
        mk = jax.nn.silu(causal_conv(mk, conv_k[l]))
        i_pre = mi + b_gates[l, :M_HEADS]
        f_pre = mf + b_gates[l, M_HEADS:]
        hm = mlstm_chunkwise(mq.reshape(B, S, M_HEADS, M_HEAD_DIM),
                             mk.reshape(B, S, M_HEADS, M_HEAD_DIM),
                             mv.reshape(B, S, M_HEADS, M_HEAD_DIM), i_pre, f_pre)
        hm = rmsnorm(hm, g_mhead[l].reshape(M_HEADS, M_HEAD_DIM))
        hm = jax.nn.sigmoid(mo.astype(jnp.float32)).reshape(B, S, M_HEADS, M_HEAD_DIM) * hm
        hs = stick_breaking(sq.reshape(B, S, SB_HEADS, SB_HEAD_DIM),
                            sk.reshape(B, S, SB_HEADS, SB_HEAD_DIM),
                            sv.reshape(B, S, SB_HEADS, SB_HEAD_DIM))
        mixed = jnp.concatenate([hm.reshape(B, S, M_WIDTH), hs.reshape(B, S, SB_WIDTH)], axis=-1)
        h = h + mixed.astype(h.dtype) @ w_out[l]
        c = rmsnorm(h, g_ffn[l])
        h = h + hier_moe(c, w_router_group[l], b_router_group[l], w_router_expert[l],
                         b_router_expert[l], w_exp_gate[l], w_exp_up[l], w_exp_down[l])
        gate = jax.nn.sigmoid((rmsnorm(h, g_ple[l]) @ w_ple_gate[l]).astype(jnp.float32))
        ple = rmsnorm(p[l].astype(h.dtype) @ w_ple_proj[l], g_ple_post[l])
        h = h + (gate * ple.astype(jnp.float32)).astype(h.dtype)
    return rmsnorm(h, g_final)
```

```python
import math
from contextlib import ExitStack
import numpy as np
import concourse.bass as bass
import concourse.mybir as mybir
from concourse.bass_utils import run_bass_kernel_spmd

F32 = mybir.dt.float32
BF16 = mybir.dt.bfloat16
AF = mybir.ActivationFunctionType
ALU = mybir.AluOpType
AX = mybir.AxisListType

D = 1024
NCOL = 3592
EPS = 1e-6
LNC = math.log(128.0 ** -0.5)
NEXP = 32


class Res:
    __slots__ = ("name", "last_w", "readers", "dsem")

    def __init__(self, name):
        self.name = name
        self.last_w = None
        self.readers = []
        self.dsem = None


class Sched:
    ENGS = ("pe", "act", "dve", "pool", "sp")

    def __init__(self, nc):
        self.nc = nc
        self.ops = {e: [] for e in self.ENGS}
        self.cnt = {e: 0 for e in self.ENGS}
        self.sem = {}
        self.waited = {e: {} for e in self.ENGS}
        self.dcnt = {}
        self.dsem_objs = {}
        self.free_dsems = []
        import os
        self.limit = int(os.environ.get("KSTOP", "0")) or None
        self.total = 0
        self.log = []

    def set_sems(self, sems, dma_sems):
        for e, s in zip(self.ENGS, sems):
            self.sem[e] = s
        self.free_dsems = list(dma_sems)

    def res(self, name):
        return Res(name)

    def _deps(self, eng, reads, writes, self_sync):
        deps = {}

        def add(ev):
            if ev is None:
                return
            s, v, src = ev
            if src == eng and not self_sync:
                return
            k = id(s)
            if k not in deps or deps[k][1] < v:
                deps[k] = (s, v)
        for r in reads:
            add(r.last_w)
        for w in writes:
            add(w.last_w)
            for e in w.readers:
                add(e)
        waits = []
        wd = self.waited[eng]
        for k, (s, v) in deps.items():
            if wd.get(k, 0) < v:
                wd[k] = v
                waits.append((s, v))
        return waits

    def _commit(self, ev, reads, writes):
        for r in reads:
            r.readers.append(ev)
            if len(r.readers) > 64:
                r.readers = r.readers[-48:] if False else r.readers
        for w in writes:
            w.last_w = ev
            w.readers = []

    def op(self, eng, fn, reads=(), writes=(), self_sync=None):
        self.total += 1
        if self.limit and self.total > self.limit:
            return None
        import sys as _sys
        self.log.append((self.total, eng, _sys._getframe(1).f_lineno))
        if self_sync is None:
            self_sync = eng != "pe"
        waits = self._deps(eng, reads, writes, self_sync)
        self.cnt[eng] += 1
        ev = (self.sem[eng], self.cnt[eng], eng)
        self.ops[eng].append((waits, fn, (self.sem[eng], 1)))
        self._commit(ev, reads, writes)
        return ev

    def dma(self, eng, fn, reads=(), writes=(), sem_res=None):
        self.total += 1
        if self.limit and self.total > self.limit:
            return None
        import sys as _sys
        self.log.append((self.total, "dma-" + eng, _sys._getframe(1).f_lineno))
        owner = sem_res or (writes[0] if writes else reads[0])
        if owner.dsem is None:
            owner.dsem = self.free_dsems.pop()
            self.dcnt.setdefault(id(owner.dsem), 0)
            self.dsem_objs[id(owner.dsem)] = owner.dsem
        s = owner.dsem
        waits = self._deps(eng, reads, writes, True)
        self.dcnt[id(s)] += 16
        ev = (s, self.dcnt[id(s)], "dma")
        self.ops[eng].append((waits, fn, (s, 16)))
        self._commit(ev, reads, writes)
        return ev

    def release(self, res_list):
        for r in res_list:
            if r.dsem is not None:
                self.free_dsems.append(r.dsem)
                r.dsem = None

    def barrier(self):
        for eng in self.ENGS:
            waits = []
            wd = self.waited[eng]
            for x in self.ENGS:
                s, v = self.sem[x], self.cnt[x]
                if v > 0 and wd.get(id(s), 0) < v:
                    wd[id(s)] = v
                    waits.append((s, v))
            for k, v in self.dcnt.items():
                if v > 0 and wd.get(k, 0) < v:
                    wd[k] = v
                    waits.append((self.dsem_objs[k], v))
            if waits:
                self.ops[eng].append((waits, None, None))

    def emit(self, block):
        sched = self

        def run(engname, handle):
            for waits, fn, inc in sched.ops[engname]:
                for (s, v) in waits:
                    handle.wait_ge(s, v)
                if fn is not None:
                    ins = fn(handle)
                    ins.then_inc(inc[0], inc[1])

        @block.tensor
        def _(e):
            run("pe", e)

        @block.scalar
        def _(e):
            run("act", e)

        @block.vector
        def _(e):
            run("dve", e)

        @block.gpsimd
        def _(e):
            run("pool", e)

        @block.sync
        def _(e):
            run("sp", e)


def build_nc(SP, SO, phases="ABC", dbg=False):
    assert SP % 512 == 0 and SO % 512 == 0
    ST = SP + SO
    NGP, NGO = SP // 512, SO // 512
    NG = NGP + NGO
    NT = ST // 128
    NTO = SO // 128
    nc = bass.Bass("TRN2", target_bir_lowering=False)

    def din(name, shape, dt=F32):
        return nc.dram_tensor(name, list(shape), dt, kind="ExternalInput").ap()

    xp = din("xp", [SP, D])
    xo = din("xo", [SO, D])
    po = din("po", [SO, 256])
    win = din("win", [D, NCOL])
    gvec = din("gvec", [5, D])
    gmh = din("gmh", [1, 512])
    bg = din("bg", [1, 8])
    cq = din("cq", [128, 16])
    ck = din("ck", [128, 16])
    wout = din("wout", [D, D])
    wr = din("wr", [D, 36])
    br = din("br", [1, 36])
    wg = din("wg", [NEXP, D, 512])
    wu = din("wu", [NEXP, D, 512])
    wd = din("wd", [NEXP, 512, D])
    wpg = din("wpg", [D, D])
    wpp = din("wpp", [256, D])
    out = nc.dram_tensor("out", [SO, D], F32, kind="ExternalOutput").ap()

    skind = "ExternalOutput" if dbg else "Internal"
    dbgA = nc.dram_tensor("dbgA", [64, 2048], F32, kind=skind).ap()
    KT_d = nc.dram_tensor("KT_d", [8, 64, ST], BF16, kind=skind).ap()
    QT_d = nc.dram_tensor("QT_d", [8, 64, SO], BF16, kind=skind).ap()
    V_d = nc.dram_tensor("V_d", [8, 128, NT, 64], BF16, kind=skind).ap()
    mixTm_d = nc.dram_tensor("mixTm_d", [NTO, 128, 4, 128], BF16, kind=skind).ap()
    mixTs_d = nc.dram_tensor("mixTs_d", [NTO, 64, 8, 128], BF16, kind=skind).ap()

    es = ExitStack()
    with es:
        sems = [es.enter_context(nc.semaphore(f"s_{e}")) for e in Sched.ENGS]
        dsems = [es.enter_context(nc.semaphore(f"d_{i}")) for i in range(48)]
        block = es.enter_context(nc.Block())
        S = Sched(nc)
        S.set_sems(sems, dsems)
        R = S.res

        uid = [0]

        def sbt(stack, name, shape, dt):
            uid[0] += 1
            return stack.enter_context(nc.sbuf_tensor(f"{name}_{uid[0]}", list(shape), dt))

        def pst(stack, name, shape, dt):
            uid[0] += 1
            return stack.enter_context(nc.psum_tensor(f"{name}_{uid[0]}", list(shape), dt))

        identf = sbt(es, "identf", [128, 128], F32)
        identb = sbt(es, "identb", [128, 128], BF16)
        onesb = sbt(es, "onesb", [128, 128], BF16)
        trilb = sbt(es, "trilb", [128, 128], BF16)
        tmpf = sbt(es, "tmpf", [128, 128], F32)
        triu64 = sbt(es, "triu64", [64, 64], F32)
        ones64 = sbt(es, "ones64", [64, 128], F32)
        negm = sbt(es, "negm", [64, 64], F32)
        r_const = R("const")

        S.op("pool", lambda e: e.memset(identf[:], 0.0), writes=[r_const])
        S.op("pool", lambda e: e.affine_select(out=identf[:], in_=identf[:], pattern=[[-1, 128]], compare_op=ALU.not_equal,
                                               fill=1.0, base=0, channel_multiplier=1), reads=[r_const], writes=[r_const])
        S.op("dve", lambda e: e.tensor_copy(out=identb[:], in_=identf[:]), reads=[r_const], writes=[r_const])
        S.op("pool", lambda e: e.memset(onesb[:], 1.0), writes=[r_const])
        S.op("pool", lambda e: e.memset(tmpf[:], 1.0), writes=[r_const])
        S.op("pool", lambda e: e.affine_select(out=tmpf[:], in_=tmpf[:], pattern=[[-1, 128]], compare_op=ALU.is_ge,
                                               fill=0.0, base=0, channel_multiplier=1), reads=[r_const], writes=[r_const])
        S.op("dve", lambda e: e.tensor_copy(out=trilb[:], in_=tmpf[:]), reads=[r_const], writes=[r_const])
        S.op("pool", lambda e: e.memset(triu64[:], 1.0), writes=[r_const])
        S.op("pool", lambda e: e.affine_select(out=triu64[:], in_=triu64[:], pattern=[[1, 64]], compare_op=ALU.is_ge,
                                               fill=0.0, base=0, channel_multiplier=-1), reads=[r_const], writes=[r_const])
        S.op("pool", lambda e: e.memset(ones64[:], 1.0), writes=[r_const])
        S.op("pool", lambda e: e.memset(negm[:], 0.0), writes=[r_const])
        S.op("pool", lambda e: e.affine_select(out=negm[:], in_=negm[:], pattern=[[1, 64]], compare_op=ALU.is_ge,
                                               fill=30000.0, base=0, channel_multiplier=-1), reads=[r_const], writes=[r_const])
        triu64b = sbt(es, "triu64b", [64, 64], BF16)
        ones64b = sbt(es, "ones64b", [64, 128], BF16)
        negmb = sbt(es, "negmb", [64, 64], BF16)
        S.op("dve", lambda e: e.tensor_copy(out=triu64b[:], in_=triu64[:]), reads=[r_const], writes=[r_const])
        S.op("dve", lambda e: e.tensor_copy(out=ones64b[:], in_=ones64[:]), reads=[r_const], writes=[r_const])
        S.op("dve", lambda e: e.tensor_copy(out=negmb[:], in_=negm[:]), reads=[r_const], writes=[r_const])
        S.barrier()

        def rmsnorm_stats(src_ap, junk, ss, rstd, r_src, r_junk, r_ss, n):
            S.op("pool", lambda e: e.memset(ss, 0.0), writes=[r_ss])
            S.op("act", lambda e: e.activation(out=junk, in_=src_ap, func=AF.Square, accum_out=ss), reads=[r_src, r_ss], writes=[r_junk, r_ss])
            S.op("dve", lambda e: e.tensor_scalar(out=rstd, in0=ss, scalar1=1.0 / n, scalar2=EPS, op0=ALU.mult, op1=ALU.add),
                 reads=[r_ss], writes=[r_ss])
            S.op("act", lambda e: e.sqrt(out=rstd, in_=rstd), reads=[r_ss], writes=[r_ss])
            S.op("dve", lambda e: e.reciprocal(out=rstd, in_=rstd), reads=[r_ss], writes=[r_ss])

        def phase_A():
          with ExitStack() as ea:
            winb = sbt(ea, "winb", [128, 8, NCOL], BF16)
            gmix = sbt(ea, "gmix", [128, D], F32)
            gmht = sbt(ea, "gmht", [64, 512], F32)
            bgt = sbt(ea, "bgt", [64, 8], F32)
            cqt = sbt(ea, "cqt", [128, 16], F32)
            ckt = sbt(ea, "ckt", [128, 16], F32)
            xts = [sbt(ea, f"xt{i}", [128, D], F32) for i in range(2)]
            junk = sbt(ea, "junk", [128, D], BF16)
            ssq = sbt(ea, "ssq", [128, 2], F32)
            a_bf = sbt(ea, "a_bf", [128, D], BF16)
            aT = sbt(ea, "aT", [128, 8, 512], BF16)
            qpre = sbt(ea, "qpre", [128, 4, 515], F32)
            kpre = sbt(ea, "kpre", [128, 4, 515], F32)
            ctmp = [sbt(ea, f"ctmp{i}", [128, 512], F32) for i in range(2)]
            qTm = sbt(ea, "qTm", [128, 4, 512], BF16)
            kTm = sbt(ea, "kTm", [128, 4, 512], BF16)
            sqT = sbt(ea, "sqT", [128, 4, 512], BF16)
            skT = sbt(ea, "skT", [128, 4, 512], BF16)
            svt = sbt(ea, "svt", [128, 8, 4, 64], BF16)
            ktok = sbt(ea, "ktok", [64, 4, 128], BF16)
            vext = sbt(ea, "vext", [64, 512], BF16)
            onecol = sbt(ea, "onecol", [64, 2], BF16)
            gvext = sbt(ea, "gvext", [64, 512], BF16)
            gamb = sbt(ea, "gamb", [64, 4], BF16)
            gsb = sbt(ea, "gsb", [64, 8], F32)
            lfs = sbt(ea, "lfs", [64, 4], F32)
            lfb = sbt(ea, "lfb", [64, 8, 64], BF16)
            lf2 = sbt(ea, "lf2", [64, 8], BF16)
            lft = sbt(ea, "lft", [64, 4], F32)
            bs = sbt(ea, "bs", [64, 8], F32)
            alpha = sbt(ea, "alpha", [64, 4], F32)
            gamma = sbt(ea, "gamma", [64, 4], F32)
            garg = sbt(ea, "garg", [64, 4], F32)
            dcoef = sbt(ea, "dcoef", [64, 4], F32)
            eB = sbt(ea, "eB", [128, 4], F32)
            DTs = sbt(ea, "DTs", [64, 4, 64], F32)
            WT = sbt(ea, "WT", [64, 4, 64], BF16)
            n2s = sbt(ea, "n2s", [64, 4, 128], F32)
            numsb = sbt(ea, "numsb", [64, 4, 128], F32)
            sqh = sbt(ea, "sqh", [64, 4, 128], F32)
            dsb = sbt(ea, "dsb", [64, 8], F32)
            den = sbt(ea, "den", [64, 4], F32)
            ssh = sbt(ea, "ssh", [64, 4], F32)
            sig = sbt(ea, "sig", [64, 512], F32)
            mixtok = sbt(ea, "mixtok", [64, 4, 128], BF16)
            mixTg = sbt(ea, "mixTg", [128, 4, 512], BF16)
            Cst = sbt(ea, "Cst", [128, 4, 128], F32)
            nst = sbt(ea, "nst", [128, 4], F32)
            Cbf = sbt(ea, "Cbf", [128, 512], BF16)
            nbf = sbt(ea, "nbf", [128, 4], BF16)

            bF = pst(ea, "bF", [128, 512], F32)
            bSm = pst(ea, "bSm", [128, 512], F32)
            bV = pst(ea, "bV", [128, 512], F32)
            bO = pst(ea, "bO", [128, 512], F32)
            bAB = pst(ea, "bAB", [128, 512], F32)
            bN = pst(ea, "bN", [128, 512], F32)
            bN2 = pst(ea, "bN2", [128, 512], F32)
            bT = pst(ea, "bT", [128, 1024], BF16)

            names = ["winb", "gmix", "small", "xt0", "xt1", "junk", "ssq", "a_bf", "aT", "qpre", "kpre", "ctmp0", "ctmp1", "qTm", "kTm",
                     "sqT", "skT", "svt", "ktok", "vext", "gvext", "gsb", "lfs", "lfb", "bs", "alpha", "gamma", "garg", "dcoef", "eB",
                     "DTs", "WT", "n2s", "numsb", "sqh", "dsb", "den", "ssh", "sig", "mixtok", "mixTg", "Cst", "nst", "Cbf",
                     "bF", "bG", "bS", "bE", "bD", "bDn", "bV", "bO", "bA", "bB", "bN", "bN2", "bT",
                     "KTd", "QTd", "Vd", "mixTmd"]
            r = {k: R(k) for k in names}

            for c in range(8):
                S.dma("pool", lambda e, c=c: e.dma_start(out=winb[:, c, :], in_=win[c * 128:(c + 1) * 128, :]), writes=[r["winb"]])
            S.dma("sp", lambda e: e.dma_start(out=gmix[:], in_=gvec[0:1, :].partition_broadcast(128)), writes=[r["gmix"]])
            S.dma("sp", lambda e: e.dma_start(out=gmht[:], in_=gmh.partition_broadcast(64)), writes=[r["small"]])
            S.dma("sp", lambda e: e.dma_start(out=bgt[:], in_=bg.partition_broadcast(64)), writes=[r["small"]])
            S.dma("sp", lambda e: e.dma_start(out=cqt[:], in_=cq), writes=[r["small"]])
            S.dma("sp", lambda e: e.dma_start(out=ckt[:], in_=ck), writes=[r["small"]])
            S.op("pool", lambda e: e.memset(Cst[:], 0.0), writes=[r["Cst"]])
            S.op("pool", lambda e: e.memset(nst[:], 0.0), writes=[r["nst"]])
            S.op("pool", lambda e: e.memset(Cbf[:], 0.0), writes=[r["Cbf"]])
            S.op("pool", lambda e: e.memset(onecol[:], 1.0), writes=[r["vext"]])
            S.op("pool", lambda e: e.memset(nbf[:], 0.0), writes=[r["Cbf"]])
            S.op("pool", lambda e: e.memset(qpre[:], 0.0), writes=[r["qpre"]])
            S.op("pool", lambda e: e.memset(kpre[:], 0.0), writes=[r["kpre"]])

            evac_i = [0]

            def evac(out_ap, in_ap, reads, writes, scale=None):
                evac_i[0] += 1
                if scale is not None or evac_i[0] % 2 == 0:
                    if scale is None:
                        S.op("act", lambda e: e.copy(out=out_ap, in_=in_ap), reads=reads, writes=writes)
                    else:
                        S.op("act", lambda e: e.mul(out=out_ap, in_=in_ap, mul=scale), reads=reads, writes=writes)
                else:
                    S.op("dve", lambda e: e.tensor_copy(out=out_ap, in_=in_ap), reads=reads, writes=writes)

            for G in range(NG):
                own = G >= NGP
                src = xo if own else xp
                row0 = (G - NGP if own else G) * 512
                for ti in range(4):
                    xi = ti % 2
                    xt = xts[xi]
                    rx = r[f"xt{xi}"]
                    S.dma("sp", lambda e, xt=xt, a=row0 + ti * 128, src=src: e.dma_start(out=xt[:], in_=src[a:a + 128, :]), writes=[rx])
                    rmsnorm_stats(xt[:], junk[:], ssq[:, 0:1], ssq[:, 1:2], rx, r["junk"], r["ssq"], D)
                    S.op("dve", lambda e, xt=xt: e.scalar_tensor_tensor(out=a_bf[:], in0=xt[:], scalar=ssq[:, 1:2], in1=gmix[:], op0=ALU.mult, op1=ALU.mult),
                         reads=[rx, r["ssq"], r["gmix"]], writes=[r["a_bf"]])
                    for c in range(8):
                        S.op("pe", lambda e, c=c: e.transpose(out=bT[:, c * 128:(c + 1) * 128], in_=a_bf[:, c * 128:(c + 1) * 128], identity=identb[:]),
                             reads=[r["a_bf"]], writes=[r["bT"]])
                    evac(aT[:, :, ti * 128:(ti + 1) * 128], bT[:].rearrange("p (c t) -> p c t", c=8), [r["bT"]], [r["aT"]])

                def fproj(col0, dst_ap, r_dst, scale=None):
                    for c in range(8):
                        S.op("pe", lambda e, c=c: e.matmul(out=bF[:], lhsT=winb[:, c, col0:col0 + 128], rhs=aT[:, c, :], start=(c == 0), stop=(c == 7)),
                             reads=[r["winb"], r["aT"]], writes=[r["bF"]])
                    evac(dst_ap, bF[:], [r["bF"]], [r_dst], scale=scale)

                for h in range(4):
                    if own:
                        fproj(h * 128, qpre[:, h, 3:515], r["qpre"])
                    fproj(512 + h * 128, kpre[:, h, 3:515], r["kpre"])
                for j in range(4):
                    if own:
                        fproj(2056 + j * 128, sqT[:, j, :], r["sqT"], scale=0.125)
                    fproj(2568 + j * 128, skT[:, j, :], r["skT"])

                def conv(pre, cwt, dstT, r_pre, r_dst):
                    for h in range(4):
                        tmp = ctmp[h % 2]
                        rt = r[f"ctmp{h % 2}"]
                        S.op("dve", lambda e, h=h, tmp=tmp: e.tensor_scalar(out=tmp[:], in0=pre[:, h, 0:512], scalar1=cwt[:, h * 4:h * 4 + 1], scalar2=None, op0=ALU.mult),
                             reads=[r_pre, r["small"]], writes=[rt])
                        for j in range(1, 4):
                            S.op("dve", lambda e, h=h, j=j, tmp=tmp: e.scalar_tensor_tensor(out=tmp[:], in0=pre[:, h, j:j + 512], scalar=cwt[:, h * 4 + j:h * 4 + j + 1],
                                                                                          in1=tmp[:], op0=ALU.mult, op1=ALU.add),
                                 reads=[r_pre, rt], writes=[rt])
                        S.op("act", lambda e, h=h, tmp=tmp: e.activation(out=dstT[:, h, :], in_=tmp[:], func=AF.Silu), reads=[rt], writes=[r_dst])
                    S.op("pool", lambda e: e.tensor_copy(out=pre[:, :, 0:3], in_=pre[:, :, 512:515]), reads=[r_pre], writes=[r_pre])

                if own:
                    conv(qpre, cqt, qTm, r["qpre"], r["qTm"])
                else:
                    if G == NGP - 1:
                        for h in range(4):
                            for c in range(8):
                                S.op("pe", lambda e, c=c, h=h: e.matmul(out=bF[:], lhsT=winb[:, c, h * 128:h * 128 + 128], rhs=aT[:, c, :], start=(c == 0), stop=(c == 7)),
                                     reads=[r["winb"], r["aT"]], writes=[r["bF"]])
                            evac(qpre[:, h, 0:3], bF[:, 509:512], [r["bF"]], [r["qpre"]])
                conv(kpre, ckt, kTm, r["kpre"], r["kTm"])

                tokg = G * 512
                S.dma("sp", lambda e, tokg=tokg: e.dma_start(out=KT_d.rearrange("(j two) d t -> (two d) j t", two=2)[:, :, tokg:tokg + 512], in_=skT[:]),
                      reads=[r["skT"]], writes=[r["KTd"]], sem_res=r["skT"])
                if own:
                    S.dma("sp", lambda e, a=row0: e.dma_start(out=QT_d.rearrange("(j two) d t -> (two d) j t", two=2)[:, :, a:a + 512], in_=sqT[:]),
                          reads=[r["sqT"]], writes=[r["QTd"]], sem_res=r["sqT"])
                for ti in range(4):
                    for c in range(8):
                        S.op("pe", lambda e, c=c, ti=ti: e.matmul(out=bF[:], lhsT=aT[:, c, ti * 128:(ti + 1) * 128], rhs=winb[:, c, 3080:3592], start=(c == 0), stop=(c == 7)),
                             reads=[r["winb"], r["aT"]], writes=[r["bF"]])
                    evac(svt[:, :, ti, :], bF[:].rearrange("p (h d) -> p h d", h=8), [r["bF"]], [r["svt"]])
                S.dma("sp", lambda e, G=G: e.dma_start(out=V_d.rearrange("h s j d -> s h j d")[:, :, 4 * G:4 * G + 4, :], in_=svt[:]),
                      reads=[r["svt"]], writes=[r["Vd"]], sem_res=r["svt"])

                for ci in range(8):
                    c0 = ci * 64
                    for c in range(8):
                        S.op("pe", lambda e, c=c, c0=c0: e.matmul(out=bV[0:64, :], lhsT=aT[:, c, c0:c0 + 64], rhs=winb[:, c, 1024:1536], start=(c == 0), stop=(c == 7)),
                             reads=[r["winb"], r["aT"]], writes=[r["bV"]])
                    for c in range(8):
                        S.op("pe", lambda e, c=c, c0=c0: e.matmul(out=bSm[0:64, 0:8], lhsT=aT[:, c, c0:c0 + 64], rhs=winb[:, c, 2048:2056], start=(c == 0), stop=(c == 7)),
                             reads=[r["winb"], r["aT"]], writes=[r["bG"]])
                    if own:
                        for c in range(8):
                            S.op("pe", lambda e, c=c, c0=c0: e.matmul(out=bO[0:64, :], lhsT=aT[:, c, c0:c0 + 64], rhs=winb[:, c, 1536:2048], start=(c == 0), stop=(c == 7)),
                                 reads=[r["winb"], r["aT"]], writes=[r["bO"]])
                    for h in range(4):
                        S.op("pe", lambda e, h=h, c0=c0: e.transpose(out=bT[0:64, h * 128:(h + 1) * 128], in_=kTm[:, h, c0:c0 + 64], identity=identb[:]),
                             reads=[r["kTm"]], writes=[r["bT"]])
                    evac(ktok[:], bT[0:64, 0:512].rearrange("p (h d) -> p h d", h=4), [r["bT"]], [r["ktok"]])
                    S.op("dve", lambda e: e.tensor_tensor(out=gsb[:], in0=bSm[0:64, 0:8], in1=bgt[:], op=ALU.add), reads=[r["bG"], r["small"]], writes=[r["gsb"]])
                    S.op("act", lambda e: e.activation(out=lfs[:], in_=gsb[:, 4:8], func=AF.Exp, scale=-1.0), reads=[r["gsb"]], writes=[r["lfs"]])
                    S.op("act", lambda e: e.activation(out=lfs[:], in_=lfs[:], func=AF.Ln, bias=1.0), reads=[r["lfs"]], writes=[r["lfs"]])
                    S.op("dve", lambda e: e.tensor_copy(out=lf2[:, 0:4], in_=lfs[:]), reads=[r["lfs"]], writes=[r["lfb"]])
                    S.op("dve", lambda e: e.tensor_tensor(out=lft[:], in0=lfs[:], in1=lf2[:, 0:4], op=ALU.subtract), reads=[r["lfs"], r["lfb"]], writes=[r["lfb"]])
                    S.op("dve", lambda e: e.tensor_copy(out=lf2[:, 4:8], in_=lft[:]), reads=[r["lfb"]], writes=[r["lfb"]])
                    for q_ in range(2):
                        S.op("pe", lambda e, q_=q_: e.matmul(out=bSm[0:64, 8:12], lhsT=triu64b[:], rhs=lf2[:, q_ * 4:q_ * 4 + 4], start=(q_ == 0), stop=(q_ == 1)), reads=[r["lfb"]], writes=[r["bS"]])
                    for q_ in range(2):
                        S.op("pe", lambda e, q_=q_: e.matmul(out=bSm[0:64, 12:16], lhsT=ones64b[:, 0:64], rhs=lf2[:, q_ * 4:q_ * 4 + 4], start=(q_ == 0), stop=(q_ == 1)), reads=[r["lfb"]], writes=[r["bS"]])
                    for q_ in range(2):
                        S.op("pe", lambda e, q_=q_: e.matmul(out=bSm[:, 16:20], lhsT=ones64b[:], rhs=lf2[:, q_ * 4:q_ * 4 + 4], start=(q_ == 0), stop=(q_ == 1)), reads=[r["lfb"]], writes=[r["bE"]])
                    S.op("act", lambda e: e.copy(out=bs[:], in_=bSm[0:64, 8:16]), reads=[r["bS"]], writes=[r["bs"]])
                    S.op("act", lambda e: e.activation(out=eB[:], in_=bSm[:, 16:20], func=AF.Exp, scale=-1.0), reads=[r["bE"]], writes=[r["eB"]])
                    S.op("dve", lambda e: e.tensor_tensor(out=garg[:], in0=bs[:, 0:4], in1=bs[:, 4:8], op=ALU.subtract), reads=[r["bs"]], writes=[r["garg"]])
                    S.op("dve", lambda e: e.tensor_tensor(out=garg[:], in0=garg[:], in1=gsb[:, 0:4], op=ALU.add), reads=[r["garg"], r["gsb"]], writes=[r["garg"]])
                    S.op("act", lambda e: e.activation(out=gamma[:], in_=garg[:], func=AF.Exp), reads=[r["garg"]], writes=[r["gamma"]])
                    S.op("dve", lambda e: e.tensor_tensor(out=gvext[:].rearrange("p (h d) -> p h d", h=4), in0=bV[0:64, :].rearrange("p (h d) -> p h d", h=4),
                                                          in1=gamma[:].unsqueeze(2).to_broadcast([64, 4, 128]), op=ALU.mult),
                         reads=[r["bV"], r["gamma"]], writes=[r["gvext"]])
                    S.op("dve", lambda e: e.tensor_copy(out=gamb[:], in_=gamma[:]), reads=[r["gamma"]], writes=[r["gvext"]])
                    if own:
                        S.op("dve", lambda e: e.tensor_copy(out=vext[:], in_=bV[0:64, :]), reads=[r["bV"]], writes=[r["vext"]])
                        S.op("dve", lambda e: e.tensor_scalar(out=alpha[:], in0=bs[:, 0:4], scalar1=-1.0, scalar2=LNC, op0=ALU.mult, op1=ALU.add), reads=[r["bs"]], writes=[r["alpha"]])
                        S.op("act", lambda e: e.activation(out=alpha[:], in_=alpha[:], func=AF.Exp), reads=[r["alpha"]], writes=[r["alpha"]])
                        S.op("dve", lambda e: e.scalar_tensor_tensor(out=dcoef[:], in0=bs[:, 0:4], scalar=LNC, in1=gsb[:, 0:4], op0=ALU.add, op1=ALU.add),
                             reads=[r["bs"], r["gsb"]], writes=[r["dcoef"]])
                        S.op("dve", lambda e: e.tensor_copy(out=lfb[:], in_=lf2[:].unsqueeze(2).to_broadcast([64, 8, 64])), reads=[r["lfb"]], writes=[r["lfb"]])
                        for h in range(4):
                            S.op("pe", lambda e, h=h, c0=c0: e.matmul(out=bAB[0:64, h * 64:(h + 1) * 64], lhsT=kTm[:, h, c0:c0 + 64], rhs=qTm[:, h, c0:c0 + 64], start=True, stop=True),
                                 reads=[r["kTm"], r["qTm"]], writes=[r["bA"]])
                            S.op("pe", lambda e, h=h: e.matmul(out=bAB[0:64, 256 + h * 64:256 + (h + 1) * 64], lhsT=lfb[:, h, :], rhs=triu64b[:], start=True, stop=False),
                                 reads=[r["lfb"]], writes=[r["bB"]])
                            S.op("pe", lambda e, h=h: e.matmul(out=bAB[0:64, 256 + h * 64:256 + (h + 1) * 64], lhsT=lfb[:, 4 + h, :], rhs=triu64b[:], start=False, stop=False),
                                 reads=[r["lfb"]], writes=[r["bB"]])
                            S.op("pe", lambda e, h=h: e.matmul(out=bAB[0:64, 256 + h * 64:256 + (h + 1) * 64], lhsT=identb[0:64, 0:64], rhs=negmb[:], start=False, stop=True),
                                 reads=[r["lfb"]], writes=[r["bB"]])
                        for h in range(4):
                            S.op("act", lambda e, h=h: e.activation(out=DTs[:, h, :], in_=bAB[0:64, 256 + h * 64:256 + (h + 1) * 64], func=AF.Exp, scale=-1.0, bias=dcoef[:, h:h + 1]),
                                 reads=[r["bB"], r["dcoef"]], writes=[r["DTs"]])
                        S.op("dve", lambda e: e.tensor_tensor(out=WT[:].rearrange("p h t -> p (h t)"), in0=bAB[0:64, 0:256], in1=DTs[:].rearrange("p h t -> p (h t)"), op=ALU.mult),
                             reads=[r["bA"], r["DTs"]], writes=[r["WT"]])
                        for h in range(4):
                            S.op("pe", lambda e, h=h: e.matmul(out=bN[0:64, h * 128:(h + 1) * 128], lhsT=WT[:, h, :], rhs=vext[:, h * 128:(h + 1) * 128], start=True, stop=True),
                                 reads=[r["WT"], r["vext"]], writes=[r["bN"]])
                            S.op("pe", lambda e, h=h: e.matmul(out=bSm[0:64, 20 + h:21 + h], lhsT=WT[:, h, :], rhs=onecol[:, 0:1], start=True, stop=True),
                                 reads=[r["WT"], r["vext"]], writes=[r["bD"]])
                            S.op("pe", lambda e, h=h, c0=c0: e.matmul(out=bN2[0:64, h * 128:(h + 1) * 128], lhsT=qTm[:, h, c0:c0 + 64], rhs=Cbf[:, h * 128:(h + 1) * 128], start=True, stop=True),
                                 reads=[r["qTm"], r["Cbf"]], writes=[r["bN2"]])
                            S.op("pe", lambda e, h=h, c0=c0: e.matmul(out=bSm[0:64, 24 + h:25 + h], lhsT=qTm[:, h, c0:c0 + 64], rhs=nbf[:, h:h + 1], start=True, stop=True),
                                 reads=[r["qTm"], r["Cbf"]], writes=[r["bD"]])
                        S.op("dve", lambda e: e.tensor_tensor(out=n2s[:], in0=bN2[0:64, :].rearrange("p (h d) -> p h d", h=4), in1=alpha[:].unsqueeze(2).to_broadcast([64, 4, 128]), op=ALU.mult),
                             reads=[r["bN2"], r["alpha"]], writes=[r["n2s"]])
                        S.op("dve", lambda e: e.tensor_tensor(out=numsb[:], in0=bN[0:64, :].rearrange("p (h d) -> p h d", h=4), in1=n2s[:], op=ALU.add),
                             reads=[r["bN"], r["n2s"]], writes=[r["numsb"]])
                        S.op("act", lambda e: e.copy(out=dsb[:], in_=bSm[0:64, 20:28]), reads=[r["bD"]], writes=[r["dsb"]])
                        S.op("dve", lambda e: e.tensor_tensor(out=den[:], in0=dsb[:, 4:8], in1=alpha[:], op=ALU.mult), reads=[r["dsb"], r["alpha"]], writes=[r["den"]])
                        S.op("dve", lambda e: e.tensor_tensor(out=den[:], in0=den[:], in1=dsb[:, 0:4], op=ALU.add), reads=[r["dsb"], r["den"]], writes=[r["den"]])
                        S.op("dve", lambda e: e.tensor_scalar(out=ssh[:], in0=den[:], scalar1=-1.0, scalar2=None, op0=ALU.mult), reads=[r["den"]], writes=[r["ssh"]])
                        S.op("dve", lambda e: e.tensor_tensor(out=den[:], in0=den[:], in1=ssh[:], op=ALU.max), reads=[r["den"], r["ssh"]], writes=[r["den"]])
                        S.op("dve", lambda e: e.tensor_scalar_max(out=den[:], in0=den[:], scalar1=1.0), reads=[r["den"]], writes=[r["den"]])
                        S.op("dve", lambda e: e.reciprocal(out=den[:], in_=den[:]), reads=[r["den"]], writes=[r["den"]])
                        S.op("dve", lambda e: e.tensor_tensor(out=numsb[:], in0=numsb[:], in1=den[:].unsqueeze(2).to_broadcast([64, 4, 128]), op=ALU.mult),
                             reads=[r["numsb"], r["den"]], writes=[r["numsb"]])
                        S.op("dve", lambda e: e.tensor_tensor(out=sqh[:], in0=numsb[:], in1=numsb[:], op=ALU.mult), reads=[r["numsb"]], writes=[r["sqh"]])
                        S.op("dve", lambda e: e.tensor_reduce(out=ssh[:], in_=sqh[:], axis=AX.X, op=ALU.add), reads=[r["sqh"]], writes=[r["ssh"]])
                        S.op("dve", lambda e: e.tensor_scalar(out=ssh[:], in0=ssh[:], scalar1=1.0 / 128, scalar2=EPS, op0=ALU.mult, op1=ALU.add), reads=[r["ssh"]], writes=[r["ssh"]])
                        S.op("act", lambda e: e.sqrt(out=ssh[:], in_=ssh[:]), reads=[r["ssh"]], writes=[r["ssh"]])
                        S.op("dve", lambda e: e.reciprocal(out=ssh[:], in_=ssh[:]), reads=[r["ssh"]], writes=[r["ssh"]])
                        S.op("dve", lambda e: e.tensor_tensor(out=numsb[:], in0=numsb[:], in1=ssh[:].unsqueeze(2).to_broadcast([64, 4, 128]), op=ALU.mult),
                             reads=[r["numsb"], r["ssh"]], writes=[r["numsb"]])
                        S.op("dve", lambda e: e.tensor_tensor(out=numsb[:], in0=numsb[:], in1=gmht[:].rearrange("p (h d) -> p h d", h=4), op=ALU.mult),
                             reads=[r["numsb"], r["small"]], writes=[r["numsb"]])
                        S.op("act", lambda e: e.activation(out=sig[:], in_=bO[0:64, :], func=AF.Sigmoid), reads=[r["bO"]], writes=[r["sig"]])
                        S.op("dve", lambda e: e.tensor_tensor(out=mixtok[:], in0=numsb[:], in1=sig[:].rearrange("p (h d) -> p h d", h=4), op=ALU.mult),
                             reads=[r["numsb"], r["sig"]], writes=[r["mixtok"]])
                        if dbg and G == NGP and ci == 1:
                            rdb = R("dbg")
                            dl = [(gsb, 0, 8), (lfs, 8, 4), (bs, 12, 8), (alpha, 20, 4), (gamma, 24, 4), (dcoef, 28, 4), (den, 32, 4), (dsb, 36, 8), (ssh, 44, 4)]
                            for (tt_, o_, n_) in dl:
                                S.dma("sp", lambda e, tt_=tt_, o_=o_, n_=n_: e.dma_start(out=dbgA[:, o_:o_ + n_], in_=tt_[:]), reads=[r["numsb"], r["ssh"], r["den"]], writes=[rdb])
                            S.dma("sp", lambda e: e.dma_start(out=dbgA[:, 64:320], in_=DTs[:].rearrange("p h t -> p (h t)")), reads=[r["DTs"]], writes=[rdb])
                            S.dma("sp", lambda e: e.dma_start(out=dbgA[:, 512:1024], in_=numsb[:].rearrange("p h t -> p (h t)")), reads=[r["numsb"]], writes=[rdb])
                            S.dma("sp", lambda e: e.dma_start(out=dbgA[:, 1024:1536], in_=n2s[:].rearrange("p h t -> p (h t)")), reads=[r["n2s"]], writes=[rdb])
                            S.dma("sp", lambda e: e.dma_start(out=dbgA[:, 1536:2048], in_=sig[:]), reads=[r["sig"]], writes=[rdb])
                        for h in range(4):
                            S.op("pe", lambda e, h=h: e.transpose(out=bT[:, 512 + h * 64:512 + (h + 1) * 64], in_=mixtok[:, h, :], identity=identb[0:64, 0:64]),
                                 reads=[r["mixtok"]], writes=[r["bT"]])
                        evac(mixTg[:, :, c0:c0 + 64], bT[:, 512:768].rearrange("p (h t) -> p h t", h=4), [r["bT"]], [r["mixTg"]])
                    for h in range(4):
                        S.op("pe", lambda e, h=h: e.matmul(out=bN[:, h * 128:(h + 1) * 128], lhsT=ktok[:, h, :], rhs=gvext[:, h * 128:(h + 1) * 128], start=True, stop=True),
                             reads=[r["ktok"], r["gvext"]], writes=[r["bN"]])
                        S.op("pe", lambda e, h=h: e.matmul(out=bSm[:, 28 + h:29 + h], lhsT=ktok[:, h, :], rhs=gamb[:, h:h + 1], start=True, stop=True),
                             reads=[r["ktok"], r["gvext"]], writes=[r["bDn"]])
                    for h in range(4):
                        S.op("dve", lambda e, h=h: e.scalar_tensor_tensor(out=Cst[:, h, :], in0=Cst[:, h, :], scalar=eB[:, h:h + 1], in1=bN[:, h * 128:(h + 1) * 128], op0=ALU.mult, op1=ALU.add),
                             reads=[r["Cst"], r["eB"], r["bN"]], writes=[r["Cst"]])
                    S.op("dve", lambda e: e.tensor_tensor(out=nst[:], in0=nst[:], in1=eB[:], op=ALU.mult), reads=[r["nst"], r["eB"]], writes=[r["nst"]])
                    S.op("dve", lambda e: e.tensor_tensor(out=nst[:], in0=nst[:], in1=bSm[:, 28:32], op=ALU.add), reads=[r["nst"], r["bDn"]], writes=[r["nst"]])
                    S.op("act", lambda e: e.copy(out=Cbf[:], in_=Cst[:].rearrange("p h d -> p (h d)")), reads=[r["Cst"]], writes=[r["Cbf"]])
                    S.op("act", lambda e: e.copy(out=nbf[:], in_=nst[:]), reads=[r["nst"]], writes=[r["Cbf"]])
                if own:
                    go = G - NGP
                    for j in range(4):
                        S.dma("sp", lambda e, go=go, j=j: e.dma_start(out=mixTm_d[4 * go + j], in_=mixTg[:, :, j * 128:(j + 1) * 128]),
                              reads=[r["mixTg"]], writes=[r["mixTmd"]], sem_res=r["mixTg"])
            S.barrier()
            S.release(list(r.values()))

        def phase_B():
          with ExitStack() as eb:
            KTh = [sbt(eb, f"KTh{i}", [64, ST], BF16) for i in range(2)]
            Vh = [sbt(eb, f"Vh{i}", [128, NT, 64], BF16) for i in range(2)]
            QTh = [sbt(eb, f"QTh{i}", [64, SO], BF16) for i in range(2)]
            maskf = sbt(eb, "maskf", [128, 4, 512], F32)
            Es = [sbt(eb, f"Es{i}", [128, 512], F32) for i in range(2)]
            Ls = [sbt(eb, f"Ls{i}", [128, 512], BF16) for i in range(2)]
            Aes = [sbt(eb, f"Aes{i}", [128, 512], F32) for i in range(2)]
            As = [sbt(eb, f"As{i}", [128, 512], BF16) for i in range(2)]
            CSs = [sbt(eb, f"CSs{i}", [128, 512], BF16) for i in range(2)]
            ost = [sbt(eb, f"ost{i}", [64, 512], BF16) for i in range(2)]
            bZ = [pst(eb, f"bZ{i}", [128, 512], F32) for i in range(2)]
            bRC = [pst(eb, f"bRC{i}", [128, 512], F32) for i in range(2)]
            bOT = [pst(eb, f"bOT{i}", [128, 512], F32) for i in range(2)]
            rn = ["mask", "mixTsd"] + [f"{n}{i}" for n in ["KTh", "Vh", "QTh", "Es", "Ls", "Aes", "As", "CSs", "ost", "bZ", "bRC", "bOT"] for i in range(2)]
            r = {k: R(k) for k in rn}
            for m in range(4):
                S.op("pool", lambda e, m=m: e.memset(maskf[:, m, :], 1.0), writes=[r["mask"]])
                S.op("pool", lambda e, m=m: e.affine_select(out=maskf[:, m, :], in_=maskf[:, m, :], pattern=[[1, 512]], compare_op=ALU.is_ge,
                                                            fill=0.0, base=-(m * 128) - 1, channel_multiplier=-1), reads=[r["mask"]], writes=[r["mask"]])
            blk = 0
            for h in range(8):
                hs = h % 2
                S.dma("sp", lambda e, h=h, hs=hs: e.dma_start(out=KTh[hs][:], in_=KT_d[h]), writes=[r[f"KTh{hs}"]])
                S.dma("sp", lambda e, h=h, hs=hs: e.dma_start(out=Vh[hs][:], in_=V_d[h]), writes=[r[f"Vh{hs}"]])
                S.dma("sp", lambda e, h=h, hs=hs: e.dma_start(out=QTh[hs][:], in_=QT_d[h]), writes=[r[f"QTh{hs}"]])
                for gq in range(NGO):
                    oi = (h * NGO + gq) % 2
                    jd0 = (NGP + gq) * 4
                    jmax = jd0 + 3
                    first = True
                    prev_cs = None
                    for j in range(jmax, -1, -1):
                        b2 = blk % 2
                        blk += 1
                        m = j - jd0
                        S.op("pe", lambda e, hs=hs, j=j, gq=gq, b2=b2: e.matmul(out=bZ[b2][:], lhsT=KTh[hs][:, j * 128:(j + 1) * 128], rhs=QTh[hs][:, gq * 512:(gq + 1) * 512], start=True, stop=True),
                             reads=[r[f"KTh{hs}"], r[f"QTh{hs}"]], writes=[r[f"bZ{b2}"]])
                        S.op("act", lambda e, b2=b2: e.activation(out=Es[b2][:], in_=bZ[b2][:], func=AF.Exp), reads=[r[f"bZ{b2}"]], writes=[r[f"Es{b2}"]])
                        if m >= 0:
                            S.op("dve", lambda e, b2=b2, m=m: e.tensor_tensor(out=Es[b2][:], in0=Es[b2][:], in1=maskf[:, m, :], op=ALU.mult),
                                 reads=[r[f"Es{b2}"], r["mask"]], writes=[r[f"Es{b2}"]])
                        S.op("act", lambda e, b2=b2: e.activation(out=Ls[b2][:], in_=Es[b2][:], func=AF.Ln, bias=1.0), reads=[r[f"Es{b2}"]], writes=[r[f"Ls{b2}"]])
                        S.op("pe", lambda e, b2=b2, first=first: e.matmul(out=bRC[b2][:], lhsT=trilb[:], rhs=Ls[b2][:], start=True, stop=first),
                             reads=[r[f"Ls{b2}"]], writes=[r[f"bRC{b2}"]])
                        if not first:
                            S.op("pe", lambda e, b2=b2, pc=prev_cs: e.matmul(out=bRC[b2][:], lhsT=onesb[:], rhs=CSs[pc][:], start=False, stop=True),
                                 reads=[r[f"CSs{prev_cs}"]], writes=[r[f"bRC{b2}"]])
                        S.op("act", lambda e, b2=b2: e.activation(out=Aes[b2][:], in_=bRC[b2][:], func=AF.Exp, scale=-1.0), reads=[r[f"bRC{b2}"]], writes=[r[f"Aes{b2}"]])
                        S.op("dve", lambda e, b2=b2: e.tensor_tensor(out=As[b2][:], in0=Es[b2][:], in1=Aes[b2][:], op=ALU.mult),
                             reads=[r[f"Es{b2}"], r[f"Aes{b2}"]], writes=[r[f"As{b2}"]])
                        if j > 0:
                            if first:
                                S.op("pool", lambda e, b2=b2: e.tensor_copy(out=CSs[b2][:], in_=Ls[b2][:]), reads=[r[f"Ls{b2}"]], writes=[r[f"CSs{b2}"]])
                            else:
                                S.op("pool", lambda e, b2=b2, pc=prev_cs: e.tensor_tensor(out=CSs[b2][:], in0=CSs[pc][:], in1=Ls[b2][:], op=ALU.add),
                                     reads=[r[f"Ls{b2}"], r[f"CSs{prev_cs}"]], writes=[r[f"CSs{b2}"]])
                            prev_cs = b2
                        S.op("pe", lambda e, hs=hs, j=j, b2=b2, oi=oi, first=first: e.matmul(out=bOT[oi][0:64, :], lhsT=Vh[hs][:, j, :], rhs=As[b2][:], start=first, stop=(j == 0)),
                             reads=[r[f"Vh{hs}"], r[f"As{b2}"]], writes=[r[f"bOT{oi}"]])
                        first = False
                    S.op("act", lambda e, oi=oi: e.copy(out=ost[oi][:], in_=bOT[oi][0:64, :]), reads=[r[f"bOT{oi}"]], writes=[r[f"ost{oi}"]])
                    S.dma("sp", lambda e, oi=oi, gq=gq, h=h: e.dma_start(out=mixTs_d[4 * gq:4 * gq + 4, :, h, :].rearrange("j d t -> d j t"),
                                                                        in_=ost[oi][:].rearrange("d (j t) -> d j t", j=4)),
                          reads=[r[f"ost{oi}"]], writes=[r["mixTsd"]], sem_res=r[f"ost{oi}"])
            S.barrier()
            S.release(list(r.values()))

        def phase_C():
          NPASS = 4 if NTO >= 8 else 2
          NTH = NTO // NPASS
          TG = min(4, NTH)
          with ExitStack() as ec:
            gv = sbt(ec, "gv", [128, 4, D], F32)
            brt = sbt(ec, "brt", [128, 36], F32)
            wrt = sbt(ec, "wrt", [128, 8, 36], F32)
            acc = sbt(ec, "acc", [128, NTH, D], F32)
            cTb = sbt(ec, "cTb", [128, 8, NTH * 128], BF16)
            wfull = sbt(ec, "wfull", [128, NTH, 32], F32)
            xts = [sbt(ec, f"xc{i}", [128, D], F32) for i in range(2)]
            junk = sbt(ec, "junkc", [128, D], BF16)
            ssq = sbt(ec, "ssqc", [128, 2], F32)
            r0 = {k: R(k) for k in ["gv", "brt", "wrt", "acc", "cTb", "wfull", "xc0", "xc1", "junk", "ssq", "outd"]}
            S.dma("sp", lambda e: e.dma_start(out=gv[:], in_=gvec[1:5, :].partition_broadcast(128)), writes=[r0["gv"]])
            S.dma("sp", lambda e: e.dma_start(out=brt[:], in_=br.partition_broadcast(128)), writes=[r0["brt"]])
            S.dma("sp", lambda e: e.dma_start(out=wrt[:], in_=wr.rearrange("(c p) n -> p c n", p=128)), writes=[r0["wrt"]])

            for ps_i in range(NPASS):
                t0 = ps_i * NTH
                with ExitStack() as e1:
                    woutm = sbt(e1, "woutm", [128, 4, D], BF16)
                    wouts = sbt(e1, "wouts", [64, 8, D], BF16)
                    mTm = [sbt(e1, f"mTm{i}", [128, 4, 128], BF16) for i in range(2)]
                    mTs = [sbt(e1, f"mTs{i}", [64, 8, 128], BF16) for i in range(2)]
                    c32 = sbt(e1, "c32", [128, D], F32)
                    cT32 = sbt(e1, "cT32", [128, 8, 128], F32)
                    lg = sbt(e1, "lg", [128, 36], F32)
                    gmax = sbt(e1, "gmax", [128, 8], F32)
                    ohg = sbt(e1, "ohg", [128, 4], F32)
                    eg = sbt(e1, "eg", [128, 4], F32)
                    esel = sbt(e1, "esel", [128, 4, 8], F32)
                    es8 = sbt(e1, "es8", [128, 8], F32)
                    mk1 = sbt(e1, "mk1", [128, 8], F32)
                    mk2 = sbt(e1, "mk2", [128, 8], F32)
                    e2 = sbt(e1, "e2", [128, 8], F32)
                    wsel = sbt(e1, "wsel", [128, 8], F32)
                    bH = [pst(e1, f"bH{i}", [128, 512], F32) for i in range(2)]
                    bTf = [pst(e1, f"bTf{i}", [128, 512], F32) for i in range(2)]
                    bL = pst(e1, "bL", [128, 512], F32)
                    r = {k: R(k) for k in ["woutm", "wouts", "mTm0", "mTm1", "mTs0", "mTs1", "c32", "cT32", "lg", "rt", "bH0", "bH1", "bTf0", "bTf1", "bL"]}
                    S.dma("pool", lambda e: e.dma_start(out=woutm[:], in_=wout[0:512, :].rearrange("(h p) n -> p h n", p=128)), writes=[r["woutm"]])
                    S.dma("pool", lambda e: e.dma_start(out=wouts[:], in_=wout[512:1024, :].rearrange("(h p) n -> p h n", p=64)), writes=[r["wouts"]])
                    for tl in range(NTH):
                        t = t0 + tl
                        xi = tl % 2
                        xt = xts[xi]
                        rx = r0[f"xc{xi}"]
                        S.dma("sp", lambda e, xt=xt, t=t: e.dma_start(out=xt[:], in_=xo[t * 128:(t + 1) * 128, :]), writes=[rx])
                        S.dma("sp", lambda e, xi=xi, t=t: e.dma_start(out=mTm[xi][:], in_=mixTm_d[t]), writes=[r[f"mTm{xi}"]])
                        S.dma("sp", lambda e, xi=xi, t=t: e.dma_start(out=mTs[xi][:], in_=mixTs_d[t]), writes=[r[f"mTs{xi}"]])
                        for dh in range(2):
                            for h in range(4):
                                S.op("pe", lambda e, h=h, dh=dh, xi=xi: e.matmul(out=bH[dh][:], lhsT=mTm[xi][:, h, :], rhs=woutm[:, h, dh * 512:(dh + 1) * 512], start=(h == 0), stop=False),
                                     reads=[r[f"mTm{xi}"], r["woutm"]], writes=[r[f"bH{dh}"]])
                            for h in range(8):
                                S.op("pe", lambda e, h=h, dh=dh, xi=xi: e.matmul(out=bH[dh][:], lhsT=mTs[xi][:, h, :], rhs=wouts[:, h, dh * 512:(dh + 1) * 512], start=False, stop=(h == 7)),
                                     reads=[r[f"mTs{xi}"], r["wouts"]], writes=[r[f"bH{dh}"]])
                            S.op("dve", lambda e, dh=dh, tl=tl, xt=xt: e.tensor_tensor(out=acc[:, tl, dh * 512:(dh + 1) * 512], in0=bH[dh][:], in1=xt[:, dh * 512:(dh + 1) * 512], op=ALU.add),
                                 reads=[r[f"bH{dh}"], rx], writes=[r0["acc"]])
                        rmsnorm_stats(acc[:, tl, :], junk[:], ssq[:, 0:1], ssq[:, 1:2], r0["acc"], r0["junk"], r0["ssq"], D)
                        S.op("dve", lambda e, tl=tl: e.scalar_tensor_tensor(out=c32[:], in0=acc[:, tl, :], scalar=ssq[:, 1:2], in1=gv[:, 0, :], op0=ALU.mult, op1=ALU.mult),
                             reads=[r0["acc"], r0["ssq"], r0["gv"]], writes=[r["c32"]])
                        for c in range(8):
                            S.op("pe", lambda e, c=c: e.transpose(out=bTf[c // 4][:, (c % 4) * 128:(c % 4 + 1) * 128], in_=c32[:, c * 128:(c + 1) * 128], identity=identf[:]),
                                 reads=[r["c32"]], writes=[r[f"bTf{c // 4}"]])
                        for hh in range(2):
                            S.op("act", lambda e, hh=hh: e.copy(out=cT32[:, hh * 4:(hh + 1) * 4, :], in_=bTf[hh][:].rearrange("p (c t) -> p c t", c=4)),
                                 reads=[r[f"bTf{hh}"]], writes=[r["cT32"]])
                            S.op("dve", lambda e, hh=hh, tl=tl: e.tensor_copy(out=cTb[:, hh * 4:(hh + 1) * 4, tl * 128:(tl + 1) * 128], in_=cT32[:, hh * 4:(hh + 1) * 4, :]),
                                 reads=[r["cT32"]], writes=[r0["cTb"]])
                        for c in range(8):
                            S.op("pe", lambda e, c=c: e.matmul(out=bL[:, 0:36], lhsT=cT32[:, c, :], rhs=wrt[:, c, :], start=(c == 0), stop=(c == 7)),
                                 reads=[r["cT32"], r0["wrt"]], writes=[r["bL"]])
                        rt = r["rt"]
                        S.op("dve", lambda e: e.tensor_tensor(out=lg[:], in0=bL[:, 0:36], in1=brt[:], op=ALU.add), reads=[r["bL"], r0["brt"]], writes=[rt])
                        S.op("dve", lambda e: e.tensor_reduce(out=gmax[:, 0:1], in_=lg[:, 0:4], axis=AX.X, op=ALU.max), reads=[rt], writes=[rt])
                        S.op("dve", lambda e: e.tensor_scalar(out=ohg[:], in0=lg[:, 0:4], scalar1=gmax[:, 0:1], scalar2=None, op0=ALU.is_equal), reads=[rt], writes=[rt])
                        S.op("dve", lambda e: e.tensor_scalar(out=eg[:], in0=lg[:, 0:4], scalar1=gmax[:, 0:1], scalar2=None, op0=ALU.subtract), reads=[rt], writes=[rt])
                        S.op("pool", lambda e: e.memset(gmax[:, 1:2], 0.0), reads=[rt], writes=[rt])
                        S.op("act", lambda e: e.activation(out=eg[:], in_=eg[:], func=AF.Exp, accum_out=gmax[:, 1:2]), reads=[rt], writes=[rt])
                        S.op("dve", lambda e: e.reciprocal(out=gmax[:, 2:3], in_=gmax[:, 1:2]), reads=[rt], writes=[rt])
                        S.op("dve", lambda e: e.tensor_tensor(out=esel[:], in0=lg[:, 4:36].rearrange("p (g j) -> p g j", g=4), in1=ohg[:].unsqueeze(2).to_broadcast([128, 4, 8]), op=ALU.mult),
                             reads=[rt], writes=[rt])
                        S.op("dve", lambda e: e.tensor_reduce(out=es8[:], in_=esel[:].rearrange("p g j -> p j g"), axis=AX.X, op=ALU.add), reads=[rt], writes=[rt])
                        S.op("dve", lambda e: e.tensor_reduce(out=gmax[:, 3:4], in_=es8[:], axis=AX.X, op=ALU.max), reads=[rt], writes=[rt])
                        S.op("dve", lambda e: e.tensor_scalar(out=mk1[:], in0=es8[:], scalar1=gmax[:, 3:4], scalar2=None, op0=ALU.is_equal), reads=[rt], writes=[rt])
                        S.op("dve", lambda e: e.scalar_tensor_tensor(out=e2[:], in0=mk1[:], scalar=-1e30, in1=es8[:], op0=ALU.mult, op1=ALU.add), reads=[rt], writes=[rt])
                        S.op("dve", lambda e: e.tensor_reduce(out=gmax[:, 4:5], in_=e2[:], axis=AX.X, op=ALU.max), reads=[rt], writes=[rt])
                        S.op("dve", lambda e: e.tensor_scalar(out=mk2[:], in0=e2[:], scalar1=gmax[:, 4:5], scalar2=None, op0=ALU.is_equal), reads=[rt], writes=[rt])
                        S.op("dve", lambda e: e.tensor_tensor(out=gmax[:, 5:6], in0=gmax[:, 4:5], in1=gmax[:, 3:4], op=ALU.subtract), reads=[rt], writes=[rt])
                        S.op("act", lambda e: e.activation(out=gmax[:, 5:6], in_=gmax[:, 5:6], func=AF.Exp), reads=[rt], writes=[rt])
                        S.op("dve", lambda e: e.tensor_scalar(out=gmax[:, 6:7], in0=gmax[:, 5:6], scalar1=1.0, scalar2=None, op0=ALU.add), reads=[rt], writes=[rt])
                        S.op("dve", lambda e: e.reciprocal(out=gmax[:, 6:7], in_=gmax[:, 6:7]), reads=[rt], writes=[rt])
                        S.op("dve", lambda e: e.tensor_tensor(out=gmax[:, 6:7], in0=gmax[:, 6:7], in1=gmax[:, 2:3], op=ALU.mult), reads=[rt], writes=[rt])
                        S.op("dve", lambda e: e.tensor_tensor(out=gmax[:, 7:8], in0=gmax[:, 6:7], in1=gmax[:, 5:6], op=ALU.mult), reads=[rt], writes=[rt])
                        S.op("dve", lambda e: e.tensor_scalar(out=wsel[:], in0=mk1[:], scalar1=gmax[:, 6:7], scalar2=None, op0=ALU.mult), reads=[rt], writes=[rt])
                        S.op("dve", lambda e: e.scalar_tensor_tensor(out=wsel[:], in0=mk2[:], scalar=gmax[:, 7:8], in1=wsel[:], op0=ALU.mult, op1=ALU.add), reads=[rt], writes=[rt])
                        for g in range(4):
                            S.op("dve", lambda e, g=g, tl=tl: e.tensor_scalar(out=wfull[:, tl, g * 8:(g + 1) * 8], in0=wsel[:], scalar1=ohg[:, g:g + 1], scalar2=None, op0=ALU.mult),
                                 reads=[rt], writes=[r0["wfull"]])
                    S.barrier()
                    S.release(list(r.values()))

                with ExitStack() as e2s:
                    wgt = [sbt(e2s, f"wgt{i}", [128, 8, 512], BF16) for i in range(2)]
                    wut = [sbt(e2s, f"wut{i}", [128, 8, 512], BF16) for i in range(2)]
                    wdt = [sbt(e2s, f"wdt{i}", [128, 4, D], BF16) for i in range(2)]
                    sgs = [sbt(e2s, f"sgs{i}", [128, TG * 128], F32) for i in range(2)]
                    hid = [sbt(e2s, f"hid{i}", [128, 4, TG * 128], BF16) for i in range(2)]
                    bGt = [pst(e2s, f"bGt{i}", [128, 512], F32) for i in range(2)]
                    bUt = [pst(e2s, f"bUt{i}", [128, 512], F32) for i in range(2)]
                    bY = [pst(e2s, f"bY{i}", [128, 512], F32) for i in range(2)]
                    r = {k: R(k) for k in ["wgt0", "wgt1", "wut0", "wut1", "wdt0", "wdt1", "sgs0", "sgs1", "hid0", "hid1", "bGt0", "bGt1", "bUt0", "bUt1", "bY0", "bY1"]}
                    NW = TG * 128
                    cnt = 0
                    ycnt = 0

                    def load_exp(ex):
                        s_ = ex % 2
                        S.dma("pool", lambda e: e.dma_start(out=wgt[s_][:], in_=wg[ex].rearrange("(c p) n -> p c n", p=128)), writes=[r[f"wgt{s_}"]])
                        S.dma("pool", lambda e: e.dma_start(out=wut[s_][:], in_=wu[ex].rearrange("(c p) n -> p c n", p=128)), writes=[r[f"wut{s_}"]])
                        S.dma("pool", lambda e: e.dma_start(out=wdt[s_][:], in_=wd[ex].rearrange("(c p) n -> p c n", p=128)), writes=[r[f"wdt{s_}"]])

                    load_exp(0)
                    for ex in range(NEXP):
                        s_ = ex % 2
                        if ex + 1 < NEXP:
                            load_exp(ex + 1)
                        for tg in range(NTH // TG):
                            hb = (ex * (NTH // TG) + tg) % 2
                            for fc in range(4):
                                b2 = cnt % 2
                                cnt += 1
                                for c in range(8):
                                    S.op("pe", lambda e, c=c, fc=fc, b2=b2, s_=s_, tg=tg: e.matmul(out=bGt[b2][:, 0:NW], lhsT=wgt[s_][:, c, fc * 128:(fc + 1) * 128],
                                                                                                  rhs=cTb[:, c, tg * NW:(tg + 1) * NW], start=(c == 0), stop=(c == 7)),
                                         reads=[r[f"wgt{s_}"], r0["cTb"]], writes=[r[f"bGt{b2}"]])
                                for c in range(8):
                                    S.op("pe", lambda e, c=c, fc=fc, b2=b2, s_=s_, tg=tg: e.matmul(out=bUt[b2][:, 0:NW], lhsT=wut[s_][:, c, fc * 128:(fc + 1) * 128],
                                                                                                  rhs=cTb[:, c, tg * NW:(tg + 1) * NW], start=(c == 0), stop=(c == 7)),
                                         reads=[r[f"wut{s_}"], r0["cTb"]], writes=[r[f"bUt{b2}"]])
                                S.op("act", lambda e, b2=b2: e.activation(out=sgs[b2][:], in_=bGt[b2][:, 0:NW], func=AF.Silu), reads=[r[f"bGt{b2}"]], writes=[r[f"sgs{b2}"]])
                                S.op("dve", lambda e, b2=b2, hb=hb, fc=fc: e.tensor_tensor(out=hid[hb][:, fc, :], in0=sgs[b2][:], in1=bUt[b2][:, 0:NW], op=ALU.mult),
                                     reads=[r[f"sgs{b2}"], r[f"bUt{b2}"]], writes=[r[f"hid{hb}"]])
                            for tt in range(TG):
                                tl = tg * TG + tt
                                for dh in range(2):
                                    yb = ycnt % 2
                                    ycnt += 1
                                    for fc in range(4):
                                        S.op("pe", lambda e, fc=fc, hb=hb, tt=tt, dh=dh, yb=yb, s_=s_: e.matmul(out=bY[yb][:], lhsT=hid[hb][:, fc, tt * 128:(tt + 1) * 128],
                                                                                                               rhs=wdt[s_][:, fc, dh * 512:(dh + 1) * 512], start=(fc == 0), stop=(fc == 3)),
                                             reads=[r[f"hid{hb}"], r[f"wdt{s_}"]], writes=[r[f"bY{yb}"]])
                                    S.op("dve", lambda e, yb=yb, tl=tl, dh=dh, ex=ex: e.scalar_tensor_tensor(out=acc[:, tl, dh * 512:(dh + 1) * 512], in0=bY[yb][:], scalar=wfull[:, tl, ex:ex + 1],
                                                                                                           in1=acc[:, tl, dh * 512:(dh + 1) * 512], op0=ALU.mult, op1=ALU.add),
                                         reads=[r[f"bY{yb}"], r0["wfull"], r0["acc"]], writes=[r0["acc"]])
                    S.barrier()
                    S.release(list(r.values()))

                with ExitStack() as e3:
                    wpgt = sbt(e3, "wpgt", [128, 8, D], BF16)
                    wppt = sbt(e3, "wppt", [128, 2, D], BF16)
                    n_bf = sbt(e3, "n_bf", [128, D], BF16)
                    nT = sbt(e3, "nT", [128, 8, 128], BF16)
                    gate = sbt(e3, "gate", [128, D], F32)
                    pts = [sbt(e3, f"pt{i}", [128, 256], F32) for i in range(2)]
                    p_bf = sbt(e3, "p_bf", [128, 256], BF16)
                    pT = sbt(e3, "pT", [128, 2, 128], BF16)
                    ple = sbt(e3, "ple", [128, D], F32)
                    ss2 = sbt(e3, "ss2", [128, 4], F32)
                    h3 = sbt(e3, "h3", [128, D], F32)
                    ots = [sbt(e3, f"ot{i}", [128, D], F32) for i in range(2)]
                    bT2 = pst(e3, "bT2", [128, 1024], BF16)
                    bGa = [pst(e3, f"bGa{i}", [128, 512], F32) for i in range(2)]
                    bP = [pst(e3, f"bP{i}", [128, 512], F32) for i in range(2)]
                    r = {k: R(k) for k in ["wpgt", "wppt", "n_bf", "nT", "gate", "pt0", "pt1", "p_bf", "pT", "ple", "ss2", "h3", "ot0", "ot1", "bT2", "bGa0", "bGa1", "bP0", "bP1", "junk2"]}
                    S.dma("pool", lambda e: e.dma_start(out=wpgt[:], in_=wpg.rearrange("(c p) n -> p c n", p=128)), writes=[r["wpgt"]])
                    S.dma("pool", lambda e: e.dma_start(out=wppt[:], in_=wpp.rearrange("(c p) n -> p c n", p=128)), writes=[r["wppt"]])
                    for tl in range(NTH):
                        t = t0 + tl
                        pi = tl % 2
                        S.dma("sp", lambda e, pi=pi, t=t: e.dma_start(out=pts[pi][:], in_=po[t * 128:(t + 1) * 128, :]), writes=[r[f"pt{pi}"]])
                        rmsnorm_stats(acc[:, tl, :], junk[:], ssq[:, 0:1], ssq[:, 1:2], r0["acc"], r0["junk"], r0["ssq"], D)
                        S.op("dve", lambda e, tl=tl: e.scalar_tensor_tensor(out=n_bf[:], in0=acc[:, tl, :], scalar=ssq[:, 1:2], in1=gv[:, 1, :], op0=ALU.mult, op1=ALU.mult),
                             reads=[r0["acc"], r0["ssq"], r0["gv"]], writes=[r["n_bf"]])
                        for c in range(8):
                            S.op("pe", lambda e, c=c: e.transpose(out=bT2[:, c * 128:(c + 1) * 128], in_=n_bf[:, c * 128:(c + 1) * 128], identity=identb[:]),
                                 reads=[r["n_bf"]], writes=[r["bT2"]])
                        S.op("act", lambda e: e.copy(out=nT[:], in_=bT2[:].rearrange("p (c t) -> p c t", c=8)), reads=[r["bT2"]], writes=[r["nT"]])
                        for dh in range(2):
                            for c in range(8):
                                S.op("pe", lambda e, c=c, dh=dh: e.matmul(out=bGa[dh][:], lhsT=nT[:, c, :], rhs=wpgt[:, c, dh * 512:(dh + 1) * 512], start=(c == 0), stop=(c == 7)),
                                     reads=[r["nT"], r["wpgt"]], writes=[r[f"bGa{dh}"]])
                            S.op("act", lambda e, dh=dh: e.activation(out=gate[:, dh * 512:(dh + 1) * 512], in_=bGa[dh][:], func=AF.Sigmoid), reads=[r[f"bGa{dh}"]], writes=[r["gate"]])
                        S.op("dve", lambda e, pi=pi: e.tensor_copy(out=p_bf[:], in_=pts[pi][:]), reads=[r[f"pt{pi}"]], writes=[r["p_bf"]])
                        for c in range(2):
                            S.op("pe", lambda e, c=c: e.transpose(out=bT2[:, c * 128:(c + 1) * 128], in_=p_bf[:, c * 128:(c + 1) * 128], identity=identb[:]),
                                 reads=[r["p_bf"]], writes=[r["bT2"]])
                        S.op("act", lambda e: e.copy(out=pT[:], in_=bT2[:, 0:256].rearrange("p (c t) -> p c t", c=2)), reads=[r["bT2"]], writes=[r["pT"]])
                        for dh in range(2):
                            for c in range(2):
                                S.op("pe", lambda e, c=c, dh=dh: e.matmul(out=bP[dh][:], lhsT=pT[:, c, :], rhs=wppt[:, c, dh * 512:(dh + 1) * 512], start=(c == 0), stop=(c == 1)),
                                     reads=[r["pT"], r["wppt"]], writes=[r[f"bP{dh}"]])
                            S.op("act", lambda e, dh=dh: e.copy(out=ple[:, dh * 512:(dh + 1) * 512], in_=bP[dh][:]), reads=[r[f"bP{dh}"]], writes=[r["ple"]])
                        rmsnorm_stats(ple[:], junk[:], ss2[:, 0:1], ss2[:, 1:2], r["ple"], r0["junk"], r["ss2"], D)
                        S.op("dve", lambda e: e.scalar_tensor_tensor(out=ple[:], in0=ple[:], scalar=ss2[:, 1:2], in1=gv[:, 2, :], op0=ALU.mult, op1=ALU.mult),
                             reads=[r["ple"], r["ss2"], r0["gv"]], writes=[r["ple"]])
                        S.op("dve", lambda e: e.tensor_tensor(out=ple[:], in0=ple[:], in1=gate[:], op=ALU.mult), reads=[r["ple"], r["gate"]], writes=[r["ple"]])
                        S.op("dve", lambda e, tl=tl: e.tensor_tensor(out=h3[:], in0=ple[:], in1=acc[:, tl, :], op=ALU.add), reads=[r["ple"], r0["acc"]], writes=[r["h3"]])
                        rmsnorm_stats(h3[:], junk[:], ss2[:, 2:3], ss2[:, 3:4], r["h3"], r0["junk"], r["ss2"], D)
                        S.op("dve", lambda e, pi=pi: e.scalar_tensor_tensor(out=ots[pi][:], in0=h3[:], scalar=ss2[:, 3:4], in1=gv[:, 3, :], op0=ALU.mult, op1=ALU.mult),
                             reads=[r["h3"], r["ss2"], r0["gv"]], writes=[r[f"ot{pi}"]])
                        S.dma("sp", lambda e, pi=pi, t=t: e.dma_start(out=out[t * 128:(t + 1) * 128, :], in_=ots[pi][:]), reads=[r[f"ot{pi}"]], writes=[r0["outd"]], sem_res=r[f"ot{pi}"])
                    S.barrier()
                    S.release(list(r.values()))
        if 'A' in phases:
            phase_A()
        if 'B' in phases:
            phase_B()
        if 'C' in phases:
            phase_C()
        S.barrier()
        build_nc.last_log = S.log
        S.emit(block)
    return nc


def prep_core_inputs(inp, b, half, SP, SO):
    x = inp["x"]
    f = np.float32
    if half == 0:
        xp = np.zeros((SP, D), f)
    else:
        xp = np.ascontiguousarray(x[b, 0:SP])
    xo = np.ascontiguousarray(x[b, half * SP: half * SP + SO]) if half == 1 else np.ascontiguousarray(x[b, 0:SO])
    p = inp["p"][0, b]
    po = np.ascontiguousarray(p[half * SP: half * SP + SO]) if half == 1 else np.ascontiguousarray(p[0:SO])
    cq = np.ascontiguousarray(inp["conv_q"][0].T.reshape(4, 128, 4).transpose(1, 0, 2).reshape(128, 16))
    ck = np.ascontiguousarray(inp["conv_k"][0].T.reshape(4, 128, 4).transpose(1, 0, 2).reshape(128, 16))
    gvec = np.stack([inp["g_mix"][0], inp["g_ffn"][0], inp["g_ple"][0], inp["g_ple_post"][0], inp["g_final"]]).astype(f)
    wr = np.concatenate([inp["w_router_group"][0], inp["w_router_expert"][0]], axis=1).astype(f)
    br = np.concatenate([inp["b_router_group"][0], inp["b_router_expert"][0]])[None, :].astype(f)
    return {
        "xp": xp, "xo": xo, "po": po,
        "win": np.ascontiguousarray(inp["w_in"][0]),
        "gvec": np.ascontiguousarray(gvec),
        "gmh": np.ascontiguousarray(inp["g_mhead"]),
        "bg": np.ascontiguousarray(inp["b_gates"]),
        "cq": cq, "ck": ck,
        "wout": np.ascontiguousarray(inp["w_out"][0]),
        "wr": np.ascontiguousarray(wr), "br": np.ascontiguousarray(br),
        "wg": np.ascontiguousarray(inp["w_exp_gate"][0]),
        "wu": np.ascontiguousarray(inp["w_exp_up"][0]),
        "wd": np.ascontiguousarray(inp["w_exp_down"][0]),
        "wpg": np.ascontiguousarray(inp["w_ple_gate"][0]),
        "wpp": np.ascontiguousarray(inp["w_ple_proj"][0]),
    }


def kernel(**inputs):
    inp = {k: np.asarray(v) for k, v in inputs.items()}
    x = inp["x"]
    B, SEQ, _ = x.shape
    SH = SEQ // 2
    nc = build_nc(SH, SH)
    in_maps = []
    for c in range(8):
        b, half = c // 2, c % 2
        in_maps.append(prep_core_inputs(inp, b, half, SH, SH))
    res = run_bass_kernel_spmd(nc, in_maps, core_ids=list(range(8)))
    out = np.empty((B, SEQ, D), np.float32)
    for c in range(8):
        b, half = c // 2, c % 2
        out[b, half * SH:(half + 1) * SH] = res.results[c]["out"]
    return out
```

```python
import math
from contextlib import ExitStack
import numpy as np
import concourse.bass as bass
import concourse.mybir as mybir
from concourse.bass_utils import run_bass_kernel_spmd

F32 = mybir.dt.float32
BF16 = mybir.dt.bfloat16
AF = mybir.ActivationFunctionType
ALU = mybir.AluOpType
AX = mybir.AxisListType

D = 1024
NCOL = 3592
EPS = 1e-6
LNC = math.log(128.0 ** -0.5)
NEXP = 32


class Res:
    __slots__ = ("name", "last_w", "readers", "dsem")

    def __init__(self, name):
        self.name = name
        self.last_w = None
        self.readers = []
        self.dsem = None


class Sched:
    ENGS = ("pe", "act", "dve", "pool", "sp")

    def __init__(self, nc):
        self.nc = nc
        self.ops = {e: [] for e in self.ENGS}
        self.cnt = {e: 0 for e in self.ENGS}
        self.sem = {}
        self.waited = {e: {} for e in self.ENGS}
        self.dcnt = {}
        self.dsem_objs = {}
        self.free_dsems = []
        import os
        self.limit = int(os.environ.get("KSTOP", "0")) or None
        self.total = 0
        self.log = []

    def set_sems(self, sems, dma_sems):
        for e, s in zip(self.ENGS, sems):
            self.sem[e] = s
        self.free_dsems = list(dma_sems)

    def res(self, name):
        return Res(name)

    def _deps(self, eng, reads, writes, self_sync):
        deps = {}

        def add(ev):
            if ev is None:
                return
            s, v, src = ev
            if src == eng and not self_sync:
                return
            k = id(s)
            if k not in deps or deps[k][1] < v:
                deps[k] = (s, v)
        for r in reads:
            add(r.last_w)
        for w in writes:
            add(w.last_w)
            for e in w.readers:
                add(e)
        waits = []
        wd = self.waited[eng]
        for k, (s, v) in deps.items():
            if wd.get(k, 0) < v:
                wd[k] = v
                waits.append((s, v))
        return waits

    def _commit(self, ev, reads, writes):
        for r in reads:
            r.readers.append(ev)
            if len(r.readers) > 64:
                r.readers = r.readers[-48:] if False else r.readers
        for w in writes:
            w.last_w = ev
            w.readers = []

    def op(self, eng, fn, reads=(), writes=(), self_sync=None):
        self.total += 1
        if self.limit and self.total > self.limit:
            return None
        import sys as _sys
        self.log.append((self.total, eng, _sys._getframe(1).f_lineno))
        if self_sync is None:
            self_sync = eng != "pe"
        waits = self._deps(eng, reads, writes, self_sync)
        self.cnt[eng] += 1
        ev = (self.sem[eng], self.cnt[eng], eng)
        self.ops[eng].append((waits, fn, (self.sem[eng], 1)))
        self._commit(ev, reads, writes)
        return ev

    def dma(self, eng, fn, reads=(), writes=(), sem_res=None):
        self.total += 1
        if self.limit and self.total > self.limit:
            return None
        import sys as _sys
        self.log.append((self.total, "dma-" + eng, _sys._getframe(1).f_lineno))
        owner = sem_res or (writes[0] if writes else reads[0])
        if owner.dsem is None:
            owner.dsem = self.free_dsems.pop()
            self.dcnt.setdefault(id(owner.dsem), 0)
            self.dsem_objs[id(owner.dsem)] = owner.dsem
        s = owner.dsem
        waits = self._deps(eng, reads, writes, True)
        self.dcnt[id(s)] += 16
        ev = (s, self.dcnt[id(s)], "dma")
        self.ops[eng].append((waits, fn, (s, 16)))
        self._commit(ev, reads, writes)
        return ev

    def release(self, res_list):
        for r in res_list:
            if r.dsem is not None:
                self.free_dsems.append(r.dsem)
                r.dsem = None

    def barrier(self):
        for eng in self.ENGS:
            waits = []
            wd = self.waited[eng]
            for x in self.ENGS:
                s, v = self.sem[x], self.cnt[x]
                if v > 0 and wd.get(id(s), 0) < v:
                    wd[id(s)] = v
                    waits.append((s, v))
            for k, v in self.dcnt.items():
                if v > 0 and wd.get(k, 0) < v:
                    wd[k] = v
                    waits.append((self.dsem_objs[k], v))
            if waits:
                self.ops[eng].append((waits, None, None))

    def emit(self, block):
        sched = self

        def run(engname, handle):
            for waits, fn, inc in sched.ops[engname]:
                for (s, v) in waits:
                    handle.wait_ge(s, v)
                if fn is not None:
                    ins = fn(handle)
                    ins.then_inc(inc[0], inc[1])

        @block.tensor
        def _(e):
            run("pe", e)

        @block.scalar
        def _(e):
            run("act", e)

        @block.vector
        def _(e):
            run("dve", e)

        @block.gpsimd
        def _(e):
            run("pool", e)

        @block.sync
        def _(e):
            run("sp", e)


def build_nc(SP, SO, phases="ABC", dbg=False):
    assert SP % 512 == 0 and SO % 512 == 0
    ST = SP + SO
    NGP, NGO = SP // 512, SO // 512
    NG = NGP + NGO
    NT = ST // 128
    NTO = SO // 128
    nc = bass.Bass("TRN2", target_bir_lowering=False)

    def din(name, shape, dt=F32):
        return nc.dram_tensor(name, list(shape), dt, kind="ExternalInput").ap()

    xp = din("xp", [SP, D])
    xo = din("xo", [SO, D])
    po = din("po", [SO, 256])
    win = din("win", [D, NCOL])
    gvec = din("gvec", [5, D])
    gmh = din("gmh", [1, 512])
    bg = din("bg", [1, 8])
    cq = din("cq", [128, 16])
    ck = din("ck", [128, 16])
    wout = din("wout", [D, D])
    wr = din("wr", [D, 36])
    br = din("br", [1, 36])
    wg = din("wg", [NEXP, D, 512])
    wu = din("wu", [NEXP, D, 512])
    wd = din("wd", [NEXP, 512, D])
    wpg = din("wpg", [D, D])
    wpp = din("wpp", [256, D])
    out = nc.dram_tensor("out", [SO, D], F32, kind="ExternalOutput").ap()

    skind = "ExternalOutput" if dbg else "Internal"
    dbgA = nc.dram_tensor("dbgA", [64, 2048], F32, kind=skind).ap()
    KT_d = nc.dram_tensor("KT_d", [8, 64, ST], BF16, kind=skind).ap()
    QT_d = nc.dram_tensor("QT_d", [8, 64, SO], BF16, kind=skind).ap()
    V_d = nc.dram_tensor("V_d", [8, 128, NT, 64], BF16, kind=skind).ap()
    mixTm_d = nc.dram_tensor("mixTm_d", [NTO, 128, 4, 128], BF16, kind=skind).ap()
    mixTs_d = nc.dram_tensor("mixTs_d", [NTO, 64, 8, 128], BF16, kind=skind).ap()

    es = ExitStack()
    with es:
        sems = [es.enter_context(nc.semaphore(f"s_{e}")) for e in Sched.ENGS]
        dsems = [es.enter_context(nc.semaphore(f"d_{i}")) for i in range(48)]
        block = es.enter_context(nc.Block())
        S = Sched(nc)
        S.set_sems(sems, dsems)
        R = S.res

        uid = [0]

        def sbt(stack, name, shape, dt):
            uid[0] += 1
            return stack.enter_context(nc.sbuf_tensor(f"{name}_{uid[0]}", list(shape), dt))

        def pst(stack, name, shape, dt):
            uid[0] += 1
            return stack.enter_context(nc.psum_tensor(f"{name}_{uid[0]}", list(shape), dt))

        identf = sbt(es, "identf", [128, 128], F32)
        identb = sbt(es, "identb", [128, 128], BF16)
        onesb = sbt(es, "onesb", [128, 128], BF16)
        trilb = sbt(es, "trilb", [128, 128], BF16)
        tmpf = sbt(es, "tmpf", [128, 128], F32)
        triu64 = sbt(es, "triu64", [64, 64], F32)
        ones64 = sbt(es, "ones64", [64, 128], F32)
        negm = sbt(es, "negm", [64, 64], F32)
        r_const = R("const")

        S.op("pool", lambda e: e.memset(identf[:], 0.0), writes=[r_const])
        S.op("pool", lambda e: e.affine_select(out=identf[:], in_=identf[:], pattern=[[-1, 128]], compare_op=ALU.not_equal,
                                               fill=1.0, base=0, channel_multiplier=1), reads=[r_const], writes=[r_const])
        S.op("dve", lambda e: e.tensor_copy(out=identb[:], in_=identf[:]), reads=[r_const], writes=[r_const])
        S.op("pool", lambda e: e.memset(onesb[:], 1.0), writes=[r_const])
        S.op("pool", lambda e: e.memset(tmpf[:], 1.0), writes=[r_const])
        S.op("pool", lambda e: e.affine_select(out=tmpf[:], in_=tmpf[:], pattern=[[-1, 128]], compare_op=ALU.is_ge,
                                               fill=0.0, base=0, channel_multiplier=1), reads=[r_const], writes=[r_const])
        S.op("dve", lambda e: e.tensor_copy(out=trilb[:], in_=tmpf[:]), reads=[r_const], writes=[r_const])
        S.op("pool", lambda e: e.memset(triu64[:], 1.0), writes=[r_const])
        S.op("pool", lambda e: e.affine_select(out=triu64[:], in_=triu64[:], pattern=[[1, 64]], compare_op=ALU.is_ge,
                                               fill=0.0, base=0, channel_multiplier=-1), reads=[r_const], writes=[r_const])
        S.op("pool", lambda e: e.memset(ones64[:], 1.0), writes=[r_const])
        S.op("pool", lambda e: e.memset(negm[:], 0.0), writes=[r_const])
        S.op("pool", lambda e: e.affine_select(out=negm[:], in_=negm[:], pattern=[[1, 64]], compare_op=ALU.is_ge,
                                               fill=30000.0, base=0, channel_multiplier=-1), reads=[r_const], writes=[r_const])
        triu64b = sbt(es, "triu64b", [64, 64], BF16)
        ones64b = sbt(es, "ones64b", [64, 128], BF16)
        negmb = sbt(es, "negmb", [64, 64], BF16)
        S.op("dve", lambda e: e.tensor_copy(out=triu64b[:], in_=triu64[:]), reads=[r_const], writes=[r_const])
        S.op("dve", lambda e: e.tensor_copy(out=ones64b[:], in_=ones64[:]), reads=[r_const], writes=[r_const])
        S.op("dve", lambda e: e.tensor_copy(out=negmb[:], in_=negm[:]), reads=[r_const], writes=[r_const])
        S.barrier()

        def rmsnorm_stats(src_ap, junk, ss, rstd, r_src, r_junk, r_ss, n):
            S.op("pool", lambda e: e.memset(ss, 0.0), writes=[r_ss])
            S.op("act", lambda e: e.activation(out=junk, in_=src_ap, func=AF.Square, accum_out=ss), reads=[r_src, r_ss], writes=[r_junk, r_ss])
            S.op("dve", lambda e: e.tensor_scalar(out=rstd, in0=ss, scalar1=1.0 / n, scalar2=EPS, op0=ALU.mult, op1=ALU.add),
                 reads=[r_ss], writes=[r_ss])
            S.op("act", lambda e: e.sqrt(out=rstd, in_=rstd), reads=[r_ss], writes=[r_ss])
            S.op("dve", lambda e: e.reciprocal(out=rstd, in_=rstd), reads=[r_ss], writes=[r_ss])

        def phase_A():
          with ExitStack() as ea:
            winb = sbt(ea, "winb", [128, 8, NCOL], BF16)
            gmix = sbt(ea, "gmix", [128, D], F32)
            gmht = sbt(ea, "gmht", [64, 512], F32)
            bgt = sbt(ea, "bgt", [64, 8], F32)
            cqt = sbt(ea, "cqt", [128, 16], F32)
            ckt = sbt(ea, "ckt", [128, 16], F32)
            xts = [sbt(ea, f"xt{i}", [128, D], F32) for i in range(2)]
            junk = sbt(ea, "junk", [128, D], BF16)
            ssq = sbt(ea, "ssq", [128, 2], F32)
            a_bf = sbt(ea, "a_bf", [128, D], BF16)
            aT = sbt(ea, "aT", [128, 8, 512], BF16)
            qpre = sbt(ea, "qpre", [128, 4, 515], F32)
            kpre = sbt(ea, "kpre", [128, 4, 515], F32)
            ctmp = [sbt(ea, f"ctmp{i}", [128, 512], F32) for i in range(2)]
            qTm = sbt(ea, "qTm", [128, 4, 512], BF16)
            kTm = sbt(ea, "kTm", [128, 4, 512], BF16)
            sqT = sbt(ea, "sqT", [128, 4, 512], BF16)
            skT = sbt(ea, "skT", [128, 4, 512], BF16)
            svt = sbt(ea, "svt", [128, 8, 4, 64], BF16)
            ktok = sbt(ea, "ktok", [64, 4, 128], BF16)
            vext = sbt(ea, "vext", [64, 512], BF16)
            onecol = sbt(ea, "onecol", [64, 2], BF16)
            gvext = sbt(ea, "gvext", [64, 512], BF16)
            gamb = sbt(ea, "gamb", [64, 4], BF16)
            gsb = sbt(ea, "gsb", [64, 8], F32)
            lfs = sbt(ea, "lfs", [64, 4], F32)
            lfb = sbt(ea, "lfb", [64, 8, 64], BF16)
            lf2 = sbt(ea, "lf2", [64, 8], BF16)
            lft = sbt(ea, "lft", [64, 4], F32)
            bs = sbt(ea, "bs", [64, 8], F32)
            alpha = sbt(ea, "alpha", [64, 4], F32)
            gamma = sbt(ea, "gamma", [64, 4], F32)
            garg = sbt(ea, "garg", [64, 4], F32)
            dcoef = sbt(ea, "dcoef", [64, 4], F32)
            eB = sbt(ea, "eB", [128, 4], F32)
            DTs = sbt(ea, "DTs", [64, 4, 64], F32)
            WT = sbt(ea, "WT", [64, 4, 64], BF16)
            n2s = sbt(ea, "n2s", [64, 4, 128], F32)
            numsb = sbt(ea, "numsb", [64, 4, 128], F32)
            sqh = sbt(ea, "sqh", [64, 4, 128], F32)
            dsb = sbt(ea, "dsb", [64, 8], F32)
            den = sbt(ea, "den", [64, 4], F32)
            ssh = sbt(ea, "ssh", [64, 4], F32)
            sig = sbt(ea, "sig", [64, 512], F32)
            mixtok = sbt(ea, "mixtok", [64, 4, 128], BF16)
            mixTg = sbt(ea, "mixTg", [128, 4, 512], BF16)
            Cst = sbt(ea, "Cst", [128, 4, 128], F32)
            nst = sbt(ea, "nst", [128, 4], F32)
            Cbf = sbt(ea, "Cbf", [128, 512], BF16)
            nbf = sbt(ea, "nbf", [128, 4], BF16)

            bF = pst(ea, "bF", [128, 512], F32)
            bSm = pst(ea, "bSm", [128, 512], F32)
            bV = pst(ea, "bV", [128, 512], F32)
            bO = pst(ea, "bO", [128, 512], F32)
            bAB = pst(ea, "bAB", [128, 512], F32)
            bN = pst(ea, "bN", [128, 512], F32)
            bN2 = pst(ea, "bN2", [128, 512], F32)
            bT = pst(ea, "bT", [128, 1024], BF16)

            names = ["winb", "gmix", "small", "xt0", "xt1", "junk", "ssq", "a_bf", "aT", "qpre", "kpre", "ctmp0", "ctmp1", "qTm", "kTm",
                     "sqT", "skT", "svt", "ktok", "vext", "gvext", "gsb", "lfs", "lfb", "bs", "alpha", "gamma", "garg", "dcoef", "eB",
                     "DTs", "WT", "n2s", "numsb", "sqh", "dsb", "den", "ssh", "sig", "mixtok", "mixTg", "Cst", "nst", "Cbf",
                     "bF", "bG", "bS", "bE", "bD", "bDn", "bV", "bO", "bA", "bB", "bN", "bN2", "bT",
                     "KTd", "QTd", "Vd", "mixTmd"]
            r = {k: R(k) for k in names}

            for c in range(8):
                S.dma("pool", lambda e, c=c: e.dma_start(out=winb[:, c, :], in_=win[c * 128:(c + 1) * 128, :]), writes=[r["winb"]])
            S.dma("sp", lambda e: e.dma_start(out=gmix[:], in_=gvec[0:1, :].partition_broadcast(128)), writes=[r["gmix"]])
            S.dma("sp", lambda e: e.dma_start(out=gmht[:], in_=gmh.partition_broadcast(64)), writes=[r["small"]])
            S.dma("sp", lambda e: e.dma_start(out=bgt[:], in_=bg.partition_broadcast(64)), writes=[r["small"]])
            S.dma("sp", lambda e: e.dma_start(out=cqt[:], in_=cq), writes=[r["small"]])
            S.dma("sp", lambda e: e.dma_start(out=ckt[:], in_=ck), writes=[r["small"]])
            S.op("pool", lambda e: e.memset(Cst[:], 0.0), writes=[r["Cst"]])
            S.op("pool", lambda e: e.memset(nst[:], 0.0), writes=[r["nst"]])
            S.op("pool", lambda e: e.memset(Cbf[:], 0.0), writes=[r["Cbf"]])
            S.op("pool", lambda e: e.memset(onecol[:], 1.0), writes=[r["vext"]])
            S.op("pool", lambda e: e.memset(nbf[:], 0.0), writes=[r["Cbf"]])
            S.op("pool", lambda e: e.memset(qpre[:], 0.0), writes=[r["qpre"]])
            S.op("pool", lambda e: e.memset(kpre[:], 0.0), writes=[r["kpre"]])

            evac_i = [0]

            def evac(out_ap, in_ap, reads, writes, scale=None):
                evac_i[0] += 1
                if scale is not None or evac_i[0] % 2 == 0:
                    if scale is None:
                        S.op("act", lambda e: e.copy(out=out_ap, in_=in_ap), reads=reads, writes=writes)
                    else:
                        S.op("act", lambda e: e.mul(out=out_ap, in_=in_ap, mul=scale), reads=reads, writes=writes)
                else:
                    S.op("dve", lambda e: e.tensor_copy(out=out_ap, in_=in_ap), reads=reads, writes=writes)

            for G in range(NG):
                own = G >= NGP
                src = xo if own else xp
                row0 = (G - NGP if own else G) * 512
                for ti in range(4):
                    xi = ti % 2
                    xt = xts[xi]
                    rx = r[f"xt{xi}"]
                    S.dma("sp", lambda e, xt=xt, a=row0 + ti * 128, src=src: e.dma_start(out=xt[:], in_=src[a:a + 128, :]), writes=[rx])
                    rmsnorm_stats(xt[:], junk[:], ssq[:, 0:1], ssq[:, 1:2], rx, r["junk"], r["ssq"], D)
                    S.op("dve", lambda e, xt=xt: e.scalar_tensor_tensor(out=a_bf[:], in0=xt[:], scalar=ssq[:, 1:2], in1=gmix[:], op0=ALU.mult, op1=ALU.mult),
                         reads=[rx, r["ssq"], r["gmix"]], writes=[r["a_bf"]])
                    for c in range(8):
                        S.op("pe", lambda e, c=c: e.transpose(out=bT[:, c * 128:(c + 1) * 128], in_=a_bf[:, c * 128:(c + 1) * 128], identity=identb[:]),
                             reads=[r["a_bf"]], writes=[r["bT"]])
                    evac(aT[:, :, ti * 128:(ti + 1) * 128], bT[:].rearrange("p (c t) -> p c t", c=8), [r["bT"]], [r["aT"]])

                def fproj(col0, dst_ap, r_dst, scale=None):
                    for c in range(8):
                        S.op("pe", lambda e, c=c: e.matmul(out=bF[:], lhsT=winb[:, c, col0:col0 + 128], rhs=aT[:, c, :], start=(c == 0), stop=(c == 7)),
                             reads=[r["winb"], r["aT"]], writes=[r["bF"]])
                    evac(dst_ap, bF[:], [r["bF"]], [r_dst], scale=scale)

                for h in range(4):
                    if own:
                        fproj(h * 128, qpre[:, h, 3:515], r["qpre"])
                    fproj(512 + h * 128, kpre[:, h, 3:515], r["kpre"])
                for j in range(4):
                    if own:
                        fproj(2056 + j * 128, sqT[:, j, :], r["sqT"], scale=0.125)
                    fproj(2568 + j * 128, skT[:, j, :], r["skT"])

                def conv(pre, cwt, dstT, r_pre, r_dst):
                    for h in range(4):
                        tmp = ctmp[h % 2]
                        rt = r[f"ctmp{h % 2}"]
                        S.op("dve", lambda e, h=h, tmp=tmp: e.tensor_scalar(out=tmp[:], in0=pre[:, h, 0:512], scalar1=cwt[:, h * 4:h * 4 + 1], scalar2=None, op0=ALU.mult),
                             reads=[r_pre, r["small"]], writes=[rt])
                        for j in range(1, 4):
                            S.op("dve", lambda e, h=h, j=j, tmp=tmp: e.scalar_tensor_tensor(out=tmp[:], in0=pre[:, h, j:j + 512], scalar=cwt[:, h * 4 + j:h * 4 + j + 1],
                                                                                          in1=tmp[:], op0=ALU.mult, op1=ALU.add),
                                 reads=[r_pre, rt], writes=[rt])
                        S.op("act", lambda e, h=h, tmp=tmp: e.activation(out=dstT[:, h, :], in_=tmp[:], func=AF.Silu), reads=[rt], writes=[r_dst])
                    S.op("pool", lambda e: e.tensor_copy(out=pre[:, :, 0:3], in_=pre[:, :, 512:515]), reads=[r_pre], writes=[r_pre])

                if own:
                    conv(qpre, cqt, qTm, r["qpre"], r["qTm"])
                else:
                    if G == NGP - 1:
                        for h in range(4):
                            for c in range(8):
                                S.op("pe", lambda e, c=c, h=h: e.matmul(out=bF[:], lhsT=winb[:, c, h * 128:h * 128 + 128], rhs=aT[:, c, :], start=(c == 0), stop=(c == 7)),
                                     reads=[r["winb"], r["aT"]], writes=[r["bF"]])
                            evac(qpre[:, h, 0:3], bF[:, 509:512], [r["bF"]], [r["qpre"]])
                conv(kpre, ckt, kTm, r["kpre"], r["kTm"])

                tokg = G * 512
                S.dma("sp", lambda e, tokg=tokg: e.dma_start(out=KT_d.rearrange("(j two) d t -> (two d) j t", two=2)[:, :, tokg:tokg + 512], in_=skT[:]),
                      reads=[r["skT"]], writes=[r["KTd"]], sem_res=r["skT"])
                if own:
                    S.dma("sp", lambda e, a=row0: e.dma_start(out=QT_d.rearrange("(j two) d t -> (two d) j t", two=2)[:, :, a:a + 512], in_=sqT[:]),
                          reads=[r["sqT"]], writes=[r["QTd"]], sem_res=r["sqT"])
                for ti in range(4):
                    for c in range(8):
                        S.op("pe", lambda e, c=c, ti=ti: e.matmul(out=bF[:], lhsT=aT[:, c, ti * 128:(ti + 1) * 128], rhs=winb[:, c, 3080:3592], start=(c == 0), stop=(c == 7)),
                             reads=[r["winb"], r["aT"]], writes=[r["bF"]])
                    evac(svt[:, :, ti, :], bF[:].rearrange("p (h d) -> p h d", h=8), [r["bF"]], [r["svt"]])
                S.dma("sp", lambda e, G=G: e.dma_start(out=V_d.rearrange("h s j d -> s h j d")[:, :, 4 * G:4 * G + 4, :], in_=svt[:]),
                      reads=[r["svt"]], writes=[r["Vd"]], sem_res=r["svt"])

                for ci in range(8):
                    c0 = ci * 64
                    for c in range(8):
                        S.op("pe", lambda e, c=c, c0=c0: e.matmul(out=bV[0:64, :], lhsT=aT[:, c, c0:c0 + 64], rhs=winb[:, c, 1024:1536], start=(c == 0), stop=(c == 7)),
                             reads=[r["winb"], r["aT"]], writes=[r["bV"]])
                    for c in range(8):
                        S.op("pe", lambda e, c=c, c0=c0: e.matmul(out=bSm[0:64, 0:8], lhsT=aT[:, c, c0:c0 + 64], rhs=winb[:, c, 2048:2056], start=(c == 0), stop=(c == 7)),
                             reads=[r["winb"], r["aT"]], writes=[r["bG"]])
                    if own:
                        for c in range(8):
                            S.op("pe", lambda e, c=c, c0=c0: e.matmul(out=bO[0:64, :], lhsT=aT[:, c, c0:c0 + 64], rhs=winb[:, c, 1536:2048], start=(c == 0), stop=(c == 7)),
                                 reads=[r["winb"], r["aT"]], writes=[r["bO"]])
                    for h in range(4):
                        S.op("pe", lambda e, h=h, c0=c0: e.transpose(out=bT[0:64, h * 128:(h + 1) * 128], in_=kTm[:, h, c0:c0 + 64], identity=identb[:]),
                             reads=[r["kTm"]], writes=[r["bT"]])
                    evac(ktok[:], bT[0:64, 0:512].rearrange("p (h d) -> p h d", h=4), [r["bT"]], [r["ktok"]])
                    S.op("dve", lambda e: e.tensor_tensor(out=gsb[:], in0=bSm[0:64, 0:8], in1=bgt[:], op=ALU.add), reads=[r["bG"], r["small"]], writes=[r["gsb"]])
                    S.op("act", lambda e: e.activation(out=lfs[:], in_=gsb[:, 4:8], func=AF.Exp, scale=-1.0), reads=[r["gsb"]], writes=[r["lfs"]])
                    S.op("act", lambda e: e.activation(out=lfs[:], in_=lfs[:], func=AF.Ln, bias=1.0), reads=[r["lfs"]], writes=[r["lfs"]])
                    S.op("dve", lambda e: e.tensor_copy(out=lf2[:, 0:4], in_=lfs[:]), reads=[r["lfs"]], writes=[r["lfb"]])
                    S.op("dve", lambda e: e.tensor_tensor(out=lft[:], in0=lfs[:], in1=lf2[:, 0:4], op=ALU.subtract), reads=[r["lfs"], r["lfb"]], writes=[r["lfb"]])
                    S.op("dve", lambda e: e.tensor_copy(out=lf2[:, 4:8], in_=lft[:]), reads=[r["lfb"]], writes=[r["lfb"]])
                    for q_ in range(2):
                        S.op("pe", lambda e, q_=q_: e.matmul(out=bSm[0:64, 8:12], lhsT=triu64b[:], rhs=lf2[:, q_ * 4:q_ * 4 + 4], start=(q_ == 0), stop=(q_ == 1)), reads=[r["lfb"]], writes=[r["bS"]])
                    for q_ in range(2):
                        S.op("pe", lambda e, q_=q_: e.matmul(out=bSm[0:64, 12:16], lhsT=ones64b[:, 0:64], rhs=lf2[:, q_ * 4:q_ * 4 + 4], start=(q_ == 0), stop=(q_ == 1)), reads=[r["lfb"]], writes=[r["bS"]])
                    for q_ in range(2):
                        S.op("pe", lambda e, q_=q_: e.matmul(out=bSm[:, 16:20], lhsT=ones64b[:], rhs=lf2[:, q_ * 4:q_ * 4 + 4], start=(q_ == 0), stop=(q_ == 1)), reads=[r["lfb"]], writes=[r["bE"]])
                    S.op("act", lambda e: e.copy(out=bs[:], in_=bSm[0:64, 8:16]), reads=[r["bS"]], writes=[r["bs"]])
                    S.op("act", lambda e: e.activation(out=eB[:], in_=bSm[:, 16:20], func=AF.Exp, scale=-1.0), reads=[r["bE"]], writes=[r["eB"]])
                    S.op("dve", lambda e: e.tensor_tensor(out=garg[:], in0=bs[:, 0:4], in1=bs[:, 4:8], op=ALU.subtract), reads=[r["bs"]], writes=[r["garg"]])
                    S.op("dve", lambda e: e.tensor_tensor(out=garg[:], in0=garg[:], in1=gsb[:, 0:4], op=ALU.add), reads=[r["garg"], r["gsb"]], writes=[r["garg"]])
                    S.op("act", lambda e: e.activation(out=gamma[:], in_=garg[:], func=AF.Exp), reads=[r["garg"]], writes=[r["gamma"]])
                    S.op("dve", lambda e: e.tensor_tensor(out=gvext[:].rearrange("p (h d) -> p h d", h=4), in0=bV[0:64, :].rearrange("p (h d) -> p h d", h=4),
                                                          in1=gamma[:].unsqueeze(2).to_broadcast([64, 4, 128]), op=ALU.mult),
                         reads=[r["bV"], r["gamma"]], writes=[r["gvext"]])
                    S.op("dve", lambda e: e.tensor_copy(out=gamb[:], in_=gamma[:]), reads=[r["gamma"]], writes=[r["gvext"]])
                    if own:
                        S.op("dve", lambda e: e.tensor_copy(out=vext[:], in_=bV[0:64, :]), reads=[r["bV"]], writes=[r["vext"]])
                        S.op("dve", lambda e: e.tensor_scalar(out=alpha[:], in0=bs[:, 0:4], scalar1=-1.0, scalar2=LNC, op0=ALU.mult, op1=ALU.add), reads=[r["bs"]], writes=[r["alpha"]])
                        S.op("act", lambda e: e.activation(out=alpha[:], in_=alpha[:], func=AF.Exp), reads=[r["alpha"]], writes=[r["alpha"]])
                        S.op("dve", lambda e: e.scalar_tensor_tensor(out=dcoef[:], in0=bs[:, 0:4], scalar=LNC, in1=gsb[:, 0:4], op0=ALU.add, op1=ALU.add),
                             reads=[r["bs"], r["gsb"]], writes=[r["dcoef"]])
                        S.op("dve", lambda e: e.tensor_copy(out=lfb[:], in_=lf2[:].unsqueeze(2).to_broadcast([64, 8, 64])), reads=[r["lfb"]], writes=[r["lfb"]])
                        for h in range(4):
                            S.op("pe", lambda e, h=h, c0=c0: e.matmul(out=bAB[0:64, h * 64:(h + 1) * 64], lhsT=kTm[:, h, c0:c0 + 64], rhs=qTm[:, h, c0:c0 + 64], start=True, stop=True),
                                 reads=[r["kTm"], r["qTm"]], writes=[r["bA"]])
                            S.op("pe", lambda e, h=h: e.matmul(out=bAB[0:64, 256 + h * 64:256 + (h + 1) * 64], lhsT=lfb[:, h, :], rhs=triu64b[:], start=True, stop=False),
                                 reads=[r["lfb"]], writes=[r["bB"]])
                            S.op("pe", lambda e, h=h: e.matmul(out=bAB[0:64, 256 + h * 64:256 + (h + 1) * 64], lhsT=lfb[:, 4 + h, :], rhs=triu64b[:], start=False, stop=False),
                                 reads=[r["lfb"]], writes=[r["bB"]])
                            S.op("pe", lambda e, h=h: e.matmul(out=bAB[0:64, 256 + h * 64:256 + (h + 1) * 64], lhsT=identb[0:64, 0:64], rhs=negmb[:], start=False, stop=True),
                                 reads=[r["lfb"]], writes=[r["bB"]])
                        for h in range(4):
                            S.op("act", lambda e, h=h: e.activation(out=DTs[:, h, :], in_=bAB[0:64, 256 + h * 64:256 + (h + 1) * 64], func=AF.Exp, scale=-1.0, bias=dcoef[:, h:h + 1]),
                                 reads=[r["bB"], r["dcoef"]], writes=[r["DTs"]])
                        S.op("dve", lambda e: e.tensor_tensor(out=WT[:].rearrange("p h t -> p (h t)"), in0=bAB[0:64, 0:256], in1=DTs[:].rearrange("p h t -> p (h t)"), op=ALU.mult),
                             reads=[r["bA"], r["DTs"]], writes=[r["WT"]])
                        for h in range(4):
                            S.op("pe", lambda e, h=h: e.matmul(out=bN[0:64, h * 128:(h + 1) * 128], lhsT=WT[:, h, :], rhs=vext[:, h * 128:(h + 1) * 128], start=True, stop=True),
                                 reads=[r["WT"], r["vext"]], writes=[r["bN"]])
                            S.op("pe", lambda e, h=h: e.matmul(out=bSm[0:64, 20 + h:21 + h], lhsT=WT[:, h, :], rhs=onecol[:, 0:1], start=True, stop=True),
                                 reads=[r["WT"], r["vext"]], writes=[r["bD"]])
                            S.op("pe", lambda e, h=h, c0=c0: e.matmul(out=bN2[0:64, h * 128:(h + 1) * 128], lhsT=qTm[:, h, c0:c0 + 64], rhs=Cbf[:, h * 128:(h + 1) * 128], start=True, stop=True),
                                 reads=[r["qTm"], r["Cbf"]], writes=[r["bN2"]])
                            S.op("pe", lambda e, h=h, c0=c0: e.matmul(out=bSm[0:64, 24 + h:25 + h], lhsT=qTm[:, h, c0:c0 + 64], rhs=nbf[:, h:h + 1], start=True, stop=True),
                                 reads=[r["qTm"], r["Cbf"]], writes=[r["bD"]])
                        S.op("dve", lambda e: e.tensor_tensor(out=n2s[:], in0=bN2[0:64, :].rearrange("p (h d) -> p h d", h=4), in1=alpha[:].unsqueeze(2).to_broadcast([64, 4, 128]), op=ALU.mult),
                             reads=[r["bN2"], r["alpha"]], writes=[r["n2s"]])
                        S.op("dve", lambda e: e.tensor_tensor(out=numsb[:], in0=bN[0:64, :].rearrange("p (h d) -> p h d", h=4), in1=n2s[:], op=ALU.add),
                             reads=[r["bN"], r["n2s"]], writes=[r["numsb"]])
                        S.op("act", lambda e: e.copy(out=dsb[:], in_=bSm[0:64, 20:28]), reads=[r["bD"]], writes=[r["dsb"]])
                        S.op("dve", lambda e: e.tensor_tensor(out=den[:], in0=dsb[:, 4:8], in1=alpha[:], op=ALU.mult), reads=[r["dsb"], r["alpha"]], writes=[r["den"]])
                        S.op("dve", lambda e: e.tensor_tensor(out=den[:], in0=den[:], in1=dsb[:, 0:4], op=ALU.add), reads=[r["dsb"], r["den"]], writes=[r["den"]])
                        S.op("dve", lambda e: e.tensor_scalar(out=ssh[:], in0=den[:], scalar1=-1.0, scalar2=None, op0=ALU.mult), reads=[r["den"]], writes=[r["ssh"]])
                        S.op("dve", lambda e: e.tensor_tensor(out=den[:], in0=den[:], in1=ssh[:], op=ALU.max), reads=[r["den"], r["ssh"]], writes=[r["den"]])
                        S.op("dve", lambda e: e.tensor_scalar_max(out=den[:], in0=den[:], scalar1=1.0), reads=[r["den"]], writes=[r["den"]])
                        S.op("dve", lambda e: e.reciprocal(out=den[:], in_=den[:]), reads=[r["den"]], writes=[r["den"]])
                        S.op("dve", lambda e: e.tensor_tensor(out=numsb[:], in0=numsb[:], in1=den[:].unsqueeze(2).to_broadcast([64, 4, 128]), op=ALU.mult),
                             reads=[r["numsb"], r["den"]], writes=[r["numsb"]])
                        S.op("dve", lambda e: e.tensor_tensor(out=sqh[:], in0=numsb[:], in1=numsb[:], op=ALU.mult), reads=[r["numsb"]], writes=[r["sqh"]])
                        S.op("dve", lambda e: e.tensor_reduce(out=ssh[:], in_=sqh[:], axis=AX.X, op=ALU.add), reads=[r["sqh"]], writes=[r["ssh"]])
                        S.op("dve", lambda e: e.tensor_scalar(out=ssh[:], in0=ssh[:], scalar1=1.0 / 128, scalar2=EPS, op0=ALU.mult, op1=ALU.add), reads=[r["ssh"]], writes=[r["ssh"]])
                        S.op("act", lambda e: e.sqrt(out=ssh[:], in_=ssh[:]), reads=[r["ssh"]], writes=[r["ssh"]])
                        S.op("dve", lambda e: e.reciprocal(out=ssh[:], in_=ssh[:]), reads=[r["ssh"]], writes=[r["ssh"]])
                        S.op("dve", lambda e: e.tensor_tensor(out=numsb[:], in0=numsb[:], in1=ssh[:].unsqueeze(2).to_broadcast([64, 4, 128]), op=ALU.mult),
                             reads=[r["numsb"], r["ssh"]], writes=[r["numsb"]])
                        S.op("dve", lambda e: e.tensor_tensor(out=numsb[:], in0=numsb[:], in1=gmht[:].rearrange("p (h d) -> p h d", h=4), op=ALU.mult),
                             reads=[r["numsb"], r["small"]], writes=[r["numsb"]])
                        S.op("act", lambda e: e.activation(out=sig[:], in_=bO[0:64, :], func=AF.Sigmoid), reads=[r["bO"]], writes=[r["sig"]])
                        S.op("dve", lambda e: e.tensor_tensor(out=mixtok[:], in0=numsb[:], in1=sig[:].rearrange("p (h d) -> p h d", h=4), op=ALU.mult),
                             reads=[r["numsb"], r["sig"]], writes=[r["mixtok"]])
                        if dbg and G == NGP and ci == 1:
                            rdb = R("dbg")
                            dl = [(gsb, 0, 8), (lfs, 8, 4), (bs, 12, 8), (alpha, 20, 4), (gamma, 24, 4), (dcoef, 28, 4), (den, 32, 4), (dsb, 36, 8), (ssh, 44, 4)]
                            for (tt_, o_, n_) in dl:
                                S.dma("sp", lambda e, tt_=tt_, o_=o_, n_=n_: e.dma_start(out=dbgA[:, o_:o_ + n_], in_=tt_[:]), reads=[r["numsb"], r["ssh"], r["den"]], writes=[rdb])
                            S.dma("sp", lambda e: e.dma_start(out=dbgA[:, 64:320], in_=DTs[:].rearrange("p h t -> p (h t)")), reads=[r["DTs"]], writes=[rdb])
                            S.dma("sp", lambda e: e.dma_start(out=dbgA[:, 512:1024], in_=numsb[:].rearrange("p h t -> p (h t)")), reads=[r["numsb"]], writes=[rdb])
                            S.dma("sp", lambda e: e.dma_start(out=dbgA[:, 1024:1536], in_=n2s[:].rearrange("p h t -> p (h t)")), reads=[r["n2s"]], writes=[rdb])
                            S.dma("sp", lambda e: e.dma_start(out=dbgA[:, 1536:2048], in_=sig[:]), reads=[r["sig"]], writes=[rdb])
                        for h in range(4):
                            S.op("pe", lambda e, h=h: e.transpose(out=bT[:, 512 + h * 64:512 + (h + 1) * 64], in_=mixtok[:, h, :], identity=identb[0:64, 0:64]),
                                 reads=[r["mixtok"]], writes=[r["bT"]])
                        evac(mixTg[:, :, c0:c0 + 64], bT[:, 512:768].rearrange("p (h t) -> p h t", h=4), [r["bT"]], [r["mixTg"]])
                    for h in range(4):
                        S.op("pe", lambda e, h=h: e.matmul(out=bN[:, h * 128:(h + 1) * 128], lhsT=ktok[:, h, :], rhs=gvext[:, h * 128:(h + 1) * 128], start=True, stop=True),
                             reads=[r["ktok"], r["gvext"]], writes=[r["bN"]])
                        S.op("pe", lambda e, h=h: e.matmul(out=bSm[:, 28 + h:29 + h], lhsT=ktok[:, h, :], rhs=gamb[:, h:h + 1], start=True, stop=True),
                             reads=[r["ktok"], r["gvext"]], writes=[r["bDn"]])
                    for h in range(4):
                        S.op("dve", lambda e, h=h: e.scalar_tensor_tensor(out=Cst[:, h, :], in0=Cst[:, h, :], scalar=eB[:, h:h + 1], in1=bN[:, h * 128:(h + 1) * 128], op0=ALU.mult, op1=ALU.add),
                             reads=[r["Cst"], r["eB"], r["bN"]], writes=[r["Cst"]])
                    S.op("dve", lambda e: e.tensor_tensor(out=nst[:], in0=nst[:], in1=eB[:], op=ALU.mult), reads=[r["nst"], r["eB"]], writes=[r["nst"]])
                    S.op("dve", lambda e: e.tensor_tensor(out=nst[:], in0=nst[:], in1=bSm[:, 28:32], op=ALU.add), reads=[r["nst"], r["bDn"]], writes=[r["nst"]])
                    S.op("act", lambda e: e.copy(out=Cbf[:], in_=Cst[:].rearrange("p h d -> p (h d)")), reads=[r["Cst"]], writes=[r["Cbf"]])
                    S.op("act", lambda e: e.copy(out=nbf[:], in_=nst[:]), reads=[r["nst"]], writes=[r["Cbf"]])
                if own:
                    go = G - NGP
                    for j in range(4):
                        S.dma("sp", lambda e, go=go, j=j: e.dma_start(out=mixTm_d[4 * go + j], in_=mixTg[:, :, j * 128:(j + 1) * 128]),
                              reads=[r["mixTg"]], writes=[r["mixTmd"]], sem_res=r["mixTg"])
            S.barrier()
            S.release(list(r.values()))

        def phase_B():
          with ExitStack() as eb:
            KTh = [sbt(eb, f"KTh{i}", [64, ST], BF16) for i in range(2)]
            Vh = [sbt(eb, f"Vh{i}", [128, NT, 64], BF16) for i in range(2)]
            QTh = [sbt(eb, f"QTh{i}", [64, SO], BF16) for i in range(2)]
            maskf = sbt(eb, "maskf", [128, 4, 512], F32)
            Es = [sbt(eb, f"Es{i}", [128, 512], F32) for i in range(2)]
            Ls = [sbt(eb, f"Ls{i}", [128, 512], BF16) for i in range(2)]
            Aes = [sbt(eb, f"Aes{i}", [128, 512], F32) for i in range(2)]
            As = [sbt(eb, f"As{i}", [128, 512], BF16) for i in range(2)]
            CSs = [sbt(eb, f"CSs{i}", [128, 512], BF16) for i in range(2)]
            ost = [sbt(eb, f"ost{i}", [64, 512], BF16) for i in range(2)]
            bZ = [pst(eb, f"bZ{i}", [128, 512], F32) for i in range(2)]
            bRC = [pst(eb, f"bRC{i}", [128, 512], F32) for i in range(2)]
            bOT = [pst(eb, f"bOT{i}", [128, 512], F32) for i in range(2)]
            rn = ["mask", "mixTsd"] + [f"{n}{i}" for n in ["KTh", "Vh", "QTh", "Es", "Ls", "Aes", "As", "CSs", "ost", "bZ", "bRC", "bOT"] for i in range(2)]
            r = {k: R(k) for k in rn}
            for m in range(4):
                S.op("pool", lambda e, m=m: e.memset(maskf[:, m, :], 1.0), writes=[r["mask"]])
                S.op("pool", lambda e, m=m: e.affine_select(out=maskf[:, m, :], in_=maskf[:, m, :], pattern=[[1, 512]], compare_op=ALU.is_ge,
                                                            fill=0.0, base=-(m * 128) - 1, channel_multiplier=-1), reads=[r["mask"]], writes=[r["mask"]])
            blocks = []
            head_start = {}
            for h in range(8):
                head_start[h] = len(blocks)
                for gq in range(NGO):
                    jd0 = (NGP + gq) * 4
                    first = True
                    for j in range(jd0 + 3, -1, -1):
                        blocks.append(dict(h=h, hs=h % 2, gq=gq, j=j, m=j - jd0, first=first, last=(j == 0), oi=(h * NGO + gq) % 2))
                        first = False
            NB = len(blocks)

            def load_head(h):
                hs = h % 2
                S.dma("sp", lambda e: e.dma_start(out=KTh[hs][:], in_=KT_d[h]), writes=[r[f"KTh{hs}"]])
                S.dma("sp", lambda e: e.dma_start(out=Vh[hs][:], in_=V_d[h]), writes=[r[f"Vh{hs}"]])
                S.dma("sp", lambda e: e.dma_start(out=QTh[hs][:], in_=QT_d[h]), writes=[r[f"QTh{hs}"]])

            def stage1(k):
                B_ = blocks[k]
                b2, hs, j, gq, m = k % 2, B_["hs"], B_["j"], B_["gq"], B_["m"]
                S.op("pe", lambda e: e.matmul(out=bZ[b2][:], lhsT=KTh[hs][:, j * 128:(j + 1) * 128], rhs=QTh[hs][:, gq * 512:(gq + 1) * 512], start=True, stop=True),
                     reads=[r[f"KTh{hs}"], r[f"QTh{hs}"]], writes=[r[f"bZ{b2}"]])
                S.op("act", lambda e: e.activation(out=Es[b2][:], in_=bZ[b2][:], func=AF.Exp), reads=[r[f"bZ{b2}"]], writes=[r[f"Es{b2}"]])
                if m >= 0:
                    S.op("dve", lambda e: e.tensor_tensor(out=Es[b2][:], in0=Es[b2][:], in1=maskf[:, m, :], op=ALU.mult),
                         reads=[r[f"Es{b2}"], r["mask"]], writes=[r[f"Es{b2}"]])
                S.op("act", lambda e: e.activation(out=Ls[b2][:], in_=Es[b2][:], func=AF.Ln, bias=1.0), reads=[r[f"Es{b2}"]], writes=[r[f"Ls{b2}"]])

            def stage2(k):
                B_ = blocks[k]
                b2, pc, first, last = k % 2, (k - 1) % 2, B_["first"], B_["last"]
                S.op("pe", lambda e: e.matmul(out=bRC[b2][:], lhsT=trilb[:], rhs=Ls[b2][:], start=True, stop=first),
                     reads=[r[f"Ls{b2}"]], writes=[r[f"bRC{b2}"]])
                if not first:
                    S.op("pe", lambda e: e.matmul(out=bRC[b2][:], lhsT=onesb[:], rhs=CSs[pc][:], start=False, stop=True),
                         reads=[r[f"CSs{pc}"]], writes=[r[f"bRC{b2}"]])
                S.op("act", lambda e: e.activation(out=Aes[b2][:], in_=bRC[b2][:], func=AF.Exp, scale=-1.0), reads=[r[f"bRC{b2}"]], writes=[r[f"Aes{b2}"]])
                S.op("dve", lambda e: e.tensor_tensor(out=As[b2][:], in0=Es[b2][:], in1=Aes[b2][:], op=ALU.mult),
                     reads=[r[f"Es{b2}"], r[f"Aes{b2}"]], writes=[r[f"As{b2}"]])
                if not last:
                    if first:
                        S.op("pool", lambda e: e.tensor_copy(out=CSs[b2][:], in_=Ls[b2][:]), reads=[r[f"Ls{b2}"]], writes=[r[f"CSs{b2}"]])
                    else:
                        S.op("pool", lambda e: e.tensor_tensor(out=CSs[b2][:], in0=CSs[pc][:], in1=Ls[b2][:], op=ALU.add),
                             reads=[r[f"Ls{b2}"], r[f"CSs{pc}"]], writes=[r[f"CSs{b2}"]])

            def stage3(k):
                B_ = blocks[k]
                b2, hs, j, gq, h, oi, first, last = k % 2, B_["hs"], B_["j"], B_["gq"], B_["h"], B_["oi"], B_["first"], B_["last"]
                S.op("pe", lambda e: e.matmul(out=bOT[oi][0:64, :], lhsT=Vh[hs][:, j, :], rhs=As[b2][:], start=first, stop=last),
                     reads=[r[f"Vh{hs}"], r[f"As{b2}"]], writes=[r[f"bOT{oi}"]])
                if last:
                    S.op("dve", lambda e: e.tensor_copy(out=ost[oi][:], in_=bOT[oi][0:64, :]), reads=[r[f"bOT{oi}"]], writes=[r[f"ost{oi}"]])
                    S.dma("sp", lambda e: e.dma_start(out=mixTs_d[4 * gq:4 * gq + 4, :, h, :].rearrange("j d t -> d j t"),
                                                     in_=ost[oi][:].rearrange("d (j t) -> d j t", j=4)),
                          reads=[r[f"ost{oi}"]], writes=[r["mixTsd"]], sem_res=r[f"ost{oi}"])

            load_head(0)
            for step in range(NB + 2):
                for h in range(7):
                    if step == head_start[h] + 3:
                        load_head(h + 1)
                if step < NB:
                    stage1(step)
                if 0 <= step - 1 < NB:
                    stage2(step - 1)
                if 0 <= step - 2 < NB:
                    stage3(step - 2)
            S.barrier()
            S.release(list(r.values()))

        def phase_C():
          NPASS = 4 if NTO >= 8 else 2
          NTH = NTO // NPASS
          TG = min(4, NTH)
          with ExitStack() as ec:
            gv = sbt(ec, "gv", [128, 4, D], F32)
            brt = sbt(ec, "brt", [128, 36], F32)
            wrt = sbt(ec, "wrt", [128, 8, 36], F32)
            acc = sbt(ec, "acc", [128, NTH, D], F32)
            cTb = sbt(ec, "cTb", [128, 8, NTH * 128], BF16)
            wfull = sbt(ec, "wfull", [128, NTH, 32], F32)
            xts = [sbt(ec, f"xc{i}", [128, D], F32) for i in range(2)]
            junk = sbt(ec, "junkc", [128, D], BF16)
            ssq = sbt(ec, "ssqc", [128, 2], F32)
            r0 = {k: R(k) for k in ["gv", "brt", "wrt", "acc", "cTb", "wfull", "xc0", "xc1", "junk", "ssq", "outd"]}
            S.dma("sp", lambda e: e.dma_start(out=gv[:], in_=gvec[1:5, :].partition_broadcast(128)), writes=[r0["gv"]])
            S.dma("sp", lambda e: e.dma_start(out=brt[:], in_=br.partition_broadcast(128)), writes=[r0["brt"]])
            S.dma("sp", lambda e: e.dma_start(out=wrt[:], in_=wr.rearrange("(c p) n -> p c n", p=128)), writes=[r0["wrt"]])

            for ps_i in range(NPASS):
                t0 = ps_i * NTH
                with ExitStack() as e1:
                    woutm = sbt(e1, "woutm", [128, 4, D], BF16)
                    wouts = sbt(e1, "wouts", [64, 8, D], BF16)
                    mTm = [sbt(e1, f"mTm{i}", [128, 4, 128], BF16) for i in range(2)]
                    mTs = [sbt(e1, f"mTs{i}", [64, 8, 128], BF16) for i in range(2)]
                    c32 = sbt(e1, "c32", [128, D], F32)
                    cT32 = sbt(e1, "cT32", [128, 8, 128], F32)
                    lg = sbt(e1, "lg", [128, 36], F32)
                    gmax = sbt(e1, "gmax", [128, 8], F32)
                    ohg = sbt(e1, "ohg", [128, 4], F32)
                    eg = sbt(e1, "eg", [128, 4], F32)
                    esel = sbt(e1, "esel", [128, 4, 8], F32)
                    es8 = sbt(e1, "es8", [128, 8], F32)
                    mk1 = sbt(e1, "mk1", [128, 8], F32)
                    mk2 = sbt(e1, "mk2", [128, 8], F32)
                    e2 = sbt(e1, "e2", [128, 8], F32)
                    wsel = sbt(e1, "wsel", [128, 8], F32)
                    bH = [pst(e1, f"bH{i}", [128, 512], F32) for i in range(2)]
                    bTf = [pst(e1, f"bTf{i}", [128, 512], F32) for i in range(2)]
                    bL = pst(e1, "bL", [128, 512], F32)
                    r = {k: R(k) for k in ["woutm", "wouts", "mTm0", "mTm1", "mTs0", "mTs1", "c32", "cT32", "lg", "rt", "bH0", "bH1", "bTf0", "bTf1", "bL"]}
                    S.dma("pool", lambda e: e.dma_start(out=woutm[:], in_=wout[0:512, :].rearrange("(h p) n -> p h n", p=128)), writes=[r["woutm"]])
                    S.dma("pool", lambda e: e.dma_start(out=wouts[:], in_=wout[512:1024, :].rearrange("(h p) n -> p h n", p=64)), writes=[r["wouts"]])
                    for tl in range(NTH):
                        t = t0 + tl
                        xi = tl % 2
                        xt = xts[xi]
                        rx = r0[f"xc{xi}"]
                        S.dma("sp", lambda e, xt=xt, t=t: e.dma_start(out=xt[:], in_=xo[t * 128:(t + 1) * 128, :]), writes=[rx])
                        S.dma("sp", lambda e, xi=xi, t=t: e.dma_start(out=mTm[xi][:], in_=mixTm_d[t]), writes=[r[f"mTm{xi}"]])
                        S.dma("sp", lambda e, xi=xi, t=t: e.dma_start(out=mTs[xi][:], in_=mixTs_d[t]), writes=[r[f"mTs{xi}"]])
                        for dh in range(2):
                            for h in range(4):
                                S.op("pe", lambda e, h=h, dh=dh, xi=xi: e.matmul(out=bH[dh][:], lhsT=mTm[xi][:, h, :], rhs=woutm[:, h, dh * 512:(dh + 1) * 512], start=(h == 0), stop=False),
                                     reads=[r[f"mTm{xi}"], r["woutm"]], writes=[r[f"bH{dh}"]])
                            for h in range(8):
                                S.op("pe", lambda e, h=h, dh=dh, xi=xi: e.matmul(out=bH[dh][:], lhsT=mTs[xi][:, h, :], rhs=wouts[:, h, dh * 512:(dh + 1) * 512], start=False, stop=(h == 7)),
                                     reads=[r[f"mTs{xi}"], r["wouts"]], writes=[r[f"bH{dh}"]])
                            S.op("dve", lambda e, dh=dh, tl=tl, xt=xt: e.tensor_tensor(out=acc[:, tl, dh * 512:(dh + 1) * 512], in0=bH[dh][:], in1=xt[:, dh * 512:(dh + 1) * 512], op=ALU.add),
                                 reads=[r[f"bH{dh}"], rx], writes=[r0["acc"]])
                        rmsnorm_stats(acc[:, tl, :], junk[:], ssq[:, 0:1], ssq[:, 1:2], r0["acc"], r0["junk"], r0["ssq"], D)
                        S.op("dve", lambda e, tl=tl: e.scalar_tensor_tensor(out=c32[:], in0=acc[:, tl, :], scalar=ssq[:, 1:2], in1=gv[:, 0, :], op0=ALU.mult, op1=ALU.mult),
                             reads=[r0["acc"], r0["ssq"], r0["gv"]], writes=[r["c32"]])
                        for c in range(8):
                            S.op("pe", lambda e, c=c: e.transpose(out=bTf[c // 4][:, (c % 4) * 128:(c % 4 + 1) * 128], in_=c32[:, c * 128:(c + 1) * 128], identity=identf[:]),
                                 reads=[r["c32"]], writes=[r[f"bTf{c // 4}"]])
                        for hh in range(2):
                            S.op("act", lambda e, hh=hh: e.copy(out=cT32[:, hh * 4:(hh + 1) * 4, :], in_=bTf[hh][:].rearrange("p (c t) -> p c t", c=4)),
                                 reads=[r[f"bTf{hh}"]], writes=[r["cT32"]])
                            S.op("dve", lambda e, hh=hh, tl=tl: e.tensor_copy(out=cTb[:, hh * 4:(hh + 1) * 4, tl * 128:(tl + 1) * 128], in_=cT32[:, hh * 4:(hh + 1) * 4, :]),
                                 reads=[r["cT32"]], writes=[r0["cTb"]])
                        for c in range(8):
                            S.op("pe", lambda e, c=c: e.matmul(out=bL[:, 0:36], lhsT=cT32[:, c, :], rhs=wrt[:, c, :], start=(c == 0), stop=(c == 7)),
                                 reads=[r["cT32"], r0["wrt"]], writes=[r["bL"]])
                        rt = r["rt"]
                        S.op("dve", lambda e: e.tensor_tensor(out=lg[:], in0=bL[:, 0:36], in1=brt[:], op=ALU.add), reads=[r["bL"], r0["brt"]], writes=[rt])
                        S.op("dve", lambda e: e.tensor_reduce(out=gmax[:, 0:1], in_=lg[:, 0:4], axis=AX.X, op=ALU.max), reads=[rt], writes=[rt])
                        S.op("dve", lambda e: e.tensor_scalar(out=ohg[:], in0=lg[:, 0:4], scalar1=gmax[:, 0:1], scalar2=None, op0=ALU.is_equal), reads=[rt], writes=[rt])
                        S.op("dve", lambda e: e.tensor_scalar(out=eg[:], in0=lg[:, 0:4], scalar1=gmax[:, 0:1], scalar2=None, op0=ALU.subtract), reads=[rt], writes=[rt])
                        S.op("pool", lambda e: e.memset(gmax[:, 1:2], 0.0), reads=[rt], writes=[rt])
                        S.op("act", lambda e: e.activation(out=eg[:], in_=eg[:], func=AF.Exp, accum_out=gmax[:, 1:2]), reads=[rt], writes=[rt])
                        S.op("dve", lambda e: e.reciprocal(out=gmax[:, 2:3], in_=gmax[:, 1:2]), reads=[rt], writes=[rt])
                        S.op("dve", lambda e: e.tensor_tensor(out=esel[:], in0=lg[:, 4:36].rearrange("p (g j) -> p g j", g=4), in1=ohg[:].unsqueeze(2).to_broadcast([128, 4, 8]), op=ALU.mult),
                             reads=[rt], writes=[rt])
                        S.op("dve", lambda e: e.tensor_reduce(out=es8[:], in_=esel[:].rearrange("p g j -> p j g"), axis=AX.X, op=ALU.add), reads=[rt], writes=[rt])
                        S.op("dve", lambda e: e.tensor_reduce(out=gmax[:, 3:4], in_=es8[:], axis=AX.X, op=ALU.max), reads=[rt], writes=[rt])
                        S.op("dve", lambda e: e.tensor_scalar(out=mk1[:], in0=es8[:], scalar1=gmax[:, 3:4], scalar2=None, op0=ALU.is_equal), reads=[rt], writes=[rt])
                        S.op("dve", lambda e: e.scalar_tensor_tensor(out=e2[:], in0=mk1[:], scalar=-1e30, in1=es8[:], op0=ALU.mult, op1=ALU.add), reads=[rt], writes=[rt])
                        S.op("dve", lambda e: e.tensor_reduce(out=gmax[:, 4:5], in_=e2[:], axis=AX.X, op=ALU.max), reads=[rt], writes=[rt])
                        S.op("dve", lambda e: e.tensor_scalar(out=mk2[:], in0=e2[:], scalar1=gmax[:, 4:5], scalar2=None, op0=ALU.is_equal), reads=[rt], writes=[rt])
                        S.op("dve", lambda e: e.tensor_tensor(out=gmax[:, 5:6], in0=gmax[:, 4:5], in1=gmax[:, 3:4], op=ALU.subtract), reads=[rt], writes=[rt])
                        S.op("act", lambda e: e.activation(out=gmax[:, 5:6], in_=gmax[:, 5:6], func=AF.Exp), reads=[rt], writes=[rt])
                        S.op("dve", lambda e: e.tensor_scalar(out=gmax[:, 6:7], in0=gmax[:, 5:6], scalar1=1.0, scalar2=None, op0=ALU.add), reads=[rt], writes=[rt])
                        S.op("dve", lambda e: e.reciprocal(out=gmax[:, 6:7], in_=gmax[:, 6:7]), reads=[rt], writes=[rt])
                        S.op("dve", lambda e: e.tensor_tensor(out=gmax[:, 6:7], in0=gmax[:, 6:7], in1=gmax[:, 2:3], op=ALU.mult), reads=[rt], writes=[rt])
                        S.op("dve", lambda e: e.tensor_tensor(out=gmax[:, 7:8], in0=gmax[:, 6:7], in1=gmax[:, 5:6], op=ALU.mult), reads=[rt], writes=[rt])
                        S.op("dve", lambda e: e.tensor_scalar(out=wsel[:], in0=mk1[:], scalar1=gmax[:, 6:7], scalar2=None, op0=ALU.mult), reads=[rt], writes=[rt])
                        S.op("dve", lambda e: e.scalar_tensor_tensor(out=wsel[:], in0=mk2[:], scalar=gmax[:, 7:8], in1=wsel[:], op0=ALU.mult, op1=ALU.add), reads=[rt], writes=[rt])
                        for g in range(4):
                            S.op("dve", lambda e, g=g, tl=tl: e.tensor_scalar(out=wfull[:, tl, g * 8:(g + 1) * 8], in0=wsel[:], scalar1=ohg[:, g:g + 1], scalar2=None, op0=ALU.mult),
                                 reads=[rt], writes=[r0["wfull"]])
                    S.barrier()
                    S.release(list(r.values()))

                with ExitStack() as e2s:
                    wgt = [sbt(e2s, f"wgt{i}", [128, 8, 512], BF16) for i in range(2)]
                    wut = [sbt(e2s, f"wut{i}", [128, 8, 512], BF16) for i in range(2)]
                    wdt = [sbt(e2s, f"wdt{i}", [128, 4, D], BF16) for i in range(2)]
                    sgs = [sbt(e2s, f"sgs{i}", [128, TG * 128], F32) for i in range(2)]
                    hid = [sbt(e2s, f"hid{i}", [128, 4, TG * 128], BF16) for i in range(2)]
                    bGt = [pst(e2s, f"bGt{i}", [128, 512], F32) for i in range(2)]
                    bUt = [pst(e2s, f"bUt{i}", [128, 512], F32) for i in range(2)]
                    bY = [pst(e2s, f"bY{i}", [128, 512], F32) for i in range(2)]
                    r = {k: R(k) for k in ["wgt0", "wgt1", "wut0", "wut1", "wdt0", "wdt1", "sgs0", "sgs1", "hid0", "hid1", "bGt0", "bGt1", "bUt0", "bUt1", "bY0", "bY1"]}
                    NW = TG * 128
                    cnt = 0
                    ycnt = 0

                    def load_exp(ex):
                        s_ = ex % 2
                        S.dma("pool", lambda e: e.dma_start(out=wgt[s_][:], in_=wg[ex].rearrange("(c p) n -> p c n", p=128)), writes=[r[f"wgt{s_}"]])
                        S.dma("pool", lambda e: e.dma_start(out=wut[s_][:], in_=wu[ex].rearrange("(c p) n -> p c n", p=128)), writes=[r[f"wut{s_}"]])
                        S.dma("pool", lambda e: e.dma_start(out=wdt[s_][:], in_=wd[ex].rearrange("(c p) n -> p c n", p=128)), writes=[r[f"wdt{s_}"]])

                    load_exp(0)
                    for ex in range(NEXP):
                        s_ = ex % 2
                        if ex + 1 < NEXP:
                            load_exp(ex + 1)
                        for tg in range(NTH // TG):
                            hb = (ex * (NTH // TG) + tg) % 2
                            for fc in range(4):
                                b2 = cnt % 2
                                cnt += 1
                                for c in range(8):
                                    S.op("pe", lambda e, c=c, fc=fc, b2=b2, s_=s_, tg=tg: e.matmul(out=bGt[b2][:, 0:NW], lhsT=wgt[s_][:, c, fc * 128:(fc + 1) * 128],
                                                                                                  rhs=cTb[:, c, tg * NW:(tg + 1) * NW], start=(c == 0), stop=(c == 7)),
                                         reads=[r[f"wgt{s_}"], r0["cTb"]], writes=[r[f"bGt{b2}"]])
                                for c in range(8):
                                    S.op("pe", lambda e, c=c, fc=fc, b2=b2, s_=s_, tg=tg: e.matmul(out=bUt[b2][:, 0:NW], lhsT=wut[s_][:, c, fc * 128:(fc + 1) * 128],
                                                                                                  rhs=cTb[:, c, tg * NW:(tg + 1) * NW], start=(c == 0), stop=(c == 7)),
                                         reads=[r[f"wut{s_}"], r0["cTb"]], writes=[r[f"bUt{b2}"]])
                                S.op("act", lambda e, b2=b2: e.activation(out=sgs[b2][:], in_=bGt[b2][:, 0:NW], func=AF.Silu), reads=[r[f"bGt{b2}"]], writes=[r[f"sgs{b2}"]])
                                S.op("dve", lambda e, b2=b2, hb=hb, fc=fc: e.tensor_tensor(out=hid[hb][:, fc, :], in0=sgs[b2][:], in1=bUt[b2][:, 0:NW], op=ALU.mult),
                                     reads=[r[f"sgs{b2}"], r[f"bUt{b2}"]], writes=[r[f"hid{hb}"]])
                            for tt in range(TG):
                                tl = tg * TG + tt
                                for dh in range(2):
                                    yb = ycnt % 2
                                    ycnt += 1
                                    for fc in range(4):
                                        S.op("pe", lambda e, fc=fc, hb=hb, tt=tt, dh=dh, yb=yb, s_=s_: e.matmul(out=bY[yb][:], lhsT=hid[hb][:, fc, tt * 128:(tt + 1) * 128],
                                                                                                               rhs=wdt[s_][:, fc, dh * 512:(dh + 1) * 512], start=(fc == 0), stop=(fc == 3)),
                                             reads=[r[f"hid{hb}"], r[f"wdt{s_}"]], writes=[r[f"bY{yb}"]])
                                    S.op("dve", lambda e, yb=yb, tl=tl, dh=dh, ex=ex: e.scalar_tensor_tensor(out=acc[:, tl, dh * 512:(dh + 1) * 512], in0=bY[yb][:], scalar=wfull[:, tl, ex:ex + 1],
                                                                                                           in1=acc[:, tl, dh * 512:(dh + 1) * 512], op0=ALU.mult, op1=ALU.add),
                                         reads=[r[f"bY{yb}"], r0["wfull"], r0["acc"]], writes=[r0["acc"]])
                    S.barrier()
                    S.release(list(r.values()))

                with ExitStack() as e3:
                    wpgt = sbt(e3, "wpgt", [128, 8, D], BF16)
                    wppt = sbt(e3, "wppt", [128, 2, D], BF16)
                    n_bf = sbt(e3, "n_bf", [128, D], BF16)
                    nT = sbt(e3, "nT", [128, 8, 128], BF16)
                    gate = sbt(e3, "gate", [128, D], F32)
                    pts = [sbt(e3, f"pt{i}", [128, 256], F32) for i in range(2)]
                    p_bf = sbt(e3, "p_bf", [128, 256], BF16)
                    pT = sbt(e3, "pT", [128, 2, 128], BF16)
                    ple = sbt(e3, "ple", [128, D], F32)
                    ss2 = sbt(e3, "ss2", [128, 4], F32)
                    h3 = sbt(e3, "h3", [128, D], F32)
                    ots = [sbt(e3, f"ot{i}", [128, D], F32) for i in range(2)]
                    bT2 = pst(e3, "bT2", [128, 1024], BF16)
                    bGa = [pst(e3, f"bGa{i}", [128, 512], F32) for i in range(2)]
                    bP = [pst(e3, f"bP{i}", [128, 512], F32) for i in range(2)]
                    r = {k: R(k) for k in ["wpgt", "wppt", "n_bf", "nT", "gate", "pt0", "pt1", "p_bf", "pT", "ple", "ss2", "h3", "ot0", "ot1", "bT2", "bGa0", "bGa1", "bP0", "bP1", "junk2"]}
                    S.dma("pool", lambda e: e.dma_start(out=wpgt[:], in_=wpg.rearrange("(c p) n -> p c n", p=128)), writes=[r["wpgt"]])
                    S.dma("pool", lambda e: e.dma_start(out=wppt[:], in_=wpp.rearrange("(c p) n -> p c n", p=128)), writes=[r["wppt"]])
                    for tl in range(NTH):
                        t = t0 + tl
                        pi = tl % 2
                        S.dma("sp", lambda e, pi=pi, t=t: e.dma_start(out=pts[pi][:], in_=po[t * 128:(t + 1) * 128, :]), writes=[r[f"pt{pi}"]])
                        rmsnorm_stats(acc[:, tl, :], junk[:], ssq[:, 0:1], ssq[:, 1:2], r0["acc"], r0["junk"], r0["ssq"], D)
                        S.op("dve", lambda e, tl=tl: e.scalar_tensor_tensor(out=n_bf[:], in0=acc[:, tl, :], scalar=ssq[:, 1:2], in1=gv[:, 1, :], op0=ALU.mult, op1=ALU.mult),
                             reads=[r0["acc"], r0["ssq"], r0["gv"]], writes=[r["n_bf"]])
                        for c in range(8):
                            S.op("pe", lambda e, c=c: e.transpose(out=bT2[:, c * 128:(c + 1) * 128], in_=n_bf[:, c * 128:(c + 1) * 128], identity=identb[:]),
                                 reads=[r["n_bf"]], writes=[r["bT2"]])
                        S.op("act", lambda e: e.copy(out=nT[:], in_=bT2[:].rearrange("p (c t) -> p c t", c=8)), reads=[r["bT2"]], writes=[r["nT"]])
                        for dh in range(2):
                            for c in range(8):
                                S.op("pe", lambda e, c=c, dh=dh: e.matmul(out=bGa[dh][:], lhsT=nT[:, c, :], rhs=wpgt[:, c, dh * 512:(dh + 1) * 512], start=(c == 0), stop=(c == 7)),
                                     reads=[r["nT"], r["wpgt"]], writes=[r[f"bGa{dh}"]])
                            S.op("act", lambda e, dh=dh: e.activation(out=gate[:, dh * 512:(dh + 1) * 512], in_=bGa[dh][:], func=AF.Sigmoid), reads=[r[f"bGa{dh}"]], writes=[r["gate"]])
                        S.op("dve", lambda e, pi=pi: e.tensor_copy(out=p_bf[:], in_=pts[pi][:]), reads=[r[f"pt{pi}"]], writes=[r["p_bf"]])
                        for c in range(2):
                            S.op("pe", lambda e, c=c: e.transpose(out=bT2[:, c * 128:(c + 1) * 128], in_=p_bf[:, c * 128:(c + 1) * 128], identity=identb[:]),
                                 reads=[r["p_bf"]], writes=[r["bT2"]])
                        S.op("act", lambda e: e.copy(out=pT[:], in_=bT2[:, 0:256].rearrange("p (c t) -> p c t", c=2)), reads=[r["bT2"]], writes=[r["pT"]])
                        for dh in range(2):
                            for c in range(2):
                                S.op("pe", lambda e, c=c, dh=dh: e.matmul(out=bP[dh][:], lhsT=pT[:, c, :], rhs=wppt[:, c, dh * 512:(dh + 1) * 512], start=(c == 0), stop=(c == 1)),
                                     reads=[r["pT"], r["wppt"]], writes=[r[f"bP{dh}"]])
                            S.op("act", lambda e, dh=dh: e.copy(out=ple[:, dh * 512:(dh + 1) * 512], in_=bP[dh][:]), reads=[r[f"bP{dh}"]], writes=[r["ple"]])
                        rmsnorm_stats(ple[:], junk[:], ss2[:, 0:1], ss2[:, 1:2], r["ple"], r0["junk"], r["ss2"], D)
                        S.op("dve", lambda e: e.scalar_tensor_tensor(out=ple[:], in0=ple[:], scalar=ss2[:, 1:2], in1=gv[:, 2, :], op0=ALU.mult, op1=ALU.mult),
                             reads=[r["ple"], r["ss2"], r0["gv"]], writes=[r["ple"]])
                        S.op("dve", lambda e: e.tensor_tensor(out=ple[:], in0=ple[:], in1=gate[:], op=ALU.mult), reads=[r["ple"], r["gate"]], writes=[r["ple"]])
                        S.op("dve", lambda e, tl=tl: e.tensor_tensor(out=h3[:], in0=ple[:], in1=acc[:, tl, :], op=ALU.add), reads=[r["ple"], r0["acc"]], writes=[r["h3"]])
                        rmsnorm_stats(h3[:], junk[:], ss2[:, 2:3], ss2[:, 3:4], r["h3"], r0["junk"], r["ss2"], D)
                        S.op("dve", lambda e, pi=pi: e.scalar_tensor_tensor(out=ots[pi][:], in0=h3[:], scalar=ss2[:, 3:4], in1=gv[:, 3, :], op0=ALU.mult, op1=ALU.mult),
                             reads=[r["h3"], r["ss2"], r0["gv"]], writes=[r[f"ot{pi}"]])
                        S.dma("sp", lambda e, pi=pi, t=t: e.dma_start(out=out[t * 128:(t + 1) * 128, :], in_=ots[pi][:]), reads=[r[f"ot{pi}"]], writes=[r0["outd"]], sem_res=r[f"ot{pi}"])
                    S.barrier()
                    S.release(list(r.values()))
        if 'A' in phases:
            phase_A()
        if 'B' in phases:
            phase_B()
        if 'C' in phases:
            phase_C()
        S.barrier()
        build_nc.last_log = S.log
        S.emit(block)
    return nc


def prep_core_inputs(inp, b, half, SP, SO):
    x = inp["x"]
    f = np.float32
    if half == 0:
        xp = np.zeros((SP, D), f)
    else:
        xp = np.ascontiguousarray(x[b, 0:SP])
    xo = np.ascontiguousarray(x[b, half * SP: half * SP + SO]) if half == 1 else np.ascontiguousarray(x[b, 0:SO])
    p = inp["p"][0, b]
    po = np.ascontiguousarray(p[half * SP: half * SP + SO]) if half == 1 else np.ascontiguousarray(p[0:SO])
    cq = np.ascontiguousarray(inp["conv_q"][0].T.reshape(4, 128, 4).transpose(1, 0, 2).reshape(128, 16))
    ck = np.ascontiguousarray(inp["conv_k"][0].T.reshape(4, 128, 4).transpose(1, 0, 2).reshape(128, 16))
    gvec = np.stack([inp["g_mix"][0], inp["g_ffn"][0], inp["g_ple"][0], inp["g_ple_post"][0], inp["g_final"]]).astype(f)
    wr = np.concatenate([inp["w_router_group"][0], inp["w_router_expert"][0]], axis=1).astype(f)
    br = np.concatenate([inp["b_router_group"][0], inp["b_router_expert"][0]])[None, :].astype(f)
    return {
        "xp": xp, "xo": xo, "po": po,
        "win": np.ascontiguousarray(inp["w_in"][0]),
        "gvec": np.ascontiguousarray(gvec),
        "gmh": np.ascontiguousarray(inp["g_mhead"]),
        "bg": np.ascontiguousarray(inp["b_gates"]),
        "cq": cq, "ck": ck,
        "wout": np.ascontiguousarray(inp["w_out"][0]),
        "wr": np.ascontiguousarray(wr), "br": np.ascontiguousarray(br),
        "wg": np.ascontiguousarray(inp["w_exp_gate"][0]),
        "wu": np.ascontiguousarray(inp["w_exp_up"][0]),
        "wd": np.ascontiguousarray(inp["w_exp_down"][0]),
        "wpg": np.ascontiguousarray(inp["w_ple_gate"][0]),
        "wpp": np.ascontiguousarray(inp["w_ple_proj"][0]),
    }


def kernel(**inputs):
    inp = {k: np.asarray(v) for k, v in inputs.items()}
    x = inp["x"]
    B, SEQ, _ = x.shape
    SH = SEQ // 2
    import os
    nc = build_nc(SH, SH, phases=os.environ.get("KPHASES", "ABC"))
    in_maps = []
    for c in range(8):
        b, half = c // 2, c % 2
        in_maps.append(prep_core_inputs(inp, b, half, SH, SH))
    res = run_bass_kernel_spmd(nc, in_maps, core_ids=list(range(8)))
    out = np.empty((B, SEQ, D), np.float32)
    for c in range(8):
        b, half = c // 2, c % 2
        out[b, half * SH:(half + 1) * SH] = res.results[c]["out"]
    return out
```

```python
import math
from contextlib import ExitStack
import numpy as np
import concourse.bass as bass
import concourse.mybir as mybir
from concourse.bass_utils import run_bass_kernel_spmd

F32 = mybir.dt.float32
BF16 = mybir.dt.bfloat16
AF = mybir.ActivationFunctionType
ALU = mybir.AluOpType
AX = mybir.AxisListType

D = 1024
NCOL = 3592
EPS = 1e-6
LNC = math.log(128.0 ** -0.5)
NEXP = 32


class Res:
    __slots__ = ("name", "last_w", "readers", "dsem")

    def __init__(self, name):
        self.name = name
        self.last_w = None
        self.readers = []
        self.dsem = None


class Sched:
    ENGS = ("pe", "act", "dve", "pool", "sp")

    def __init__(self, nc):
        self.nc = nc
        self.ops = {e: [] for e in self.ENGS}
        self.cnt = {e: 0 for e in self.ENGS}
        self.sem = {}
        self.waited = {e: {} for e in self.ENGS}
        self.dcnt = {}
        self.dsem_objs = {}
        self.free_dsems = []
        import os
        self.limit = int(os.environ.get("KSTOP", "0")) or None
        self.total = 0
        self.log = []

    def set_sems(self, sems, dma_sems):
        for e, s in zip(self.ENGS, sems):
            self.sem[e] = s
        self.free_dsems = list(dma_sems)

    def res(self, name):
        return Res(name)

    def _deps(self, eng, reads, writes, self_sync):
        deps = {}

        def add(ev):
            if ev is None:
                return
            s, v, src = ev
            if src == eng and not self_sync:
                return
            k = id(s)
            if k not in deps or deps[k][1] < v:
                deps[k] = (s, v)
        for r in reads:
            add(r.last_w)
        for w in writes:
            add(w.last_w)
            for e in w.readers:
                add(e)
        waits = []
        wd = self.waited[eng]
        for k, (s, v) in deps.items():
            if wd.get(k, 0) < v:
                wd[k] = v
                waits.append((s, v))
        return waits

    def _commit(self, ev, reads, writes):
        for r in reads:
            r.readers.append(ev)
            if len(r.readers) > 64:
                r.readers = r.readers[-48:] if False else r.readers
        for w in writes:
            w.last_w = ev
            w.readers = []

    def op(self, eng, fn, reads=(), writes=(), self_sync=None):
        self.total += 1
        if self.limit and self.total > self.limit:
            return None
        import sys as _sys
        self.log.append((self.total, eng, _sys._getframe(1).f_lineno))
        if self_sync is None:
            self_sync = eng != "pe"
        waits = self._deps(eng, reads, writes, self_sync)
        self.cnt[eng] += 1
        ev = (self.sem[eng], self.cnt[eng], eng)
        self.ops[eng].append((waits, fn, (self.sem[eng], 1)))
        self._commit(ev, reads, writes)
        return ev

    def dma(self, eng, fn, reads=(), writes=(), sem_res=None):
        self.total += 1
        if self.limit and self.total > self.limit:
            return None
        import sys as _sys
        self.log.append((self.total, "dma-" + eng, _sys._getframe(1).f_lineno))
        owner = sem_res or (writes[0] if writes else reads[0])
        if owner.dsem is None:
            owner.dsem = self.free_dsems.pop()
            self.dcnt.setdefault(id(owner.dsem), 0)
            self.dsem_objs[id(owner.dsem)] = owner.dsem
        s = owner.dsem
        waits = self._deps(eng, reads, writes, True)
        self.dcnt[id(s)] += 16
        ev = (s, self.dcnt[id(s)], "dma")
        self.ops[eng].append((waits, fn, (s, 16)))
        self._commit(ev, reads, writes)
        return ev

    def release(self, res_list):
        for r in res_list:
            if r.dsem is not None:
                self.free_dsems.append(r.dsem)
                r.dsem = None

    def barrier(self):
        for eng in self.ENGS:
            waits = []
            wd = self.waited[eng]
            for x in self.ENGS:
                s, v = self.sem[x], self.cnt[x]
                if v > 0 and wd.get(id(s), 0) < v:
                    wd[id(s)] = v
                    waits.append((s, v))
            for k, v in self.dcnt.items():
                if v > 0 and wd.get(k, 0) < v:
                    wd[k] = v
                    waits.append((self.dsem_objs[k], v))
            if waits:
                self.ops[eng].append((waits, None, None))

    def emit(self, block):
        sched = self

        def run(engname, handle):
            for waits, fn, inc in sched.ops[engname]:
                for (s, v) in waits:
                    handle.wait_ge(s, v)
                if fn is not None:
                    ins = fn(handle)
                    ins.then_inc(inc[0], inc[1])

        @block.tensor
        def _(e):
            run("pe", e)

        @block.scalar
        def _(e):
            run("act", e)

        @block.vector
        def _(e):
            run("dve", e)

        @block.gpsimd
        def _(e):
            run("pool", e)

        @block.sync
        def _(e):
            run("sp", e)


def build_nc(SP, SO, phases="ABC", dbg=False):
    assert SP % 512 == 0 and SO % 512 == 0
    ST = SP + SO
    NGP, NGO = SP // 512, SO // 512
    NG = NGP + NGO
    NT = ST // 128
    NTO = SO // 128
    nc = bass.Bass("TRN2", target_bir_lowering=False)

    def din(name, shape, dt=F32):
        return nc.dram_tensor(name, list(shape), dt, kind="ExternalInput").ap()

    xp = din("xp", [SP, D])
    xo = din("xo", [SO, D])
    po = din("po", [SO, 256])
    win = din("win", [D, NCOL])
    gvec = din("gvec", [5, D])
    gmh = din("gmh", [1, 512])
    bg = din("bg", [1, 8])
    cq = din("cq", [128, 16])
    ck = din("ck", [128, 16])
    wout = din("wout", [D, D])
    wr = din("wr", [D, 36])
    br = din("br", [1, 36])
    wg = din("wg", [NEXP, D, 512])
    wu = din("wu", [NEXP, D, 512])
    wd = din("wd", [NEXP, 512, D])
    wpg = din("wpg", [D, D])
    wpp = din("wpp", [256, D])
    out = nc.dram_tensor("out", [SO, D], F32, kind="ExternalOutput").ap()

    skind = "ExternalOutput" if dbg else "Internal"
    dbgA = nc.dram_tensor("dbgA", [64, 2048], F32, kind=skind).ap()
    KT_d = nc.dram_tensor("KT_d", [8, 64, ST], BF16, kind=skind).ap()
    QT_d = nc.dram_tensor("QT_d", [8, 64, SO], BF16, kind=skind).ap()
    V_d = nc.dram_tensor("V_d", [8, 128, NT, 64], BF16, kind=skind).ap()
    mixTm_d = nc.dram_tensor("mixTm_d", [NTO, 128, 4, 128], BF16, kind=skind).ap()
    mixTs_d = nc.dram_tensor("mixTs_d", [NTO, 64, 8, 128], BF16, kind=skind).ap()

    es = ExitStack()
    with es:
        sems = [es.enter_context(nc.semaphore(f"s_{e}")) for e in Sched.ENGS]
        dsems = [es.enter_context(nc.semaphore(f"d_{i}")) for i in range(48)]
        block = es.enter_context(nc.Block())
        S = Sched(nc)
        S.set_sems(sems, dsems)
        R = S.res

        uid = [0]

        def sbt(stack, name, shape, dt):
            uid[0] += 1
            return stack.enter_context(nc.sbuf_tensor(f"{name}_{uid[0]}", list(shape), dt))

        def pst(stack, name, shape, dt):
            uid[0] += 1
            return stack.enter_context(nc.psum_tensor(f"{name}_{uid[0]}", list(shape), dt))

        identf = sbt(es, "identf", [128, 128], F32)
        identb = sbt(es, "identb", [128, 128], BF16)
        onesb = sbt(es, "onesb", [128, 128], BF16)
        trilb = sbt(es, "trilb", [128, 128], BF16)
        tmpf = sbt(es, "tmpf", [128, 128], F32)
        triu64 = sbt(es, "triu64", [64, 64], F32)
        ones64 = sbt(es, "ones64", [64, 128], F32)
        negm = sbt(es, "negm", [64, 64], F32)
        r_const = R("const")

        S.op("pool", lambda e: e.memset(identf[:], 0.0), writes=[r_const])
        S.op("pool", lambda e: e.affine_select(out=identf[:], in_=identf[:], pattern=[[-1, 128]], compare_op=ALU.not_equal,
                                               fill=1.0, base=0, channel_multiplier=1), reads=[r_const], writes=[r_const])
        S.op("dve", lambda e: e.tensor_copy(out=identb[:], in_=identf[:]), reads=[r_const], writes=[r_const])
        S.op("pool", lambda e: e.memset(onesb[:], 1.0), writes=[r_const])
        S.op("pool", lambda e: e.memset(tmpf[:], 1.0), writes=[r_const])
        S.op("pool", lambda e: e.affine_select(out=tmpf[:], in_=tmpf[:], pattern=[[-1, 128]], compare_op=ALU.is_ge,
                                               fill=0.0, base=0, channel_multiplier=1), reads=[r_const], writes=[r_const])
        S.op("dve", lambda e: e.tensor_copy(out=trilb[:], in_=tmpf[:]), reads=[r_const], writes=[r_const])
        S.op("pool", lambda e: e.memset(triu64[:], 1.0), writes=[r_const])
        S.op("pool", lambda e: e.affine_select(out=triu64[:], in_=triu64[:], pattern=[[1, 64]], compare_op=ALU.is_ge,
                                               fill=0.0, base=0, channel_multiplier=-1), reads=[r_const], writes=[r_const])
        S.op("pool", lambda e: e.memset(ones64[:], 1.0), writes=[r_const])
        S.op("pool", lambda e: e.memset(negm[:], 0.0), writes=[r_const])
        S.op("pool", lambda e: e.affine_select(out=negm[:], in_=negm[:], pattern=[[1, 64]], compare_op=ALU.is_ge,
                                               fill=30000.0, base=0, channel_multiplier=-1), reads=[r_const], writes=[r_const])
        triu64b = sbt(es, "triu64b", [64, 64], BF16)
        ones64b = sbt(es, "ones64b", [64, 128], BF16)
        negmb = sbt(es, "negmb", [64, 64], BF16)
        S.op("dve", lambda e: e.tensor_copy(out=triu64b[:], in_=triu64[:]), reads=[r_const], writes=[r_const])
        S.op("dve", lambda e: e.tensor_copy(out=ones64b[:], in_=ones64[:]), reads=[r_const], writes=[r_const])
        S.op("dve", lambda e: e.tensor_copy(out=negmb[:], in_=negm[:]), reads=[r_const], writes=[r_const])
        S.barrier()

        def rmsnorm_stats(src_ap, junk, ss, rstd, r_src, r_junk, r_ss, n):
            S.op("pool", lambda e: e.memset(ss, 0.0), writes=[r_ss])
            S.op("act", lambda e: e.activation(out=junk, in_=src_ap, func=AF.Square, accum_out=ss), reads=[r_src, r_ss], writes=[r_junk, r_ss])
            S.op("dve", lambda e: e.tensor_scalar(out=rstd, in0=ss, scalar1=1.0 / n, scalar2=EPS, op0=ALU.mult, op1=ALU.add),
                 reads=[r_ss], writes=[r_ss])
            S.op("act", lambda e: e.sqrt(out=rstd, in_=rstd), reads=[r_ss], writes=[r_ss])
            S.op("dve", lambda e: e.reciprocal(out=rstd, in_=rstd), reads=[r_ss], writes=[r_ss])

        def phase_A():
          with ExitStack() as ea:
            winb = sbt(ea, "winb", [128, 8, NCOL], BF16)
            gmix = sbt(ea, "gmix", [128, D], F32)
            gmht = sbt(ea, "gmht", [64, 512], F32)
            bgt = sbt(ea, "bgt", [64, 8], F32)
            cqt = sbt(ea, "cqt", [128, 16], F32)
            ckt = sbt(ea, "ckt", [128, 16], F32)
            xts = [sbt(ea, f"xt{i}", [128, D], F32) for i in range(2)]
            junk = sbt(ea, "junk", [128, D], BF16)
            ssq = sbt(ea, "ssq", [128, 2], F32)
            a_bf = sbt(ea, "a_bf", [128, D], BF16)
            aT = sbt(ea, "aT", [128, 8, 512], BF16)
            qpre = sbt(ea, "qpre", [128, 4, 515], F32)
            kpre = sbt(ea, "kpre", [128, 4, 515], F32)
            ctmp = [sbt(ea, f"ctmp{i}", [128, 512], F32) for i in range(2)]
            qTm = sbt(ea, "qTm", [128, 4, 512], BF16)
            kTm = sbt(ea, "kTm", [128, 4, 512], BF16)
            sqT = sbt(ea, "sqT", [128, 4, 512], BF16)
            skT = sbt(ea, "skT", [128, 4, 512], BF16)
            svt = sbt(ea, "svt", [128, 8, 4, 64], BF16)
            ktok = sbt(ea, "ktok", [64, 4, 128], BF16)
            vext = sbt(ea, "vext", [64, 512], BF16)
            onecol = sbt(ea, "onecol", [64, 2], BF16)
            gvext = sbt(ea, "gvext", [64, 512], BF16)
            gamb = sbt(ea, "gamb", [64, 4], BF16)
            gsb = sbt(ea, "gsb", [64, 8], F32)
            lfs = sbt(ea, "lfs", [64, 4], F32)
            lfb = sbt(ea, "lfb", [64, 8, 64], BF16)
            lf2 = sbt(ea, "lf2", [64, 8], BF16)
            lft = sbt(ea, "lft", [64, 4], F32)
            bs = sbt(ea, "bs", [64, 8], F32)
            alpha = sbt(ea, "alpha", [64, 4], F32)
            gamma = sbt(ea, "gamma", [64, 4], F32)
            garg = sbt(ea, "garg", [64, 4], F32)
            dcoef = sbt(ea, "dcoef", [64, 4], F32)
            eB = sbt(ea, "eB", [128, 4], F32)
            DTs = sbt(ea, "DTs", [64, 4, 64], F32)
            WT = sbt(ea, "WT", [64, 4, 64], BF16)
            n2s = sbt(ea, "n2s", [64, 4, 128], F32)
            numsb = sbt(ea, "numsb", [64, 4, 128], F32)
            sqh = sbt(ea, "sqh", [64, 4, 128], F32)
            dsb = sbt(ea, "dsb", [64, 8], F32)
            den = sbt(ea, "den", [64, 4], F32)
            ssh = sbt(ea, "ssh", [64, 4], F32)
            sig = sbt(ea, "sig", [64, 512], F32)
            mixtok = sbt(ea, "mixtok", [64, 4, 128], BF16)
            mixTg = sbt(ea, "mixTg", [128, 4, 512], BF16)
            Cst = sbt(ea, "Cst", [128, 4, 128], F32)
            nst = sbt(ea, "nst", [128, 4], F32)
            Cbf = sbt(ea, "Cbf", [128, 512], BF16)
            nbf = sbt(ea, "nbf", [128, 4], BF16)

            bF = pst(ea, "bF", [128, 512], F32)
            bSm = pst(ea, "bSm", [128, 512], F32)
            bV = pst(ea, "bV", [128, 512], F32)
            bO = pst(ea, "bO", [128, 512], F32)
            bAB = pst(ea, "bAB", [128, 512], F32)
            bN = pst(ea, "bN", [128, 512], F32)
            bN2 = pst(ea, "bN2", [128, 512], F32)
            bT = pst(ea, "bT", [128, 1024], BF16)

            names = ["winb", "gmix", "small", "xt0", "xt1", "junk", "ssq", "a_bf", "aT", "qpre", "kpre", "ctmp0", "ctmp1", "qTm", "kTm",
                     "sqT", "skT", "svt", "ktok", "vext", "gvext", "gsb", "lfs", "lfb", "bs", "alpha", "gamma", "garg", "dcoef", "eB",
                     "DTs", "WT", "n2s", "numsb", "sqh", "dsb", "den", "ssh", "sig", "mixtok", "mixTg", "Cst", "nst", "Cbf",
                     "bF", "bG", "bS", "bE", "bD", "bDn", "bV", "bO", "bA", "bB", "bN", "bN2", "bT",
                     "KTd", "QTd", "Vd", "mixTmd"]
            r = {k: R(k) for k in names}

            for c in range(8):
                S.dma("pool", lambda e, c=c: e.dma_start(out=winb[:, c, :], in_=win[c * 128:(c + 1) * 128, :]), writes=[r["winb"]])
            S.dma("sp", lambda e: e.dma_start(out=gmix[:], in_=gvec[0:1, :].partition_broadcast(128)), writes=[r["gmix"]])
            S.dma("sp", lambda e: e.dma_start(out=gmht[:], in_=gmh.partition_broadcast(64)), writes=[r["small"]])
            S.dma("sp", lambda e: e.dma_start(out=bgt[:], in_=bg.partition_broadcast(64)), writes=[r["small"]])
            S.dma("sp", lambda e: e.dma_start(out=cqt[:], in_=cq), writes=[r["small"]])
            S.dma("sp", lambda e: e.dma_start(out=ckt[:], in_=ck), writes=[r["small"]])
            S.op("pool", lambda e: e.memset(Cst[:], 0.0), writes=[r["Cst"]])
            S.op("pool", lambda e: e.memset(nst[:], 0.0), writes=[r["nst"]])
            S.op("pool", lambda e: e.memset(Cbf[:], 0.0), writes=[r["Cbf"]])
            S.op("pool", lambda e: e.memset(onecol[:], 1.0), writes=[r["vext"]])
            S.op("pool", lambda e: e.memset(nbf[:], 0.0), writes=[r["Cbf"]])
            S.op("pool", lambda e: e.memset(qpre[:], 0.0), writes=[r["qpre"]])
            S.op("pool", lambda e: e.memset(kpre[:], 0.0), writes=[r["kpre"]])

            evac_i = [0]

            def evac(out_ap, in_ap, reads, writes, scale=None):
                evac_i[0] += 1
                if scale is not None or evac_i[0] % 2 == 0:
                    if scale is None:
                        S.op("act", lambda e: e.copy(out=out_ap, in_=in_ap), reads=reads, writes=writes)
                    else:
                        S.op("act", lambda e: e.mul(out=out_ap, in_=in_ap, mul=scale), reads=reads, writes=writes)
                else:
                    S.op("dve", lambda e: e.tensor_copy(out=out_ap, in_=in_ap), reads=reads, writes=writes)

            for G in range(NG):
                own = G >= NGP
                src = xo if own else xp
                row0 = (G - NGP if own else G) * 512
                for ti in range(4):
                    xi = ti % 2
                    xt = xts[xi]
                    rx = r[f"xt{xi}"]
                    S.dma("sp", lambda e, xt=xt, a=row0 + ti * 128, src=src: e.dma_start(out=xt[:], in_=src[a:a + 128, :]), writes=[rx])
                    rmsnorm_stats(xt[:], junk[:], ssq[:, 0:1], ssq[:, 1:2], rx, r["junk"], r["ssq"], D)
                    S.op("dve", lambda e, xt=xt: e.scalar_tensor_tensor(out=a_bf[:], in0=xt[:], scalar=ssq[:, 1:2], in1=gmix[:], op0=ALU.mult, op1=ALU.mult),
                         reads=[rx, r["ssq"], r["gmix"]], writes=[r["a_bf"]])
                    for c in range(8):
                        S.op("pe", lambda e, c=c: e.transpose(out=bT[:, c * 128:(c + 1) * 128], in_=a_bf[:, c * 128:(c + 1) * 128], identity=identb[:]),
                             reads=[r["a_bf"]], writes=[r["bT"]])
                    evac(aT[:, :, ti * 128:(ti + 1) * 128], bT[:].rearrange("p (c t) -> p c t", c=8), [r["bT"]], [r["aT"]])

                def fproj(col0, dst_ap, r_dst, scale=None):
                    for c in range(8):
                        S.op("pe", lambda e, c=c: e.matmul(out=bF[:], lhsT=winb[:, c, col0:col0 + 128], rhs=aT[:, c, :], start=(c == 0), stop=(c == 7)),
                             reads=[r["winb"], r["aT"]], writes=[r["bF"]])
                    evac(dst_ap, bF[:], [r["bF"]], [r_dst], scale=scale)

                for h in range(4):
                    if own:
                        fproj(h * 128, qpre[:, h, 3:515], r["qpre"])
                    fproj(512 + h * 128, kpre[:, h, 3:515], r["kpre"])
                for j in range(4):
                    if own:
                        fproj(2056 + j * 128, sqT[:, j, :], r["sqT"], scale=0.125)
                    fproj(2568 + j * 128, skT[:, j, :], r["skT"])

                def conv(pre, cwt, dstT, r_pre, r_dst):
                    for h in range(4):
                        tmp = ctmp[h % 2]
                        rt = r[f"ctmp{h % 2}"]
                        S.op("dve", lambda e, h=h, tmp=tmp: e.tensor_scalar(out=tmp[:], in0=pre[:, h, 0:512], scalar1=cwt[:, h * 4:h * 4 + 1], scalar2=None, op0=ALU.mult),
                             reads=[r_pre, r["small"]], writes=[rt])
                        for j in range(1, 4):
                            S.op("dve", lambda e, h=h, j=j, tmp=tmp: e.scalar_tensor_tensor(out=tmp[:], in0=pre[:, h, j:j + 512], scalar=cwt[:, h * 4 + j:h * 4 + j + 1],
                                                                                          in1=tmp[:], op0=ALU.mult, op1=ALU.add),
                                 reads=[r_pre, rt], writes=[rt])
                        S.op("act", lambda e, h=h, tmp=tmp: e.activation(out=dstT[:, h, :], in_=tmp[:], func=AF.Silu), reads=[rt], writes=[r_dst])
                    S.op("pool", lambda e: e.tensor_copy(out=pre[:, :, 0:3], in_=pre[:, :, 512:515]), reads=[r_pre], writes=[r_pre])

                if own:
                    conv(qpre, cqt, qTm, r["qpre"], r["qTm"])
                else:
                    if G == NGP - 1:
                        for h in range(4):
                            for c in range(8):
                                S.op("pe", lambda e, c=c, h=h: e.matmul(out=bF[:], lhsT=winb[:, c, h * 128:h * 128 + 128], rhs=aT[:, c, :], start=(c == 0), stop=(c == 7)),
                                     reads=[r["winb"], r["aT"]], writes=[r["bF"]])
                            evac(qpre[:, h, 0:3], bF[:, 509:512], [r["bF"]], [r["qpre"]])
                conv(kpre, ckt, kTm, r["kpre"], r["kTm"])

                tokg = G * 512
                S.dma("sp", lambda e, tokg=tokg: e.dma_start(out=KT_d.rearrange("(j two) d t -> (two d) j t", two=2)[:, :, tokg:tokg + 512], in_=skT[:]),
                      reads=[r["skT"]], writes=[r["KTd"]], sem_res=r["skT"])
                if own:
                    S.dma("sp", lambda e, a=row0: e.dma_start(out=QT_d.rearrange("(j two) d t -> (two d) j t", two=2)[:, :, a:a + 512], in_=sqT[:]),
                          reads=[r["sqT"]], writes=[r["QTd"]], sem_res=r["sqT"])
                for ti in range(4):
                    for c in range(8):
                        S.op("pe", lambda e, c=c, ti=ti: e.matmul(out=bF[:], lhsT=aT[:, c, ti * 128:(ti + 1) * 128], rhs=winb[:, c, 3080:3592], start=(c == 0), stop=(c == 7)),
                             reads=[r["winb"], r["aT"]], writes=[r["bF"]])
                    evac(svt[:, :, ti, :], bF[:].rearrange("p (h d) -> p h d", h=8), [r["bF"]], [r["svt"]])
                S.dma("sp", lambda e, G=G: e.dma_start(out=V_d.rearrange("h s j d -> s h j d")[:, :, 4 * G:4 * G + 4, :], in_=svt[:]),
                      reads=[r["svt"]], writes=[r["Vd"]], sem_res=r["svt"])

                for ci in range(8):
                    c0 = ci * 64
                    for c in range(8):
                        S.op("pe", lambda e, c=c, c0=c0: e.matmul(out=bV[0:64, :], lhsT=aT[:, c, c0:c0 + 64], rhs=winb[:, c, 1024:1536], start=(c == 0), stop=(c == 7)),
                             reads=[r["winb"], r["aT"]], writes=[r["bV"]])
                    for c in range(8):
                        S.op("pe", lambda e, c=c, c0=c0: e.matmul(out=bSm[0:64, 0:8], lhsT=aT[:, c, c0:c0 + 64], rhs=winb[:, c, 2048:2056], start=(c == 0), stop=(c == 7)),
                             reads=[r["winb"], r["aT"]], writes=[r["bG"]])
                    if own:
                        for c in range(8):
                            S.op("pe", lambda e, c=c, c0=c0: e.matmul(out=bO[0:64, :], lhsT=aT[:, c, c0:c0 + 64], rhs=winb[:, c, 1536:2048], start=(c == 0), stop=(c == 7)),
                                 reads=[r["winb"], r["aT"]], writes=[r["bO"]])
                    for h in range(4):
                        S.op("pe", lambda e, h=h, c0=c0: e.transpose(out=bT[0:64, h * 128:(h + 1) * 128], in_=kTm[:, h, c0:c0 + 64], identity=identb[:]),
                             reads=[r["kTm"]], writes=[r["bT"]])
                    evac(ktok[:], bT[0:64, 0:512].rearrange("p (h d) -> p h d", h=4), [r["bT"]], [r["ktok"]])
                    S.op("dve", lambda e: e.tensor_tensor(out=gsb[:], in0=bSm[0:64, 0:8], in1=bgt[:], op=ALU.add), reads=[r["bG"], r["small"]], writes=[r["gsb"]])
                    S.op("act", lambda e: e.activation(out=lfs[:], in_=gsb[:, 4:8], func=AF.Exp, scale=-1.0), reads=[r["gsb"]], writes=[r["lfs"]])
                    S.op("act", lambda e: e.activation(out=lfs[:], in_=lfs[:], func=AF.Ln, bias=1.0), reads=[r["lfs"]], writes=[r["lfs"]])
                    S.op("dve", lambda e: e.tensor_copy(out=lf2[:, 0:4], in_=lfs[:]), reads=[r["lfs"]], writes=[r["lfb"]])
                    S.op("dve", lambda e: e.tensor_tensor(out=lft[:], in0=lfs[:], in1=lf2[:, 0:4], op=ALU.subtract), reads=[r["lfs"], r["lfb"]], writes=[r["lfb"]])
                    S.op("dve", lambda e: e.tensor_copy(out=lf2[:, 4:8], in_=lft[:]), reads=[r["lfb"]], writes=[r["lfb"]])
                    for q_ in range(2):
                        S.op("pe", lambda e, q_=q_: e.matmul(out=bSm[0:64, 8:12], lhsT=triu64b[:], rhs=lf2[:, q_ * 4:q_ * 4 + 4], start=(q_ == 0), stop=(q_ == 1)), reads=[r["lfb"]], writes=[r["bS"]])
                    for q_ in range(2):
                        S.op("pe", lambda e, q_=q_: e.matmul(out=bSm[0:64, 12:16], lhsT=ones64b[:, 0:64], rhs=lf2[:, q_ * 4:q_ * 4 + 4], start=(q_ == 0), stop=(q_ == 1)), reads=[r["lfb"]], writes=[r["bS"]])
                    for q_ in range(2):
                        S.op("pe", lambda e, q_=q_: e.matmul(out=bSm[:, 16:20], lhsT=ones64b[:], rhs=lf2[:, q_ * 4:q_ * 4 + 4], start=(q_ == 0), stop=(q_ == 1)), reads=[r["lfb"]], writes=[r["bE"]])
                    S.op("act", lambda e: e.copy(out=bs[:], in_=bSm[0:64, 8:16]), reads=[r["bS"]], writes=[r["bs"]])
                    S.op("act", lambda e: e.activation(out=eB[:], in_=bSm[:, 16:20], func=AF.Exp, scale=-1.0), reads=[r["bE"]], writes=[r["eB"]])
                    S.op("dve", lambda e: e.tensor_tensor(out=garg[:], in0=bs[:, 0:4], in1=bs[:, 4:8], op=ALU.subtract), reads=[r["bs"]], writes=[r["garg"]])
                    S.op("dve", lambda e: e.tensor_tensor(out=garg[:], in0=garg[:], in1=gsb[:, 0:4], op=ALU.add), reads=[r["garg"], r["gsb"]], writes=[r["garg"]])
                    S.op("act", lambda e: e.activation(out=gamma[:], in_=garg[:], func=AF.Exp), reads=[r["garg"]], writes=[r["gamma"]])
                    S.op("dve", lambda e: e.tensor_tensor(out=gvext[:].rearrange("p (h d) -> p h d", h=4), in0=bV[0:64, :].rearrange("p (h d) -> p h d", h=4),
                                                          in1=gamma[:].unsqueeze(2).to_broadcast([64, 4, 128]), op=ALU.mult),
                         reads=[r["bV"], r["gamma"]], writes=[r["gvext"]])
                    S.op("dve", lambda e: e.tensor_copy(out=gamb[:], in_=gamma[:]), reads=[r["gamma"]], writes=[r["gvext"]])
                    if own:
                        S.op("dve", lambda e: e.tensor_copy(out=vext[:], in_=bV[0:64, :]), reads=[r["bV"]], writes=[r["vext"]])
                        S.op("dve", lambda e: e.tensor_scalar(out=alpha[:], in0=bs[:, 0:4], scalar1=-1.0, scalar2=LNC, op0=ALU.mult, op1=ALU.add), reads=[r["bs"]], writes=[r["alpha"]])
                        S.op("act", lambda e: e.activation(out=alpha[:], in_=alpha[:], func=AF.Exp), reads=[r["alpha"]], writes=[r["alpha"]])
                        S.op("dve", lambda e: e.scalar_tensor_tensor(out=dcoef[:], in0=bs[:, 0:4], scalar=LNC, in1=gsb[:, 0:4], op0=ALU.add, op1=ALU.add),
                             reads=[r["bs"], r["gsb"]], writes=[r["dcoef"]])
                        S.op("dve", lambda e: e.tensor_copy(out=lfb[:], in_=lf2[:].unsqueeze(2).to_broadcast([64, 8, 64])), reads=[r["lfb"]], writes=[r["lfb"]])
                        for h in range(4):
                            S.op("pe", lambda e, h=h, c0=c0: e.matmul(out=bAB[0:64, h * 64:(h + 1) * 64], lhsT=kTm[:, h, c0:c0 + 64], rhs=qTm[:, h, c0:c0 + 64], start=True, stop=True),
                                 reads=[r["kTm"], r["qTm"]], writes=[r["bA"]])
                            S.op("pe", lambda e, h=h: e.matmul(out=bAB[0:64, 256 + h * 64:256 + (h + 1) * 64], lhsT=lfb[:, h, :], rhs=triu64b[:], start=True, stop=False),
                                 reads=[r["lfb"]], writes=[r["bB"]])
                            S.op("pe", lambda e, h=h: e.matmul(out=bAB[0:64, 256 + h * 64:256 + (h + 1) * 64], lhsT=lfb[:, 4 + h, :], rhs=triu64b[:], start=False, stop=False),
                                 reads=[r["lfb"]], writes=[r["bB"]])
                            S.op("pe", lambda e, h=h: e.matmul(out=bAB[0:64, 256 + h * 64:256 + (h + 1) * 64], lhsT=identb[0:64, 0:64], rhs=negmb[:], start=False, stop=True),
                                 reads=[r["lfb"]], writes=[r["bB"]])
                        for h in range(4):
                            S.op("act", lambda e, h=h: e.activation(out=DTs[:, h, :], in_=bAB[0:64, 256 + h * 64:256 + (h + 1) * 64], func=AF.Exp, scale=-1.0, bias=dcoef[:, h:h + 1]),
                                 reads=[r["bB"], r["dcoef"]], writes=[r["DTs"]])
                        S.op("dve", lambda e: e.tensor_tensor(out=WT[:].rearrange("p h t -> p (h t)"), in0=bAB[0:64, 0:256], in1=DTs[:].rearrange("p h t -> p (h t)"), op=ALU.mult),
                             reads=[r["bA"], r["DTs"]], writes=[r["WT"]])
                        for h in range(4):
                            S.op("pe", lambda e, h=h: e.matmul(out=bN[0:64, h * 128:(h + 1) * 128], lhsT=WT[:, h, :], rhs=vext[:, h * 128:(h + 1) * 128], start=True, stop=True),
                                 reads=[r["WT"], r["vext"]], writes=[r["bN"]])
                            S.op("pe", lambda e, h=h: e.matmul(out=bSm[0:64, 20 + h:21 + h], lhsT=WT[:, h, :], rhs=onecol[:, 0:1], start=True, stop=True),
                                 reads=[r["WT"], r["vext"]], writes=[r["bD"]])
                            S.op("pe", lambda e, h=h, c0=c0: e.matmul(out=bN2[0:64, h * 128:(h + 1) * 128], lhsT=qTm[:, h, c0:c0 + 64], rhs=Cbf[:, h * 128:(h + 1) * 128], start=True, stop=True),
                                 reads=[r["qTm"], r["Cbf"]], writes=[r["bN2"]])
                            S.op("pe", lambda e, h=h, c0=c0: e.matmul(out=bSm[0:64, 24 + h:25 + h], lhsT=qTm[:, h, c0:c0 + 64], rhs=nbf[:, h:h + 1], start=True, stop=True),
                                 reads=[r["qTm"], r["Cbf"]], writes=[r["bD"]])
                        S.op("dve", lambda e: e.tensor_tensor(out=n2s[:], in0=bN2[0:64, :].rearrange("p (h d) -> p h d", h=4), in1=alpha[:].unsqueeze(2).to_broadcast([64, 4, 128]), op=ALU.mult),
                             reads=[r["bN2"], r["alpha"]], writes=[r["n2s"]])
                        S.op("dve", lambda e: e.tensor_tensor(out=numsb[:], in0=bN[0:64, :].rearrange("p (h d) -> p h d", h=4), in1=n2s[:], op=ALU.add),
                             reads=[r["bN"], r["n2s"]], writes=[r["numsb"]])
                        S.op("act", lambda e: e.copy(out=dsb[:], in_=bSm[0:64, 20:28]), reads=[r["bD"]], writes=[r["dsb"]])
                        S.op("dve", lambda e: e.tensor_tensor(out=den[:], in0=dsb[:, 4:8], in1=alpha[:], op=ALU.mult), reads=[r["dsb"], r["alpha"]], writes=[r["den"]])
                        S.op("dve", lambda e: e.tensor_tensor(out=den[:], in0=den[:], in1=dsb[:, 0:4], op=ALU.add), reads=[r["dsb"], r["den"]], writes=[r["den"]])
                        S.op("dve", lambda e: e.tensor_scalar(out=ssh[:], in0=den[:], scalar1=-1.0, scalar2=None, op0=ALU.mult), reads=[r["den"]], writes=[r["ssh"]])
                        S.op("dve", lambda e: e.tensor_tensor(out=den[:], in0=den[:], in1=ssh[:], op=ALU.max), reads=[r["den"], r["ssh"]], writes=[r["den"]])
                        S.op("dve", lambda e: e.tensor_scalar_max(out=den[:], in0=den[:], scalar1=1.0), reads=[r["den"]], writes=[r["den"]])
                        S.op("dve", lambda e: e.reciprocal(out=den[:], in_=den[:]), reads=[r["den"]], writes=[r["den"]])
                        S.op("dve", lambda e: e.tensor_tensor(out=numsb[:], in0=numsb[:], in1=den[:].unsqueeze(2).to_broadcast([64, 4, 128]), op=ALU.mult),
                             reads=[r["numsb"], r["den"]], writes=[r["numsb"]])
                        S.op("dve", lambda e: e.tensor_tensor(out=sqh[:], in0=numsb[:], in1=numsb[:], op=ALU.mult), reads=[r["numsb"]], writes=[r["sqh"]])
                        S.op("dve", lambda e: e.tensor_reduce(out=ssh[:], in_=sqh[:], axis=AX.X, op=ALU.add), reads=[r["sqh"]], writes=[r["ssh"]])
                        S.op("dve", lambda e: e.tensor_scalar(out=ssh[:], in0=ssh[:], scalar1=1.0 / 128, scalar2=EPS, op0=ALU.mult, op1=ALU.add), reads=[r["ssh"]], writes=[r["ssh"]])
                        S.op("act", lambda e: e.sqrt(out=ssh[:], in_=ssh[:]), reads=[r["ssh"]], writes=[r["ssh"]])
                        S.op("dve", lambda e: e.reciprocal(out=ssh[:], in_=ssh[:]), reads=[r["ssh"]], writes=[r["ssh"]])
                        S.op("dve", lambda e: e.tensor_tensor(out=numsb[:], in0=numsb[:], in1=ssh[:].unsqueeze(2).to_broadcast([64, 4, 128]), op=ALU.mult),
                             reads=[r["numsb"], r["ssh"]], writes=[r["numsb"]])
                        S.op("dve", lambda e: e.tensor_tensor(out=numsb[:], in0=numsb[:], in1=gmht[:].rearrange("p (h d) -> p h d", h=4), op=ALU.mult),
                             reads=[r["numsb"], r["small"]], writes=[r["numsb"]])
                        S.op("act", lambda e: e.activation(out=sig[:], in_=bO[0:64, :], func=AF.Sigmoid), reads=[r["bO"]], writes=[r["sig"]])
                        S.op("dve", lambda e: e.tensor_tensor(out=mixtok[:], in0=numsb[:], in1=sig[:].rearrange("p (h d) -> p h d", h=4), op=ALU.mult),
                             reads=[r["numsb"], r["sig"]], writes=[r["mixtok"]])
                        if dbg and G == NGP and ci == 1:
                            rdb = R("dbg")
                            dl = [(gsb, 0, 8), (lfs, 8, 4), (bs, 12, 8), (alpha, 20, 4), (gamma, 24, 4), (dcoef, 28, 4), (den, 32, 4), (dsb, 36, 8), (ssh, 44, 4)]
                            for (tt_, o_, n_) in dl:
                                S.dma("sp", lambda e, tt_=tt_, o_=o_, n_=n_: e.dma_start(out=dbgA[:, o_:o_ + n_], in_=tt_[:]), reads=[r["numsb"], r["ssh"], r["den"]], writes=[rdb])
                            S.dma("sp", lambda e: e.dma_start(out=dbgA[:, 64:320], in_=DTs[:].rearrange("p h t -> p (h t)")), reads=[r["DTs"]], writes=[rdb])
                            S.dma("sp", lambda e: e.dma_start(out=dbgA[:, 512:1024], in_=numsb[:].rearrange("p h t -> p (h t)")), reads=[r["numsb"]], writes=[rdb])
                            S.dma("sp", lambda e: e.dma_start(out=dbgA[:, 1024:1536], in_=n2s[:].rearrange("p h t -> p (h t)")), reads=[r["n2s"]], writes=[rdb])
                            S.dma("sp", lambda e: e.dma_start(out=dbgA[:, 1536:2048], in_=sig[:]), reads=[r["sig"]], writes=[rdb])
                        for h in range(4):
                            S.op("pe", lambda e, h=h: e.transpose(out=bT[:, 512 + h * 64:512 + (h + 1) * 64], in_=mixtok[:, h, :], identity=identb[0:64, 0:64]),
                                 reads=[r["mixtok"]], writes=[r["bT"]])
                        evac(mixTg[:, :, c0:c0 + 64], bT[:, 512:768].rearrange("p (h t) -> p h t", h=4), [r["bT"]], [r["mixTg"]])
                    for h in range(4):
                        S.op("pe", lambda e, h=h: e.matmul(out=bN[:, h * 128:(h + 1) * 128], lhsT=ktok[:, h, :], rhs=gvext[:, h * 128:(h + 1) * 128], start=True, stop=True),
                             reads=[r["ktok"], r["gvext"]], writes=[r["bN"]])
                        S.op("pe", lambda e, h=h: e.matmul(out=bSm[:, 28 + h:29 + h], lhsT=ktok[:, h, :], rhs=gamb[:, h:h + 1], start=True, stop=True),
                             reads=[r["ktok"], r["gvext"]], writes=[r["bDn"]])
                    for h in range(4):
                        S.op("dve", lambda e, h=h: e.scalar_tensor_tensor(out=Cst[:, h, :], in0=Cst[:, h, :], scalar=eB[:, h:h + 1], in1=bN[:, h * 128:(h + 1) * 128], op0=ALU.mult, op1=ALU.add),
                             reads=[r["Cst"], r["eB"], r["bN"]], writes=[r["Cst"]])
                    S.op("dve", lambda e: e.tensor_tensor(out=nst[:], in0=nst[:], in1=eB[:], op=ALU.mult), reads=[r["nst"], r["eB"]], writes=[r["nst"]])
                    S.op("dve", lambda e: e.tensor_tensor(out=nst[:], in0=nst[:], in1=bSm[:, 28:32], op=ALU.add), reads=[r["nst"], r["bDn"]], writes=[r["nst"]])
                    S.op("act", lambda e: e.copy(out=Cbf[:], in_=Cst[:].rearrange("p h d -> p (h d)")), reads=[r["Cst"]], writes=[r["Cbf"]])
                    S.op("act", lambda e: e.copy(out=nbf[:], in_=nst[:]), reads=[r["nst"]], writes=[r["Cbf"]])
                if own:
                    go = G - NGP
                    for j in range(4):
                        S.dma("sp", lambda e, go=go, j=j: e.dma_start(out=mixTm_d[4 * go + j], in_=mixTg[:, :, j * 128:(j + 1) * 128]),
                              reads=[r["mixTg"]], writes=[r["mixTmd"]], sem_res=r["mixTg"])
            S.barrier()
            S.release(list(r.values()))

        def phase_B():
          with ExitStack() as eb:
            KTh = [sbt(eb, f"KTh{i}", [64, ST], BF16) for i in range(2)]
            Vh = [sbt(eb, f"Vh{i}", [128, NT, 64], BF16) for i in range(2)]
            QTh = [sbt(eb, f"QTh{i}", [64, SO], BF16) for i in range(2)]
            maskf = sbt(eb, "maskf", [128, 4, 512], F32)
            Es = [sbt(eb, f"Es{i}", [128, 512], F32) for i in range(2)]
            Ls = [sbt(eb, f"Ls{i}", [128, 512], BF16) for i in range(2)]
            Aes = [sbt(eb, f"Aes{i}", [128, 512], F32) for i in range(2)]
            As = [sbt(eb, f"As{i}", [128, 512], BF16) for i in range(2)]
            CSs = [sbt(eb, f"CSs{i}", [128, 512], BF16) for i in range(2)]
            ost = [sbt(eb, f"ost{i}", [64, 512], BF16) for i in range(2)]
            bZ = [pst(eb, f"bZ{i}", [128, 512], F32) for i in range(2)]
            bRC = [pst(eb, f"bRC{i}", [128, 512], F32) for i in range(2)]
            bOT = [pst(eb, f"bOT{i}", [128, 512], F32) for i in range(2)]
            rn = ["mask", "mixTsd"] + [f"{n}{i}" for n in ["KTh", "Vh", "QTh", "Es", "Ls", "Aes", "As", "CSs", "ost", "bZ", "bRC", "bOT"] for i in range(2)]
            r = {k: R(k) for k in rn}
            for m in range(4):
                S.op("pool", lambda e, m=m: e.memset(maskf[:, m, :], 1.0), writes=[r["mask"]])
                S.op("pool", lambda e, m=m: e.affine_select(out=maskf[:, m, :], in_=maskf[:, m, :], pattern=[[1, 512]], compare_op=ALU.is_ge,
                                                            fill=0.0, base=-(m * 128) - 1, channel_multiplier=-1), reads=[r["mask"]], writes=[r["mask"]])
            blocks = []
            head_start = {}
            for h in range(8):
                head_start[h] = len(blocks)
                for gq in range(NGO):
                    jd0 = (NGP + gq) * 4
                    first = True
                    for j in range(jd0 + 3, -1, -1):
                        blocks.append(dict(h=h, hs=h % 2, gq=gq, j=j, m=j - jd0, first=first, last=(j == 0), oi=(h * NGO + gq) % 2))
                        first = False
            NB = len(blocks)

            def load_head(h):
                hs = h % 2
                S.dma("sp", lambda e: e.dma_start(out=KTh[hs][:], in_=KT_d[h]), writes=[r[f"KTh{hs}"]])
                S.dma("sp", lambda e: e.dma_start(out=Vh[hs][:], in_=V_d[h]), writes=[r[f"Vh{hs}"]])
                S.dma("sp", lambda e: e.dma_start(out=QTh[hs][:], in_=QT_d[h]), writes=[r[f"QTh{hs}"]])

            def stage1(k):
                B_ = blocks[k]
                b2, hs, j, gq, m = k % 2, B_["hs"], B_["j"], B_["gq"], B_["m"]
                S.op("pe", lambda e: e.matmul(out=bZ[b2][:], lhsT=KTh[hs][:, j * 128:(j + 1) * 128], rhs=QTh[hs][:, gq * 512:(gq + 1) * 512], start=True, stop=True),
                     reads=[r[f"KTh{hs}"], r[f"QTh{hs}"]], writes=[r[f"bZ{b2}"]])
                S.op("act", lambda e: e.activation(out=Es[b2][:], in_=bZ[b2][:], func=AF.Exp), reads=[r[f"bZ{b2}"]], writes=[r[f"Es{b2}"]])
                if m >= 0:
                    S.op("dve", lambda e: e.tensor_tensor(out=Es[b2][:], in0=Es[b2][:], in1=maskf[:, m, :], op=ALU.mult),
                         reads=[r[f"Es{b2}"], r["mask"]], writes=[r[f"Es{b2}"]])

            def stage1b(k):
                b2 = k % 2
                S.op("act", lambda e: e.activation(out=Ls[b2][:], in_=Es[b2][:], func=AF.Ln, bias=1.0), reads=[r[f"Es{b2}"]], writes=[r[f"Ls{b2}"]])

            def stage2(k):
                B_ = blocks[k]
                b2, pc, first, last = k % 2, (k - 1) % 2, B_["first"], B_["last"]
                S.op("pe", lambda e: e.matmul(out=bRC[b2][:], lhsT=trilb[:], rhs=Ls[b2][:], start=True, stop=first),
                     reads=[r[f"Ls{b2}"]], writes=[r[f"bRC{b2}"]])
                if not first:
                    S.op("pe", lambda e: e.matmul(out=bRC[b2][:], lhsT=onesb[:], rhs=CSs[pc][:], start=False, stop=True),
                         reads=[r[f"CSs{pc}"]], writes=[r[f"bRC{b2}"]])
                S.op("act", lambda e: e.activation(out=Aes[b2][:], in_=bRC[b2][:], func=AF.Exp, scale=-1.0), reads=[r[f"bRC{b2}"]], writes=[r[f"Aes{b2}"]])
                S.op("dve", lambda e: e.tensor_tensor(out=As[b2][:], in0=Es[b2][:], in1=Aes[b2][:], op=ALU.mult),
                     reads=[r[f"Es{b2}"], r[f"Aes{b2}"]], writes=[r[f"As{b2}"]])
                if not last:
                    if first:
                        S.op("dve", lambda e: e.tensor_copy(out=CSs[b2][:], in_=Ls[b2][:]), reads=[r[f"Ls{b2}"]], writes=[r[f"CSs{b2}"]])
                    else:
                        S.op("dve", lambda e: e.tensor_tensor(out=CSs[b2][:], in0=CSs[pc][:], in1=Ls[b2][:], op=ALU.add),
                             reads=[r[f"Ls{b2}"], r[f"CSs{pc}"]], writes=[r[f"CSs{b2}"]])

            def stage3(k):
                B_ = blocks[k]
                b2, hs, j, gq, h, oi, first, last = k % 2, B_["hs"], B_["j"], B_["gq"], B_["h"], B_["oi"], B_["first"], B_["last"]
                S.op("pe", lambda e: e.matmul(out=bOT[oi][0:64, :], lhsT=Vh[hs][:, j, :], rhs=As[b2][:], start=first, stop=last),
                     reads=[r[f"Vh{hs}"], r[f"As{b2}"]], writes=[r[f"bOT{oi}"]])
                if last:
                    S.op("dve", lambda e: e.tensor_copy(out=ost[oi][:], in_=bOT[oi][0:64, :]), reads=[r[f"bOT{oi}"]], writes=[r[f"ost{oi}"]])
                    S.dma("sp", lambda e: e.dma_start(out=mixTs_d[4 * gq:4 * gq + 4, :, h, :].rearrange("j d t -> d j t"),
                                                     in_=ost[oi][:].rearrange("d (j t) -> d j t", j=4)),
                          reads=[r[f"ost{oi}"]], writes=[r["mixTsd"]], sem_res=r[f"ost{oi}"])

            load_head(0)
            for step in range(NB + 2):
                for h in range(7):
                    if step == head_start[h] + 3:
                        load_head(h + 1)
                if step < NB:
                    stage1(step)
                if 0 <= step - 1 < NB:
                    stage2(step - 1)
                if step < NB:
                    stage1b(step)
                if 0 <= step - 2 < NB:
                    stage3(step - 2)
            S.barrier()
            S.release(list(r.values()))

        def phase_C():
          NPASS = 4 if NTO >= 8 else 2
          NTH = NTO // NPASS
          TG = min(4, NTH)
          with ExitStack() as ec:
            gv = sbt(ec, "gv", [128, 4, D], F32)
            brt = sbt(ec, "brt", [128, 36], F32)
            wrt = sbt(ec, "wrt", [128, 8, 36], F32)
            acc = sbt(ec, "acc", [128, NTH, D], F32)
            cTb = sbt(ec, "cTb", [128, 8, NTH * 128], BF16)
            wfull = sbt(ec, "wfull", [128, NTH, 32], F32)
            xts = [sbt(ec, f"xc{i}", [128, D], F32) for i in range(2)]
            junk = sbt(ec, "junkc", [128, D], BF16)
            ssq = sbt(ec, "ssqc", [128, 2], F32)
            r0 = {k: R(k) for k in ["gv", "brt", "wrt", "acc", "cTb", "wfull", "xc0", "xc1", "junk", "ssq", "outd"]}
            S.dma("sp", lambda e: e.dma_start(out=gv[:], in_=gvec[1:5, :].partition_broadcast(128)), writes=[r0["gv"]])
            S.dma("sp", lambda e: e.dma_start(out=brt[:], in_=br.partition_broadcast(128)), writes=[r0["brt"]])
            S.dma("sp", lambda e: e.dma_start(out=wrt[:], in_=wr.rearrange("(c p) n -> p c n", p=128)), writes=[r0["wrt"]])

            for ps_i in range(NPASS):
                t0 = ps_i * NTH
                with ExitStack() as e1:
                    woutm = sbt(e1, "woutm", [128, 4, D], BF16)
                    wouts = sbt(e1, "wouts", [64, 8, D], BF16)
                    mTm = [sbt(e1, f"mTm{i}", [128, 4, 128], BF16) for i in range(2)]
                    mTs = [sbt(e1, f"mTs{i}", [64, 8, 128], BF16) for i in range(2)]
                    c32 = sbt(e1, "c32", [128, D], F32)
                    cT32 = sbt(e1, "cT32", [128, 8, 128], F32)
                    lg = sbt(e1, "lg", [128, 36], F32)
                    gmax = sbt(e1, "gmax", [128, 8], F32)
                    ohg = sbt(e1, "ohg", [128, 4], F32)
                    eg = sbt(e1, "eg", [128, 4], F32)
                    esel = sbt(e1, "esel", [128, 4, 8], F32)
                    es8 = sbt(e1, "es8", [128, 8], F32)
                    mk1 = sbt(e1, "mk1", [128, 8], F32)
                    mk2 = sbt(e1, "mk2", [128, 8], F32)
                    e2 = sbt(e1, "e2", [128, 8], F32)
                    wsel = sbt(e1, "wsel", [128, 8], F32)
                    bH = [pst(e1, f"bH{i}", [128, 512], F32) for i in range(2)]
                    bTf = [pst(e1, f"bTf{i}", [128, 512], F32) for i in range(2)]
                    bL = pst(e1, "bL", [128, 512], F32)
                    r = {k: R(k) for k in ["woutm", "wouts", "mTm0", "mTm1", "mTs0", "mTs1", "c32", "cT32", "lg", "rt", "bH0", "bH1", "bTf0", "bTf1", "bL"]}
                    S.dma("pool", lambda e: e.dma_start(out=woutm[:], in_=wout[0:512, :].rearrange("(h p) n -> p h n", p=128)), writes=[r["woutm"]])
                    S.dma("pool", lambda e: e.dma_start(out=wouts[:], in_=wout[512:1024, :].rearrange("(h p) n -> p h n", p=64)), writes=[r["wouts"]])
                    for tl in range(NTH):
                        t = t0 + tl
                        xi = tl % 2
                        xt = xts[xi]
                        rx = r0[f"xc{xi}"]
                        S.dma("sp", lambda e, xt=xt, t=t: e.dma_start(out=xt[:], in_=xo[t * 128:(t + 1) * 128, :]), writes=[rx])
                        S.dma("sp", lambda e, xi=xi, t=t: e.dma_start(out=mTm[xi][:], in_=mixTm_d[t]), writes=[r[f"mTm{xi}"]])
                        S.dma("sp", lambda e, xi=xi, t=t: e.dma_start(out=mTs[xi][:], in_=mixTs_d[t]), writes=[r[f"mTs{xi}"]])
                        for dh in range(2):
                            for h in range(4):
                                S.op("pe", lambda e, h=h, dh=dh, xi=xi: e.matmul(out=bH[dh][:], lhsT=mTm[xi][:, h, :], rhs=woutm[:, h, dh * 512:(dh + 1) * 512], start=(h == 0), stop=False),
                                     reads=[r[f"mTm{xi}"], r["woutm"]], writes=[r[f"bH{dh}"]])
                            for h in range(8):
                                S.op("pe", lambda e, h=h, dh=dh, xi=xi: e.matmul(out=bH[dh][:], lhsT=mTs[xi][:, h, :], rhs=wouts[:, h, dh * 512:(dh + 1) * 512], start=False, stop=(h == 7)),
                                     reads=[r[f"mTs{xi}"], r["wouts"]], writes=[r[f"bH{dh}"]])
                            S.op("dve", lambda e, dh=dh, tl=tl, xt=xt: e.tensor_tensor(out=acc[:, tl, dh * 512:(dh + 1) * 512], in0=bH[dh][:], in1=xt[:, dh * 512:(dh + 1) * 512], op=ALU.add),
                                 reads=[r[f"bH{dh}"], rx], writes=[r0["acc"]])
                        rmsnorm_stats(acc[:, tl, :], junk[:], ssq[:, 0:1], ssq[:, 1:2], r0["acc"], r0["junk"], r0["ssq"], D)
                        S.op("dve", lambda e, tl=tl: e.scalar_tensor_tensor(out=c32[:], in0=acc[:, tl, :], scalar=ssq[:, 1:2], in1=gv[:, 0, :], op0=ALU.mult, op1=ALU.mult),
                             reads=[r0["acc"], r0["ssq"], r0["gv"]], writes=[r["c32"]])
                        for c in range(8):
                            S.op("pe", lambda e, c=c: e.transpose(out=bTf[c // 4][:, (c % 4) * 128:(c % 4 + 1) * 128], in_=c32[:, c * 128:(c + 1) * 128], identity=identf[:]),
                                 reads=[r["c32"]], writes=[r[f"bTf{c // 4}"]])
                        for hh in range(2):
                            S.op("act", lambda e, hh=hh: e.copy(out=cT32[:, hh * 4:(hh + 1) * 4, :], in_=bTf[hh][:].rearrange("p (c t) -> p c t", c=4)),
                                 reads=[r[f"bTf{hh}"]], writes=[r["cT32"]])
                            S.op("dve", lambda e, hh=hh, tl=tl: e.tensor_copy(out=cTb[:, hh * 4:(hh + 1) * 4, tl * 128:(tl + 1) * 128], in_=cT32[:, hh * 4:(hh + 1) * 4, :]),
                                 reads=[r["cT32"]], writes=[r0["cTb"]])
                        for c in range(8):
                            S.op("pe", lambda e, c=c: e.matmul(out=bL[:, 0:36], lhsT=cT32[:, c, :], rhs=wrt[:, c, :], start=(c == 0), stop=(c == 7)),
                                 reads=[r["cT32"], r0["wrt"]], writes=[r["bL"]])
                        rt = r["rt"]
                        S.op("dve", lambda e: e.tensor_tensor(out=lg[:], in0=bL[:, 0:36], in1=brt[:], op=ALU.add), reads=[r["bL"], r0["brt"]], writes=[rt])
                        S.op("dve", lambda e: e.tensor_reduce(out=gmax[:, 0:1], in_=lg[:, 0:4], axis=AX.X, op=ALU.max), reads=[rt], writes=[rt])
                        S.op("dve", lambda e: e.tensor_scalar(out=ohg[:], in0=lg[:, 0:4], scalar1=gmax[:, 0:1], scalar2=None, op0=ALU.is_equal), reads=[rt], writes=[rt])
                        S.op("dve", lambda e: e.tensor_scalar(out=eg[:], in0=lg[:, 0:4], scalar1=gmax[:, 0:1], scalar2=None, op0=ALU.subtract), reads=[rt], writes=[rt])
                        S.op("pool", lambda e: e.memset(gmax[:, 1:2], 0.0), reads=[rt], writes=[rt])
                        S.op("act", lambda e: e.activation(out=eg[:], in_=eg[:], func=AF.Exp, accum_out=gmax[:, 1:2]), reads=[rt], writes=[rt])
                        S.op("dve", lambda e: e.reciprocal(out=gmax[:, 2:3], in_=gmax[:, 1:2]), reads=[rt], writes=[rt])
                        S.op("dve", lambda e: e.tensor_tensor(out=esel[:], in0=lg[:, 4:36].rearrange("p (g j) -> p g j", g=4), in1=ohg[:].unsqueeze(2).to_broadcast([128, 4, 8]), op=ALU.mult),
                             reads=[rt], writes=[rt])
                        S.op("dve", lambda e: e.tensor_reduce(out=es8[:], in_=esel[:].rearrange("p g j -> p j g"), axis=AX.X, op=ALU.add), reads=[rt], writes=[rt])
                        S.op("dve", lambda e: e.tensor_reduce(out=gmax[:, 3:4], in_=es8[:], axis=AX.X, op=ALU.max), reads=[rt], writes=[rt])
                        S.op("dve", lambda e: e.tensor_scalar(out=mk1[:], in0=es8[:], scalar1=gmax[:, 3:4], scalar2=None, op0=ALU.is_equal), reads=[rt], writes=[rt])
                        S.op("dve", lambda e: e.scalar_tensor_tensor(out=e2[:], in0=mk1[:], scalar=-1e30, in1=es8[:], op0=ALU.mult, op1=ALU.add), reads=[rt], writes=[rt])
                        S.op("dve", lambda e: e.tensor_reduce(out=gmax[:, 4:5], in_=e2[:], axis=AX.X, op=ALU.max), reads=[rt], writes=[rt])
                        S.op("dve", lambda e: e.tensor_scalar(out=mk2[:], in0=e2[:], scalar1=gmax[:, 4:5], scalar2=None, op0=ALU.is_equal), reads=[rt], writes=[rt])
                        S.op("dve", lambda e: e.tensor_tensor(out=gmax[:, 5:6], in0=gmax[:, 4:5], in1=gmax[:, 3:4], op=ALU.subtract), reads=[rt], writes=[rt])
                        S.op("act", lambda e: e.activation(out=gmax[:, 5:6], in_=gmax[:, 5:6], func=AF.Exp), reads=[rt], writes=[rt])
                        S.op("dve", lambda e: e.tensor_scalar(out=gmax[:, 6:7], in0=gmax[:, 5:6], scalar1=1.0, scalar2=None, op0=ALU.add), reads=[rt], writes=[rt])
                        S.op("dve", lambda e: e.reciprocal(out=gmax[:, 6:7], in_=gmax[:, 6:7]), reads=[rt], writes=[rt])
                        S.op("dve", lambda e: e.tensor_tensor(out=gmax[:, 6:7], in0=gmax[:, 6:7], in1=gmax[:, 2:3], op=ALU.mult), reads=[rt], writes=[rt])
                        S.op("dve", lambda e: e.tensor_tensor(out=gmax[:, 7:8], in0=gmax[:, 6:7], in1=gmax[:, 5:6], op=ALU.mult), reads=[rt], writes=[rt])
                        S.op("dve", lambda e: e.tensor_scalar(out=wsel[:], in0=mk1[:], scalar1=gmax[:, 6:7], scalar2=None, op0=ALU.mult), reads=[rt], writes=[rt])
                        S.op("dve", lambda e: e.scalar_tensor_tensor(out=wsel[:], in0=mk2[:], scalar=gmax[:, 7:8], in1=wsel[:], op0=ALU.mult, op1=ALU.add), reads=[rt], writes=[rt])
                        for g in range(4):
                            S.op("dve", lambda e, g=g, tl=tl: e.tensor_scalar(out=wfull[:, tl, g * 8:(g + 1) * 8], in0=wsel[:], scalar1=ohg[:, g:g + 1], scalar2=None, op0=ALU.mult),
                                 reads=[rt], writes=[r0["wfull"]])
                    S.barrier()
                    S.release(list(r.values()))

                with ExitStack() as e2s:
                    wgt = [sbt(e2s, f"wgt{i}", [128, 8, 512], BF16) for i in range(2)]
                    wut = [sbt(e2s, f"wut{i}", [128, 8, 512], BF16) for i in range(2)]
                    wdt = [sbt(e2s, f"wdt{i}", [128, 4, D], BF16) for i in range(2)]
                    sgs = [sbt(e2s, f"sgs{i}", [128, TG * 128], F32) for i in range(2)]
                    hid = [sbt(e2s, f"hid{i}", [128, 4, TG * 128], BF16) for i in range(2)]
                    bGt = [pst(e2s, f"bGt{i}", [128, 512], F32) for i in range(2)]
                    bUt = [pst(e2s, f"bUt{i}", [128, 512], F32) for i in range(2)]
                    bY = [pst(e2s, f"bY{i}", [128, 512], F32) for i in range(2)]
                    r = {k: R(k) for k in ["wgt0", "wgt1", "wut0", "wut1", "wdt0", "wdt1", "sgs0", "sgs1", "hid0", "hid1", "bGt0", "bGt1", "bUt0", "bUt1", "bY0", "bY1"]}
                    NW = TG * 128
                    cnt = 0
                    ycnt = 0

                    def load_exp(ex):
                        s_ = ex % 2
                        S.dma("pool", lambda e: e.dma_start(out=wgt[s_][:], in_=wg[ex].rearrange("(c p) n -> p c n", p=128)), writes=[r[f"wgt{s_}"]])
                        S.dma("pool", lambda e: e.dma_start(out=wut[s_][:], in_=wu[ex].rearrange("(c p) n -> p c n", p=128)), writes=[r[f"wut{s_}"]])
                        S.dma("pool", lambda e: e.dma_start(out=wdt[s_][:], in_=wd[ex].rearrange("(c p) n -> p c n", p=128)), writes=[r[f"wdt{s_}"]])

                    load_exp(0)
                    for ex in range(NEXP):
                        s_ = ex % 2
                        if ex + 1 < NEXP:
                            load_exp(ex + 1)
                        for tg in range(NTH // TG):
                            hb = (ex * (NTH // TG) + tg) % 2
                            for fc in range(4):
                                b2 = cnt % 2
                                cnt += 1
                                for c in range(8):
                                    S.op("pe", lambda e, c=c, fc=fc, b2=b2, s_=s_, tg=tg: e.matmul(out=bGt[b2][:, 0:NW], lhsT=wgt[s_][:, c, fc * 128:(fc + 1) * 128],
                                                                                                  rhs=cTb[:, c, tg * NW:(tg + 1) * NW], start=(c == 0), stop=(c == 7)),
                                         reads=[r[f"wgt{s_}"], r0["cTb"]], writes=[r[f"bGt{b2}"]])
                                for c in range(8):
                                    S.op("pe", lambda e, c=c, fc=fc, b2=b2, s_=s_, tg=tg: e.matmul(out=bUt[b2][:, 0:NW], lhsT=wut[s_][:, c, fc * 128:(fc + 1) * 128],
                                                                                                  rhs=cTb[:, c, tg * NW:(tg + 1) * NW], start=(c == 0), stop=(c == 7)),
                                         reads=[r[f"wut{s_}"], r0["cTb"]], writes=[r[f"bUt{b2}"]])
                                S.op("act", lambda e, b2=b2: e.activation(out=sgs[b2][:], in_=bGt[b2][:, 0:NW], func=AF.Silu), reads=[r[f"bGt{b2}"]], writes=[r[f"sgs{b2}"]])
                                S.op("dve", lambda e, b2=b2, hb=hb, fc=fc: e.tensor_tensor(out=hid[hb][:, fc, :], in0=sgs[b2][:], in1=bUt[b2][:, 0:NW], op=ALU.mult),
                                     reads=[r[f"sgs{b2}"], r[f"bUt{b2}"]], writes=[r[f"hid{hb}"]])
                            for tt in range(TG):
                                tl = tg * TG + tt
                                for dh in range(2):
                                    yb = ycnt % 2
                                    ycnt += 1
                                    for fc in range(4):
                                        S.op("pe", lambda e, fc=fc, hb=hb, tt=tt, dh=dh, yb=yb, s_=s_: e.matmul(out=bY[yb][:], lhsT=hid[hb][:, fc, tt * 128:(tt + 1) * 128],
                                                                                                               rhs=wdt[s_][:, fc, dh * 512:(dh + 1) * 512], start=(fc == 0), stop=(fc == 3)),
                                             reads=[r[f"hid{hb}"], r[f"wdt{s_}"]], writes=[r[f"bY{yb}"]])
                                    S.op("dve", lambda e, yb=yb, tl=tl, dh=dh, ex=ex: e.scalar_tensor_tensor(out=acc[:, tl, dh * 512:(dh + 1) * 512], in0=bY[yb][:], scalar=wfull[:, tl, ex:ex + 1],
                                                                                                           in1=acc[:, tl, dh * 512:(dh + 1) * 512], op0=ALU.mult, op1=ALU.add),
                                         reads=[r[f"bY{yb}"], r0["wfull"], r0["acc"]], writes=[r0["acc"]])
                    S.barrier()
                    S.release(list(r.values()))

                with ExitStack() as e3:
                    wpgt = sbt(e3, "wpgt", [128, 8, D], BF16)
                    wppt = sbt(e3, "wppt", [128, 2, D], BF16)
                    n_bf = sbt(e3, "n_bf", [128, D], BF16)
                    nT = sbt(e3, "nT", [128, 8, 128], BF16)
                    gate = sbt(e3, "gate", [128, D], F32)
                    pts = [sbt(e3, f"pt{i}", [128, 256], F32) for i in range(2)]
                    p_bf = sbt(e3, "p_bf", [128, 256], BF16)
                    pT = sbt(e3, "pT", [128, 2, 128], BF16)
                    ple = sbt(e3, "ple", [128, D], F32)
                    ss2 = sbt(e3, "ss2", [128, 4], F32)
                    h3 = sbt(e3, "h3", [128, D], F32)
                    ots = [sbt(e3, f"ot{i}", [128, D], F32) for i in range(2)]
                    bT2 = pst(e3, "bT2", [128, 1024], BF16)
                    bGa = [pst(e3, f"bGa{i}", [128, 512], F32) for i in range(2)]
                    bP = [pst(e3, f"bP{i}", [128, 512], F32) for i in range(2)]
                    r = {k: R(k) for k in ["wpgt", "wppt", "n_bf", "nT", "gate", "pt0", "pt1", "p_bf", "pT", "ple", "ss2", "h3", "ot0", "ot1", "bT2", "bGa0", "bGa1", "bP0", "bP1", "junk2"]}
                    S.dma("pool", lambda e: e.dma_start(out=wpgt[:], in_=wpg.rearrange("(c p) n -> p c n", p=128)), writes=[r["wpgt"]])
                    S.dma("pool", lambda e: e.dma_start(out=wppt[:], in_=wpp.rearrange("(c p) n -> p c n", p=128)), writes=[r["wppt"]])
                    for tl in range(NTH):
                        t = t0 + tl
                        pi = tl % 2
                        S.dma("sp", lambda e, pi=pi, t=t: e.dma_start(out=pts[pi][:], in_=po[t * 128:(t + 1) * 128, :]), writes=[r[f"pt{pi}"]])
                        rmsnorm_stats(acc[:, tl, :], junk[:], ssq[:, 0:1], ssq[:, 1:2], r0["acc"], r0["junk"], r0["ssq"], D)
                        S.op("dve", lambda e, tl=tl: e.scalar_tensor_tensor(out=n_bf[:], in0=acc[:, tl, :], scalar=ssq[:, 1:2], in1=gv[:, 1, :], op0=ALU.mult, op1=ALU.mult),
                             reads=[r0["acc"], r0["ssq"], r0["gv"]], writes=[r["n_bf"]])
                        for c in range(8):
                            S.op("pe", lambda e, c=c: e.transpose(out=bT2[:, c * 128:(c + 1) * 128], in_=n_bf[:, c * 128:(c + 1) * 128], identity=identb[:]),
                                 reads=[r["n_bf"]], writes=[r["bT2"]])
                        S.op("act", lambda e: e.copy(out=nT[:], in_=bT2[:].rearrange("p (c t) -> p c t", c=8)), reads=[r["bT2"]], writes=[r["nT"]])
                        for dh in range(2):
                            for c in range(8):
                                S.op("pe", lambda e, c=c, dh=dh: e.matmul(out=bGa[dh][:], lhsT=nT[:, c, :], rhs=wpgt[:, c, dh * 512:(dh + 1) * 512], start=(c == 0), stop=(c == 7)),
                                     reads=[r["nT"], r["wpgt"]], writes=[r[f"bGa{dh}"]])
                            S.op("act", lambda e, dh=dh: e.activation(out=gate[:, dh * 512:(dh + 1) * 512], in_=bGa[dh][:], func=AF.Sigmoid), reads=[r[f"bGa{dh}"]], writes=[r["gate"]])
                        S.op("dve", lambda e, pi=pi: e.tensor_copy(out=p_bf[:], in_=pts[pi][:]), reads=[r[f"pt{pi}"]], writes=[r["p_bf"]])
                        for c in range(2):
                            S.op("pe", lambda e, c=c: e.transpose(out=bT2[:, c * 128:(c + 1) * 128], in_=p_bf[:, c * 128:(c + 1) * 128], identity=identb[:]),
                                 reads=[r["p_bf"]], writes=[r["bT2"]])
                        S.op("act", lambda e: e.copy(out=pT[:], in_=bT2[:, 0:256].rearrange("p (c t) -> p c t", c=2)), reads=[r["bT2"]], writes=[r["pT"]])
                        for dh in range(2):
                            for c in range(2):
                                S.op("pe", lambda e, c=c, dh=dh: e.matmul(out=bP[dh][:], lhsT=pT[:, c, :], rhs=wppt[:, c, dh * 512:(dh + 1) * 512], start=(c == 0), stop=(c == 1)),
                                     reads=[r["pT"], r["wppt"]], writes=[r[f"bP{dh}"]])
                            S.op("act", lambda e, dh=dh: e.copy(out=ple[:, dh * 512:(dh + 1) * 512], in_=bP[dh][:]), reads=[r[f"bP{dh}"]], writes=[r["ple"]])
                        rmsnorm_stats(ple[:], junk[:], ss2[:, 0:1], ss2[:, 1:2], r["ple"], r0["junk"], r["ss2"], D)
                        S.op("dve", lambda e: e.scalar_tensor_tensor(out=ple[:], in0=ple[:], scalar=ss2[:, 1:2], in1=gv[:, 2, :], op0=ALU.mult, op1=ALU.mult),
                             reads=[r["ple"], r["ss2"], r0["gv"]], writes=[r["ple"]])
                        S.op("dve", lambda e: e.tensor_tensor(out=ple[:], in0=ple[:], in1=gate[:], op=ALU.mult), reads=[r["ple"], r["gate"]], writes=[r["ple"]])
                        S.op("dve", lambda e, tl=tl: e.tensor_tensor(out=h3[:], in0=ple[:], in1=acc[:, tl, :], op=ALU.add), reads=[r["ple"], r0["acc"]], writes=[r["h3"]])
                        rmsnorm_stats(h3[:], junk[:], ss2[:, 2:3], ss2[:, 3:4], r["h3"], r0["junk"], r["ss2"], D)
                        S.op("dve", lambda e, pi=pi: e.scalar_tensor_tensor(out=ots[pi][:], in0=h3[:], scalar=ss2[:, 3:4], in1=gv[:, 3, :], op0=ALU.mult, op1=ALU.mult),
                             reads=[r["h3"], r["ss2"], r0["gv"]], writes=[r[f"ot{pi}"]])
                        S.dma("sp", lambda e, pi=pi, t=t: e.dma_start(out=out[t * 128:(t + 1) * 128, :], in_=ots[pi][:]), reads=[r[f"ot{pi}"]], writes=[r0["outd"]], sem_res=r[f"ot{pi}"])
                    S.barrier()
                    S.release(list(r.values()))
        if 'A' in phases:
            phase_A()
        if 'B' in phases:
            phase_B()
        if 'C' in phases:
            phase_C()
        S.barrier()
        build_nc.last_log = S.log
        S.emit(block)
    return nc


def prep_core_inputs(inp, b, half, SP, SO):
    x = inp["x"]
    f = np.float32
    if half == 0:
        xp = np.zeros((SP, D), f)
    else:
        xp = np.ascontiguousarray(x[b, 0:SP])
    xo = np.ascontiguousarray(x[b, half * SP: half * SP + SO]) if half == 1 else np.ascontiguousarray(x[b, 0:SO])
    p = inp["p"][0, b]
    po = np.ascontiguousarray(p[half * SP: half * SP + SO]) if half == 1 else np.ascontiguousarray(p[0:SO])
    cq = np.ascontiguousarray(inp["conv_q"][0].T.reshape(4, 128, 4).transpose(1, 0, 2).reshape(128, 16))
    ck = np.ascontiguousarray(inp["conv_k"][0].T.reshape(4, 128, 4).transpose(1, 0, 2).reshape(128, 16))
    gvec = np.stack([inp["g_mix"][0], inp["g_ffn"][0], inp["g_ple"][0], inp["g_ple_post"][0], inp["g_final"]]).astype(f)
    wr = np.concatenate([inp["w_router_group"][0], inp["w_router_expert"][0]], axis=1).astype(f)
    br = np.concatenate([inp["b_router_group"][0], inp["b_router_expert"][0]])[None, :].astype(f)
    return {
        "xp": xp, "xo": xo, "po": po,
        "win": np.ascontiguousarray(inp["w_in"][0]),
        "gvec": np.ascontiguousarray(gvec),
        "gmh": np.ascontiguousarray(inp["g_mhead"]),
        "bg": np.ascontiguousarray(inp["b_gates"]),
        "cq": cq, "ck": ck,
        "wout": np.ascontiguousarray(inp["w_out"][0]),
        "wr": np.ascontiguousarray(wr), "br": np.ascontiguousarray(br),
        "wg": np.ascontiguousarray(inp["w_exp_gate"][0]),
        "wu": np.ascontiguousarray(inp["w_exp_up"][0]),
        "wd": np.ascontiguousarray(inp["w_exp_down"][0]),
        "wpg": np.ascontiguousarray(inp["w_ple_gate"][0]),
        "wpp": np.ascontiguousarray(inp["w_ple_proj"][0]),
    }


def kernel(**inputs):
    inp = {k: np.asarray(v) for k, v in inputs.items()}
    x = inp["x"]
    B, SEQ, _ = x.shape
    SH = SEQ // 2
    import os
    nc = build_nc(SH, SH, phases=os.environ.get("KPHASES", "ABC"))
    in_maps = []
    for c in range(8):
        b, half = c // 2, c % 2
        in_maps.append(prep_core_inputs(inp, b, half, SH, SH))
    res = run_bass_kernel_spmd(nc, in_maps, core_ids=list(range(8)))
    out = np.empty((B, SEQ, D), np.float32)
    for c in range(8):
        b, half = c // 2, c % 2
        out[b, half * SH:(half + 1) * SH] = res.results[c]["out"]
    return out
```

```python
import math
from contextlib import ExitStack
import numpy as np
import concourse.bass as bass
import concourse.mybir as mybir
from concourse.bass_utils import run_bass_kernel_spmd

F32 = mybir.dt.float32
BF16 = mybir.dt.bfloat16
AF = mybir.ActivationFunctionType
ALU = mybir.AluOpType
AX = mybir.AxisListType

D = 1024
NCOL = 3592
EPS = 1e-6
LNC = math.log(128.0 ** -0.5)
NEXP = 32


class Res:
    __slots__ = ("name", "last_w", "readers", "dsem")

    def __init__(self, name):
        self.name = name
        self.last_w = None
        self.readers = []
        self.dsem = None


class Sched:
    ENGS = ("pe", "act", "dve", "pool", "sp")

    def __init__(self, nc):
        self.nc = nc
        self.ops = {e: [] for e in self.ENGS}
        self.cnt = {e: 0 for e in self.ENGS}
        self.sem = {}
        self.waited = {e: {} for e in self.ENGS}
        self.dcnt = {}
        self.dsem_objs = {}
        self.free_dsems = []
        import os
        self.limit = int(os.environ.get("KSTOP", "0")) or None
        self.total = 0
        self.log = []

    def set_sems(self, sems, dma_sems):
        for e, s in zip(self.ENGS, sems):
            self.sem[e] = s
        self.free_dsems = list(dma_sems)

    def res(self, name):
        return Res(name)

    def _deps(self, eng, reads, writes, self_sync):
        deps = {}

        def add(ev):
            if ev is None:
                return
            s, v, src = ev
            if src == eng and not self_sync:
                return
            k = id(s)
            if k not in deps or deps[k][1] < v:
                deps[k] = (s, v)
        for r in reads:
            add(r.last_w)
        for w in writes:
            add(w.last_w)
            for e in w.readers:
                add(e)
        waits = []
        wd = self.waited[eng]
        for k, (s, v) in deps.items():
            if wd.get(k, 0) < v:
                wd[k] = v
                waits.append((s, v))
        return waits

    def _commit(self, ev, reads, writes):
        for r in reads:
            r.readers.append(ev)
            if len(r.readers) > 64:
                r.readers = r.readers[-48:] if False else r.readers
        for w in writes:
            w.last_w = ev
            w.readers = []

    def op(self, eng, fn, reads=(), writes=(), self_sync=None):
        self.total += 1
        if self.limit and self.total > self.limit:
            return None
        import sys as _sys
        self.log.append((self.total, eng, _sys._getframe(1).f_lineno))
        if self_sync is None:
            self_sync = eng != "pe"
        waits = self._deps(eng, reads, writes, self_sync)
        self.cnt[eng] += 1
        ev = (self.sem[eng], self.cnt[eng], eng)
        self.ops[eng].append((waits, fn, (self.sem[eng], 1)))
        self._commit(ev, reads, writes)
        return ev

    def dma(self, eng, fn, reads=(), writes=(), sem_res=None):
        self.total += 1
        if self.limit and self.total > self.limit:
            return None
        import sys as _sys
        self.log.append((self.total, "dma-" + eng, _sys._getframe(1).f_lineno))
        owner = sem_res or (writes[0] if writes else reads[0])
        if owner.dsem is None:
            owner.dsem = self.free_dsems.pop()
            self.dcnt.setdefault(id(owner.dsem), 0)
            self.dsem_objs[id(owner.dsem)] = owner.dsem
        s = owner.dsem
        waits = self._deps(eng, reads, writes, True)
        self.dcnt[id(s)] += 16
        ev = (s, self.dcnt[id(s)], "dma")
        self.ops[eng].append((waits, fn, (s, 16)))
        self._commit(ev, reads, writes)
        return ev

    def release(self, res_list):
        for r in res_list:
            if r.dsem is not None:
                self.free_dsems.append(r.dsem)
                r.dsem = None

    def barrier(self):
        for eng in self.ENGS:
            waits = []
            wd = self.waited[eng]
            for x in self.ENGS:
                s, v = self.sem[x], self.cnt[x]
                if v > 0 and wd.get(id(s), 0) < v:
                    wd[id(s)] = v
                    waits.append((s, v))
            for k, v in self.dcnt.items():
                if v > 0 and wd.get(k, 0) < v:
                    wd[k] = v
                    waits.append((self.dsem_objs[k], v))
            if waits:
                self.ops[eng].append((waits, None, None))

    def emit(self, block):
        sched = self

        def run(engname, handle):
            for waits, fn, inc in sched.ops[engname]:
                for (s, v) in waits:
                    handle.wait_ge(s, v)
                if fn is not None:
                    ins = fn(handle)
                    ins.then_inc(inc[0], inc[1])

        @block.tensor
        def _(e):
            run("pe", e)

        @block.scalar
        def _(e):
            run("act", e)

        @block.vector
        def _(e):
            run("dve", e)

        @block.gpsimd
        def _(e):
            run("pool", e)

        @block.sync
        def _(e):
            run("sp", e)


def build_nc(SP, SO, phases="ABC", dbg=False):
    assert SP % 512 == 0 and SO % 512 == 0
    ST = SP + SO
    NGP, NGO = SP // 512, SO // 512
    NG = NGP + NGO
    NT = ST // 128
    NTO = SO // 128
    nc = bass.Bass("TRN2", target_bir_lowering=False)

    def din(name, shape, dt=F32):
        return nc.dram_tensor(name, list(shape), dt, kind="ExternalInput").ap()

    xp = din("xp", [SP, D])
    xo = din("xo", [SO, D])
    po = din("po", [SO, 256])
    win = din("win", [D, NCOL])
    gvec = din("gvec", [5, D])
    gmh = din("gmh", [1, 512])
    bg = din("bg", [1, 8])
    cq = din("cq", [128, 16])
    ck = din("ck", [128, 16])
    wout = din("wout", [D, D])
    wr = din("wr", [D, 36])
    br = din("br", [1, 36])
    wg = din("wg", [NEXP, D, 512])
    wu = din("wu", [NEXP, D, 512])
    wd = din("wd", [NEXP, 512, D])
    wpg = din("wpg", [D, D])
    wpp = din("wpp", [256, D])
    out = nc.dram_tensor("out", [SO, D], F32, kind="ExternalOutput").ap()

    skind = "ExternalOutput" if dbg else "Internal"
    dbgA = nc.dram_tensor("dbgA", [64, 2048], F32, kind=skind).ap()
    KT_d = nc.dram_tensor("KT_d", [8, 64, ST], BF16, kind=skind).ap()
    QT_d = nc.dram_tensor("QT_d", [8, 64, SO], BF16, kind=skind).ap()
    V_d = nc.dram_tensor("V_d", [8, 128, NT, 64], BF16, kind=skind).ap()
    mixTm_d = nc.dram_tensor("mixTm_d", [NTO, 128, 4, 128], BF16, kind=skind).ap()
    mixTs_d = nc.dram_tensor("mixTs_d", [NTO, 64, 8, 128], BF16, kind=skind).ap()

    es = ExitStack()
    with es:
        sems = [es.enter_context(nc.semaphore(f"s_{e}")) for e in Sched.ENGS]
        dsems = [es.enter_context(nc.semaphore(f"d_{i}")) for i in range(48)]
        block = es.enter_context(nc.Block())
        S = Sched(nc)
        S.set_sems(sems, dsems)
        R = S.res

        uid = [0]

        def sbt(stack, name, shape, dt):
            uid[0] += 1
            return stack.enter_context(nc.sbuf_tensor(f"{name}_{uid[0]}", list(shape), dt))

        def pst(stack, name, shape, dt):
            uid[0] += 1
            return stack.enter_context(nc.psum_tensor(f"{name}_{uid[0]}", list(shape), dt))

        identf = sbt(es, "identf", [128, 128], F32)
        identb = sbt(es, "identb", [128, 128], BF16)
        onesb = sbt(es, "onesb", [128, 128], BF16)
        trilb = sbt(es, "trilb", [128, 128], BF16)
        tmpf = sbt(es, "tmpf", [128, 128], F32)
        triu64 = sbt(es, "triu64", [64, 64], F32)
        ones64 = sbt(es, "ones64", [64, 128], F32)
        negm = sbt(es, "negm", [64, 64], F32)
        r_const = R("const")

        S.op("pool", lambda e: e.memset(identf[:], 0.0), writes=[r_const])
        S.op("pool", lambda e: e.affine_select(out=identf[:], in_=identf[:], pattern=[[-1, 128]], compare_op=ALU.not_equal,
                                               fill=1.0, base=0, channel_multiplier=1), reads=[r_const], writes=[r_const])
        S.op("dve", lambda e: e.tensor_copy(out=identb[:], in_=identf[:]), reads=[r_const], writes=[r_const])
        S.op("pool", lambda e: e.memset(onesb[:], 1.0), writes=[r_const])
        S.op("pool", lambda e: e.memset(tmpf[:], 1.0), writes=[r_const])
        S.op("pool", lambda e: e.affine_select(out=tmpf[:], in_=tmpf[:], pattern=[[-1, 128]], compare_op=ALU.is_ge,
                                               fill=0.0, base=0, channel_multiplier=1), reads=[r_const], writes=[r_const])
        S.op("dve", lambda e: e.tensor_copy(out=trilb[:], in_=tmpf[:]), reads=[r_const], writes=[r_const])
        S.op("pool", lambda e: e.memset(triu64[:], 1.0), writes=[r_const])
        S.op("pool", lambda e: e.affine_select(out=triu64[:], in_=triu64[:], pattern=[[1, 64]], compare_op=ALU.is_ge,
                                               fill=0.0, base=0, channel_multiplier=-1), reads=[r_const], writes=[r_const])
        S.op("pool", lambda e: e.memset(ones64[:], 1.0), writes=[r_const])
        S.op("pool", lambda e: e.memset(negm[:], 0.0), writes=[r_const])
        S.op("pool", lambda e: e.affine_select(out=negm[:], in_=negm[:], pattern=[[1, 64]], compare_op=ALU.is_ge,
                                               fill=30000.0, base=0, channel_multiplier=-1), reads=[r_const], writes=[r_const])
        triu64b = sbt(es, "triu64b", [64, 64], BF16)
        ones64b = sbt(es, "ones64b", [64, 128], BF16)
        negmb = sbt(es, "negmb", [64, 64], BF16)
        S.op("dve", lambda e: e.tensor_copy(out=triu64b[:], in_=triu64[:]), reads=[r_const], writes=[r_const])
        S.op("dve", lambda e: e.tensor_copy(out=ones64b[:], in_=ones64[:]), reads=[r_const], writes=[r_const])
        S.op("dve", lambda e: e.tensor_copy(out=negmb[:], in_=negm[:]), reads=[r_const], writes=[r_const])
        S.barrier()

        def rmsnorm_stats(src_ap, junk, ss, rstd, r_src, r_junk, r_ss, n):
            S.op("pool", lambda e: e.memset(ss, 0.0), writes=[r_ss])
            S.op("act", lambda e: e.activation(out=junk, in_=src_ap, func=AF.Square, accum_out=ss), reads=[r_src, r_ss], writes=[r_junk, r_ss])
            S.op("dve", lambda e: e.tensor_scalar(out=rstd, in0=ss, scalar1=1.0 / n, scalar2=EPS, op0=ALU.mult, op1=ALU.add),
                 reads=[r_ss], writes=[r_ss])
            S.op("act", lambda e: e.sqrt(out=rstd, in_=rstd), reads=[r_ss], writes=[r_ss])
            S.op("dve", lambda e: e.reciprocal(out=rstd, in_=rstd), reads=[r_ss], writes=[r_ss])

        def phase_A():
          with ExitStack() as ea:
            winb = sbt(ea, "winb", [128, 8, NCOL], BF16)
            gmix = sbt(ea, "gmix", [128, D], F32)
            gmht = sbt(ea, "gmht", [64, 512], F32)
            bgt = sbt(ea, "bgt", [64, 8], F32)
            cqt = sbt(ea, "cqt", [128, 16], F32)
            ckt = sbt(ea, "ckt", [128, 16], F32)
            xts = [sbt(ea, f"xt{i}", [128, D], F32) for i in range(2)]
            junk = sbt(ea, "junk", [128, D], BF16)
            ssq = sbt(ea, "ssq", [128, 2], F32)
            a_bf = sbt(ea, "a_bf", [128, D], BF16)
            aT = sbt(ea, "aT", [128, 8, 512], BF16)
            qpre = sbt(ea, "qpre", [128, 4, 515], F32)
            kpre = sbt(ea, "kpre", [128, 4, 515], F32)
            ctmp = [sbt(ea, f"ctmp{i}", [128, 512], F32) for i in range(2)]
            qTm = sbt(ea, "qTm", [128, 4, 512], BF16)
            kTm = sbt(ea, "kTm", [128, 4, 512], BF16)
            sqT = sbt(ea, "sqT", [128, 4, 512], BF16)
            skT = sbt(ea, "skT", [128, 4, 512], BF16)
            svt = sbt(ea, "svt", [128, 8, 4, 64], BF16)
            ktok = sbt(ea, "ktok", [64, 4, 128], BF16)
            vext = sbt(ea, "vext", [64, 512], BF16)
            onecol = sbt(ea, "onecol", [64, 2], BF16)
            gvext = sbt(ea, "gvext", [64, 512], BF16)
            gamb = sbt(ea, "gamb", [64, 4], BF16)
            gsb = sbt(ea, "gsb", [64, 8], F32)
            lfs = sbt(ea, "lfs", [64, 4], F32)
            lfb = sbt(ea, "lfb", [64, 8, 64], BF16)
            lf2 = sbt(ea, "lf2", [64, 8], BF16)
            lft = sbt(ea, "lft", [64, 4], F32)
            bs = sbt(ea, "bs", [64, 8], F32)
            alpha = sbt(ea, "alpha", [64, 4], F32)
            gamma = sbt(ea, "gamma", [64, 4], F32)
            garg = sbt(ea, "garg", [64, 4], F32)
            dcoef = sbt(ea, "dcoef", [64, 4], F32)
            eB = sbt(ea, "eB", [128, 4], F32)
            DTs = sbt(ea, "DTs", [64, 4, 64], F32)
            WT = sbt(ea, "WT", [64, 4, 64], BF16)
            n2s = sbt(ea, "n2s", [64, 4, 128], F32)
            numsb = sbt(ea, "numsb", [64, 4, 128], F32)
            sqh = sbt(ea, "sqh", [64, 4, 128], F32)
            dsb = sbt(ea, "dsb", [64, 8], F32)
            den = sbt(ea, "den", [64, 4], F32)
            ssh = sbt(ea, "ssh", [64, 4], F32)
            sig = sbt(ea, "sig", [64, 512], F32)
            mixtok = sbt(ea, "mixtok", [64, 4, 128], BF16)
            mixTg = sbt(ea, "mixTg", [128, 4, 512], BF16)
            Cst = sbt(ea, "Cst", [128, 4, 128], F32)
            nst = sbt(ea, "nst", [128, 4], F32)
            Cbf = sbt(ea, "Cbf", [128, 512], BF16)
            nbf = sbt(ea, "nbf", [128, 4], BF16)

            bF = pst(ea, "bF", [128, 512], F32)
            bSm = pst(ea, "bSm", [128, 512], F32)
            bV = pst(ea, "bV", [128, 512], F32)
            bO = pst(ea, "bO", [128, 512], F32)
            bAB = pst(ea, "bAB", [128, 512], F32)
            bN = pst(ea, "bN", [128, 512], F32)
            bN2 = pst(ea, "bN2", [128, 512], F32)
            bT = pst(ea, "bT", [128, 1024], BF16)

            names = ["winb", "gmix", "small", "xt0", "xt1", "junk", "ssq", "a_bf", "aT", "qpre", "kpre", "ctmp0", "ctmp1", "qTm", "kTm",
                     "sqT", "skT", "svt", "ktok", "vext", "gvext", "gsb", "lfs", "lfb", "bs", "alpha", "gamma", "garg", "dcoef", "eB",
                     "DTs", "WT", "n2s", "numsb", "sqh", "dsb", "den", "ssh", "sig", "mixtok", "mixTg", "Cst", "nst", "Cbf",
                     "bF", "bG", "bS", "bE", "bD", "bDn", "bV", "bO", "bA", "bB", "bN", "bN2", "bT",
                     "KTd", "QTd", "Vd", "mixTmd"]
            r = {k: R(k) for k in names}

            for c in range(8):
                S.dma("pool", lambda e, c=c: e.dma_start(out=winb[:, c, :], in_=win[c * 128:(c + 1) * 128, :]), writes=[r["winb"]])
            S.dma("sp", lambda e: e.dma_start(out=gmix[:], in_=gvec[0:1, :].partition_broadcast(128)), writes=[r["gmix"]])
            S.dma("sp", lambda e: e.dma_start(out=gmht[:], in_=gmh.partition_broadcast(64)), writes=[r["small"]])
            S.dma("sp", lambda e: e.dma_start(out=bgt[:], in_=bg.partition_broadcast(64)), writes=[r["small"]])
            S.dma("sp", lambda e: e.dma_start(out=cqt[:], in_=cq), writes=[r["small"]])
            S.dma("sp", lambda e: e.dma_start(out=ckt[:], in_=ck), writes=[r["small"]])
            S.op("pool", lambda e: e.memset(Cst[:], 0.0), writes=[r["Cst"]])
            S.op("pool", lambda e: e.memset(nst[:], 0.0), writes=[r["nst"]])
            S.op("pool", lambda e: e.memset(Cbf[:], 0.0), writes=[r["Cbf"]])
            S.op("pool", lambda e: e.memset(onecol[:], 1.0), writes=[r["vext"]])
            S.op("pool", lambda e: e.memset(nbf[:], 0.0), writes=[r["Cbf"]])
            S.op("pool", lambda e: e.memset(qpre[:], 0.0), writes=[r["qpre"]])
            S.op("pool", lambda e: e.memset(kpre[:], 0.0), writes=[r["kpre"]])

            evac_i = [0]

            def evac(out_ap, in_ap, reads, writes, scale=None):
                evac_i[0] += 1
                if scale is not None or evac_i[0] % 2 == 0:
                    if scale is None:
                        S.op("act", lambda e: e.copy(out=out_ap, in_=in_ap), reads=reads, writes=writes)
                    else:
                        S.op("act", lambda e: e.mul(out=out_ap, in_=in_ap, mul=scale), reads=reads, writes=writes)
                else:
                    S.op("dve", lambda e: e.tensor_copy(out=out_ap, in_=in_ap), reads=reads, writes=writes)

            for G in range(NG):
                own = G >= NGP
                src = xo if own else xp
                row0 = (G - NGP if own else G) * 512
                for ti in range(4):
                    xi = ti % 2
                    xt = xts[xi]
                    rx = r[f"xt{xi}"]
                    S.dma("sp", lambda e, xt=xt, a=row0 + ti * 128, src=src: e.dma_start(out=xt[:], in_=src[a:a + 128, :]), writes=[rx])
                    rmsnorm_stats(xt[:], junk[:], ssq[:, 0:1], ssq[:, 1:2], rx, r["junk"], r["ssq"], D)
                    S.op("dve", lambda e, xt=xt: e.scalar_tensor_tensor(out=a_bf[:], in0=xt[:], scalar=ssq[:, 1:2], in1=gmix[:], op0=ALU.mult, op1=ALU.mult),
                         reads=[rx, r["ssq"], r["gmix"]], writes=[r["a_bf"]])
                    for c in range(8):
                        S.op("pe", lambda e, c=c: e.transpose(out=bT[:, c * 128:(c + 1) * 128], in_=a_bf[:, c * 128:(c + 1) * 128], identity=identb[:]),
                             reads=[r["a_bf"]], writes=[r["bT"]])
                    evac(aT[:, :, ti * 128:(ti + 1) * 128], bT[:].rearrange("p (c t) -> p c t", c=8), [r["bT"]], [r["aT"]])

                def fproj(col0, dst_ap, r_dst, scale=None):
                    for c in range(8):
                        S.op("pe", lambda e, c=c: e.matmul(out=bF[:], lhsT=winb[:, c, col0:col0 + 128], rhs=aT[:, c, :], start=(c == 0), stop=(c == 7)),
                             reads=[r["winb"], r["aT"]], writes=[r["bF"]])
                    evac(dst_ap, bF[:], [r["bF"]], [r_dst], scale=scale)

                for h in range(4):
                    if own:
                        fproj(h * 128, qpre[:, h, 3:515], r["qpre"])
                    fproj(512 + h * 128, kpre[:, h, 3:515], r["kpre"])
                for j in range(4):
                    if own:
                        fproj(2056 + j * 128, sqT[:, j, :], r["sqT"], scale=0.125)
                    fproj(2568 + j * 128, skT[:, j, :], r["skT"])

                def conv(pre, cwt, dstT, r_pre, r_dst):
                    for h in range(4):
                        tmp = ctmp[h % 2]
                        rt = r[f"ctmp{h % 2}"]
                        S.op("dve", lambda e, h=h, tmp=tmp: e.tensor_scalar(out=tmp[:], in0=pre[:, h, 0:512], scalar1=cwt[:, h * 4:h * 4 + 1], scalar2=None, op0=ALU.mult),
                             reads=[r_pre, r["small"]], writes=[rt])
                        for j in range(1, 4):
                            S.op("dve", lambda e, h=h, j=j, tmp=tmp: e.scalar_tensor_tensor(out=tmp[:], in0=pre[:, h, j:j + 512], scalar=cwt[:, h * 4 + j:h * 4 + j + 1],
                                                                                          in1=tmp[:], op0=ALU.mult, op1=ALU.add),
                                 reads=[r_pre, rt], writes=[rt])
                        S.op("act", lambda e, h=h, tmp=tmp: e.activation(out=dstT[:, h, :], in_=tmp[:], func=AF.Silu), reads=[rt], writes=[r_dst])
                    S.op("pool", lambda e: e.tensor_copy(out=pre[:, :, 0:3], in_=pre[:, :, 512:515]), reads=[r_pre], writes=[r_pre])

                if own:
                    conv(qpre, cqt, qTm, r["qpre"], r["qTm"])
                else:
                    if G == NGP - 1:
                        for h in range(4):
                            for c in range(8):
                                S.op("pe", lambda e, c=c, h=h: e.matmul(out=bF[:], lhsT=winb[:, c, h * 128:h * 128 + 128], rhs=aT[:, c, :], start=(c == 0), stop=(c == 7)),
                                     reads=[r["winb"], r["aT"]], writes=[r["bF"]])
                            evac(qpre[:, h, 0:3], bF[:, 509:512], [r["bF"]], [r["qpre"]])
                conv(kpre, ckt, kTm, r["kpre"], r["kTm"])

                tokg = G * 512
                S.dma("sp", lambda e, tokg=tokg: e.dma_start(out=KT_d.rearrange("(j two) d t -> (two d) j t", two=2)[:, :, tokg:tokg + 512], in_=skT[:]),
                      reads=[r["skT"]], writes=[r["KTd"]], sem_res=r["skT"])
                if own:
                    S.dma("sp", lambda e, a=row0: e.dma_start(out=QT_d.rearrange("(j two) d t -> (two d) j t", two=2)[:, :, a:a + 512], in_=sqT[:]),
                          reads=[r["sqT"]], writes=[r["QTd"]], sem_res=r["sqT"])
                for ti in range(4):
                    for c in range(8):
                        S.op("pe", lambda e, c=c, ti=ti: e.matmul(out=bF[:], lhsT=aT[:, c, ti * 128:(ti + 1) * 128], rhs=winb[:, c, 3080:3592], start=(c == 0), stop=(c == 7)),
                             reads=[r["winb"], r["aT"]], writes=[r["bF"]])
                    evac(svt[:, :, ti, :], bF[:].rearrange("p (h d) -> p h d", h=8), [r["bF"]], [r["svt"]])
                S.dma("sp", lambda e, G=G: e.dma_start(out=V_d.rearrange("h s j d -> s h j d")[:, :, 4 * G:4 * G + 4, :], in_=svt[:]),
                      reads=[r["svt"]], writes=[r["Vd"]], sem_res=r["svt"])

                for ci in range(8):
                    c0 = ci * 64
                    for c in range(8):
                        S.op("pe", lambda e, c=c, c0=c0: e.matmul(out=bV[0:64, :], lhsT=aT[:, c, c0:c0 + 64], rhs=winb[:, c, 1024:1536], start=(c == 0), stop=(c == 7)),
                             reads=[r["winb"], r["aT"]], writes=[r["bV"]])
                    for c in range(8):
                        S.op("pe", lambda e, c=c, c0=c0: e.matmul(out=bSm[0:64, 0:8], lhsT=aT[:, c, c0:c0 + 64], rhs=winb[:, c, 2048:2056], start=(c == 0), stop=(c == 7)),
                             reads=[r["winb"], r["aT"]], writes=[r["bG"]])
                    if own:
                        for c in range(8):
                            S.op("pe", lambda e, c=c, c0=c0: e.matmul(out=bO[0:64, :], lhsT=aT[:, c, c0:c0 + 64], rhs=winb[:, c, 1536:2048], start=(c == 0), stop=(c == 7)),
                                 reads=[r["winb"], r["aT"]], writes=[r["bO"]])
                    for h in range(4):
                        S.op("pe", lambda e, h=h, c0=c0: e.transpose(out=bT[0:64, h * 128:(h + 1) * 128], in_=kTm[:, h, c0:c0 + 64], identity=identb[:]),
                             reads=[r["kTm"]], writes=[r["bT"]])
                    evac(ktok[:], bT[0:64, 0:512].rearrange("p (h d) -> p h d", h=4), [r["bT"]], [r["ktok"]])
                    S.op("dve", lambda e: e.tensor_tensor(out=gsb[:], in0=bSm[0:64, 0:8], in1=bgt[:], op=ALU.add), reads=[r["bG"], r["small"]], writes=[r["gsb"]])
                    S.op("act", lambda e: e.activation(out=lfs[:], in_=gsb[:, 4:8], func=AF.Exp, scale=-1.0), reads=[r["gsb"]], writes=[r["lfs"]])
                    S.op("act", lambda e: e.activation(out=lfs[:], in_=lfs[:], func=AF.Ln, bias=1.0), reads=[r["lfs"]], writes=[r["lfs"]])
                    S.op("dve", lambda e: e.tensor_copy(out=lf2[:, 0:4], in_=lfs[:]), reads=[r["lfs"]], writes=[r["lfb"]])
                    S.op("dve", lambda e: e.tensor_tensor(out=lft[:], in0=lfs[:], in1=lf2[:, 0:4], op=ALU.subtract), reads=[r["lfs"], r["lfb"]], writes=[r["lfb"]])
                    S.op("dve", lambda e: e.tensor_copy(out=lf2[:, 4:8], in_=lft[:]), reads=[r["lfb"]], writes=[r["lfb"]])
                    for q_ in range(2):
                        S.op("pe", lambda e, q_=q_: e.matmul(out=bSm[0:64, 8:12], lhsT=triu64b[:], rhs=lf2[:, q_ * 4:q_ * 4 + 4], start=(q_ == 0), stop=(q_ == 1)), reads=[r["lfb"]], writes=[r["bS"]])
                    for q_ in range(2):
                        S.op("pe", lambda e, q_=q_: e.matmul(out=bSm[0:64, 12:16], lhsT=ones64b[:, 0:64], rhs=lf2[:, q_ * 4:q_ * 4 + 4], start=(q_ == 0), stop=(q_ == 1)), reads=[r["lfb"]], writes=[r["bS"]])
                    for q_ in range(2):
                        S.op("pe", lambda e, q_=q_: e.matmul(out=bSm[:, 16:20], lhsT=ones64b[:], rhs=lf2[:, q_ * 4:q_ * 4 + 4], start=(q_ == 0), stop=(q_ == 1)), reads=[r["lfb"]], writes=[r["bE"]])
                    S.op("act", lambda e: e.copy(out=bs[:], in_=bSm[0:64, 8:16]), reads=[r["bS"]], writes=[r["bs"]])
                    S.op("act", lambda e: e.activation(out=eB[:], in_=bSm[:, 16:20], func=AF.Exp, scale=-1.0), reads=[r["bE"]], writes=[r["eB"]])
                    S.op("dve", lambda e: e.tensor_tensor(out=garg[:], in0=bs[:, 0:4], in1=bs[:, 4:8], op=ALU.subtract), reads=[r["bs"]], writes=[r["garg"]])
                    S.op("dve", lambda e: e.tensor_tensor(out=garg[:], in0=garg[:], in1=gsb[:, 0:4], op=ALU.add), reads=[r["garg"], r["gsb"]], writes=[r["garg"]])
                    S.op("act", lambda e: e.activation(out=gamma[:], in_=garg[:], func=AF.Exp), reads=[r["garg"]], writes=[r["gamma"]])
                    S.op("dve", lambda e: e.tensor_tensor(out=gvext[:].rearrange("p (h d) -> p h d", h=4), in0=bV[0:64, :].rearrange("p (h d) -> p h d", h=4),
                                                          in1=gamma[:].unsqueeze(2).to_broadcast([64, 4, 128]), op=ALU.mult),
                         reads=[r["bV"], r["gamma"]], writes=[r["gvext"]])
                    S.op("dve", lambda e: e.tensor_copy(out=gamb[:], in_=gamma[:]), reads=[r["gamma"]], writes=[r["gvext"]])
                    if own:
                        S.op("dve", lambda e: e.tensor_copy(out=vext[:], in_=bV[0:64, :]), reads=[r["bV"]], writes=[r["vext"]])
                        S.op("dve", lambda e: e.tensor_scalar(out=alpha[:], in0=bs[:, 0:4], scalar1=-1.0, scalar2=LNC, op0=ALU.mult, op1=ALU.add), reads=[r["bs"]], writes=[r["alpha"]])
                        S.op("act", lambda e: e.activation(out=alpha[:], in_=alpha[:], func=AF.Exp), reads=[r["alpha"]], writes=[r["alpha"]])
                        S.op("dve", lambda e: e.scalar_tensor_tensor(out=dcoef[:], in0=bs[:, 0:4], scalar=LNC, in1=gsb[:, 0:4], op0=ALU.add, op1=ALU.add),
                             reads=[r["bs"], r["gsb"]], writes=[r["dcoef"]])
                        S.op("dve", lambda e: e.tensor_copy(out=lfb[:], in_=lf2[:].unsqueeze(2).to_broadcast([64, 8, 64])), reads=[r["lfb"]], writes=[r["lfb"]])
                        for h in range(4):
                            S.op("pe", lambda e, h=h, c0=c0: e.matmul(out=bAB[0:64, h * 64:(h + 1) * 64], lhsT=kTm[:, h, c0:c0 + 64], rhs=qTm[:, h, c0:c0 + 64], start=True, stop=True),
                                 reads=[r["kTm"], r["qTm"]], writes=[r["bA"]])
                            S.op("pe", lambda e, h=h: e.matmul(out=bAB[0:64, 256 + h * 64:256 + (h + 1) * 64], lhsT=lfb[:, h, :], rhs=triu64b[:], start=True, stop=False),
                                 reads=[r["lfb"]], writes=[r["bB"]])
                            S.op("pe", lambda e, h=h: e.matmul(out=bAB[0:64, 256 + h * 64:256 + (h + 1) * 64], lhsT=lfb[:, 4 + h, :], rhs=triu64b[:], start=False, stop=False),
                                 reads=[r["lfb"]], writes=[r["bB"]])
                            S.op("pe", lambda e, h=h: e.matmul(out=bAB[0:64, 256 + h * 64:256 + (h + 1) * 64], lhsT=identb[0:64, 0:64], rhs=negmb[:], start=False, stop=True),
                                 reads=[r["lfb"]], writes=[r["bB"]])
                        for h in range(4):
                            S.op("act", lambda e, h=h: e.activation(out=DTs[:, h, :], in_=bAB[0:64, 256 + h * 64:256 + (h + 1) * 64], func=AF.Exp, scale=-1.0, bias=dcoef[:, h:h + 1]),
                                 reads=[r["bB"], r["dcoef"]], writes=[r["DTs"]])
                        S.op("dve", lambda e: e.tensor_tensor(out=WT[:].rearrange("p h t -> p (h t)"), in0=bAB[0:64, 0:256], in1=DTs[:].rearrange("p h t -> p (h t)"), op=ALU.mult),
                             reads=[r["bA"], r["DTs"]], writes=[r["WT"]])
                        for h in range(4):
                            S.op("pe", lambda e, h=h: e.matmul(out=bN[0:64, h * 128:(h + 1) * 128], lhsT=WT[:, h, :], rhs=vext[:, h * 128:(h + 1) * 128], start=True, stop=True),
                                 reads=[r["WT"], r["vext"]], writes=[r["bN"]])
                            S.op("pe", lambda e, h=h: e.matmul(out=bSm[0:64, 20 + h:21 + h], lhsT=WT[:, h, :], rhs=onecol[:, 0:1], start=True, stop=True),
                                 reads=[r["WT"], r["vext"]], writes=[r["bD"]])
                            S.op("pe", lambda e, h=h, c0=c0: e.matmul(out=bN2[0:64, h * 128:(h + 1) * 128], lhsT=qTm[:, h, c0:c0 + 64], rhs=Cbf[:, h * 128:(h + 1) * 128], start=True, stop=True),
                                 reads=[r["qTm"], r["Cbf"]], writes=[r["bN2"]])
                            S.op("pe", lambda e, h=h, c0=c0: e.matmul(out=bSm[0:64, 24 + h:25 + h], lhsT=qTm[:, h, c0:c0 + 64], rhs=nbf[:, h:h + 1], start=True, stop=True),
                                 reads=[r["qTm"], r["Cbf"]], writes=[r["bD"]])
                        S.op("dve", lambda e: e.tensor_tensor(out=n2s[:], in0=bN2[0:64, :].rearrange("p (h d) -> p h d", h=4), in1=alpha[:].unsqueeze(2).to_broadcast([64, 4, 128]), op=ALU.mult),
                             reads=[r["bN2"], r["alpha"]], writes=[r["n2s"]])
                        S.op("dve", lambda e: e.tensor_tensor(out=numsb[:], in0=bN[0:64, :].rearrange("p (h d) -> p h d", h=4), in1=n2s[:], op=ALU.add),
                             reads=[r["bN"], r["n2s"]], writes=[r["numsb"]])
                        S.op("act", lambda e: e.copy(out=dsb[:], in_=bSm[0:64, 20:28]), reads=[r["bD"]], writes=[r["dsb"]])
                        S.op("dve", lambda e: e.tensor_tensor(out=den[:], in0=dsb[:, 4:8], in1=alpha[:], op=ALU.mult), reads=[r["dsb"], r["alpha"]], writes=[r["den"]])
                        S.op("dve", lambda e: e.tensor_tensor(out=den[:], in0=den[:], in1=dsb[:, 0:4], op=ALU.add), reads=[r["dsb"], r["den"]], writes=[r["den"]])
                        S.op("dve", lambda e: e.tensor_scalar(out=ssh[:], in0=den[:], scalar1=-1.0, scalar2=None, op0=ALU.mult), reads=[r["den"]], writes=[r["ssh"]])
                        S.op("dve", lambda e: e.tensor_tensor(out=den[:], in0=den[:], in1=ssh[:], op=ALU.max), reads=[r["den"], r["ssh"]], writes=[r["den"]])
                        S.op("dve", lambda e: e.tensor_scalar_max(out=den[:], in0=den[:], scalar1=1.0), reads=[r["den"]], writes=[r["den"]])
                        S.op("dve", lambda e: e.reciprocal(out=den[:], in_=den[:]), reads=[r["den"]], writes=[r["den"]])
                        S.op("dve", lambda e: e.tensor_tensor(out=numsb[:], in0=numsb[:], in1=den[:].unsqueeze(2).to_broadcast([64, 4, 128]), op=ALU.mult),
                             reads=[r["numsb"], r["den"]], writes=[r["numsb"]])
                        S.op("dve", lambda e: e.tensor_tensor(out=sqh[:], in0=numsb[:], in1=numsb[:], op=ALU.mult), reads=[r["numsb"]], writes=[r["sqh"]])
                        S.op("dve", lambda e: e.tensor_reduce(out=ssh[:], in_=sqh[:], axis=AX.X, op=ALU.add), reads=[r["sqh"]], writes=[r["ssh"]])
                        S.op("dve", lambda e: e.tensor_scalar(out=ssh[:], in0=ssh[:], scalar1=1.0 / 128, scalar2=EPS, op0=ALU.mult, op1=ALU.add), reads=[r["ssh"]], writes=[r["ssh"]])
                        S.op("act", lambda e: e.sqrt(out=ssh[:], in_=ssh[:]), reads=[r["ssh"]], writes=[r["ssh"]])
                        S.op("dve", lambda e: e.reciprocal(out=ssh[:], in_=ssh[:]), reads=[r["ssh"]], writes=[r["ssh"]])
                        S.op("dve", lambda e: e.tensor_tensor(out=numsb[:], in0=numsb[:], in1=ssh[:].unsqueeze(2).to_broadcast([64, 4, 128]), op=ALU.mult),
                             reads=[r["numsb"], r["ssh"]], writes=[r["numsb"]])
                        S.op("dve", lambda e: e.tensor_tensor(out=numsb[:], in0=numsb[:], in1=gmht[:].rearrange("p (h d) -> p h d", h=4), op=ALU.mult),
                             reads=[r["numsb"], r["small"]], writes=[r["numsb"]])
                        S.op("act", lambda e: e.activation(out=sig[:], in_=bO[0:64, :], func=AF.Sigmoid), reads=[r["bO"]], writes=[r["sig"]])
                        S.op("dve", lambda e: e.tensor_tensor(out=mixtok[:], in0=numsb[:], in1=sig[:].rearrange("p (h d) -> p h d", h=4), op=ALU.mult),
                             reads=[r["numsb"], r["sig"]], writes=[r["mixtok"]])
                        if dbg and G == NGP and ci == 1:
                            rdb = R("dbg")
                            dl = [(gsb, 0, 8), (lfs, 8, 4), (bs, 12, 8), (alpha, 20, 4), (gamma, 24, 4), (dcoef, 28, 4), (den, 32, 4), (dsb, 36, 8), (ssh, 44, 4)]
                            for (tt_, o_, n_) in dl:
                                S.dma("sp", lambda e, tt_=tt_, o_=o_, n_=n_: e.dma_start(out=dbgA[:, o_:o_ + n_], in_=tt_[:]), reads=[r["numsb"], r["ssh"], r["den"]], writes=[rdb])
                            S.dma("sp", lambda e: e.dma_start(out=dbgA[:, 64:320], in_=DTs[:].rearrange("p h t -> p (h t)")), reads=[r["DTs"]], writes=[rdb])
                            S.dma("sp", lambda e: e.dma_start(out=dbgA[:, 512:1024], in_=numsb[:].rearrange("p h t -> p (h t)")), reads=[r["numsb"]], writes=[rdb])
                            S.dma("sp", lambda e: e.dma_start(out=dbgA[:, 1024:1536], in_=n2s[:].rearrange("p h t -> p (h t)")), reads=[r["n2s"]], writes=[rdb])
                            S.dma("sp", lambda e: e.dma_start(out=dbgA[:, 1536:2048], in_=sig[:]), reads=[r["sig"]], writes=[rdb])
                        for h in range(4):
                            S.op("pe", lambda e, h=h: e.transpose(out=bT[:, 512 + h * 64:512 + (h + 1) * 64], in_=mixtok[:, h, :], identity=identb[0:64, 0:64]),
                                 reads=[r["mixtok"]], writes=[r["bT"]])
                        evac(mixTg[:, :, c0:c0 + 64], bT[:, 512:768].rearrange("p (h t) -> p h t", h=4), [r["bT"]], [r["mixTg"]])
                    for h in range(4):
                        S.op("pe", lambda e, h=h: e.matmul(out=bN[:, h * 128:(h + 1) * 128], lhsT=ktok[:, h, :], rhs=gvext[:, h * 128:(h + 1) * 128], start=True, stop=True),
                             reads=[r["ktok"], r["gvext"]], writes=[r["bN"]])
                        S.op("pe", lambda e, h=h: e.matmul(out=bSm[:, 28 + h:29 + h], lhsT=ktok[:, h, :], rhs=gamb[:, h:h + 1], start=True, stop=True),
                             reads=[r["ktok"], r["gvext"]], writes=[r["bDn"]])
                    for h in range(4):
                        S.op("dve", lambda e, h=h: e.scalar_tensor_tensor(out=Cst[:, h, :], in0=Cst[:, h, :], scalar=eB[:, h:h + 1], in1=bN[:, h * 128:(h + 1) * 128], op0=ALU.mult, op1=ALU.add),
                             reads=[r["Cst"], r["eB"], r["bN"]], writes=[r["Cst"]])
                    S.op("dve", lambda e: e.tensor_tensor(out=nst[:], in0=nst[:], in1=eB[:], op=ALU.mult), reads=[r["nst"], r["eB"]], writes=[r["nst"]])
                    S.op("dve", lambda e: e.tensor_tensor(out=nst[:], in0=nst[:], in1=bSm[:, 28:32], op=ALU.add), reads=[r["nst"], r["bDn"]], writes=[r["nst"]])
                    S.op("act", lambda e: e.copy(out=Cbf[:], in_=Cst[:].rearrange("p h d -> p (h d)")), reads=[r["Cst"]], writes=[r["Cbf"]])
                    S.op("act", lambda e: e.copy(out=nbf[:], in_=nst[:]), reads=[r["nst"]], writes=[r["Cbf"]])
                if own:
                    go = G - NGP
                    for j in range(4):
                        S.dma("sp", lambda e, go=go, j=j: e.dma_start(out=mixTm_d[4 * go + j], in_=mixTg[:, :, j * 128:(j + 1) * 128]),
                              reads=[r["mixTg"]], writes=[r["mixTmd"]], sem_res=r["mixTg"])
            S.barrier()
            S.release(list(r.values()))

        def phase_B():
          with ExitStack() as eb:
            KTh = [sbt(eb, f"KTh{i}", [64, ST], BF16) for i in range(2)]
            Vh = [sbt(eb, f"Vh{i}", [128, NT, 64], BF16) for i in range(2)]
            QTh = [sbt(eb, f"QTh{i}", [64, SO], BF16) for i in range(2)]
            maskf = sbt(eb, "maskf", [128, 4, 512], F32)
            Es = [sbt(eb, f"Es{i}", [128, 512], F32) for i in range(2)]
            Ls = [sbt(eb, f"Ls{i}", [128, 512], BF16) for i in range(2)]
            Aes = [sbt(eb, f"Aes{i}", [128, 512], F32) for i in range(2)]
            As = [sbt(eb, f"As{i}", [128, 512], BF16) for i in range(2)]
            CSs = [sbt(eb, f"CSs{i}", [128, 512], BF16) for i in range(2)]
            ost = [sbt(eb, f"ost{i}", [64, 512], BF16) for i in range(2)]
            bZ = [pst(eb, f"bZ{i}", [128, 512], F32) for i in range(2)]
            bRC = [pst(eb, f"bRC{i}", [128, 512], F32) for i in range(2)]
            bOT = [pst(eb, f"bOT{i}", [128, 512], F32) for i in range(2)]
            rn = ["mask", "mixTsd"] + [f"{n}{i}" for n in ["KTh", "Vh", "QTh", "Es", "Ls", "Aes", "As", "CSs", "ost", "bZ", "bRC", "bOT"] for i in range(2)]
            r = {k: R(k) for k in rn}
            for m in range(4):
                S.op("pool", lambda e, m=m: e.memset(maskf[:, m, :], 1.0), writes=[r["mask"]])
                S.op("pool", lambda e, m=m: e.affine_select(out=maskf[:, m, :], in_=maskf[:, m, :], pattern=[[1, 512]], compare_op=ALU.is_ge,
                                                            fill=0.0, base=-(m * 128) - 1, channel_multiplier=-1), reads=[r["mask"]], writes=[r["mask"]])
            blocks = []
            head_start = {}
            for h in range(8):
                head_start[h] = len(blocks)
                for gq in range(NGO):
                    jd0 = (NGP + gq) * 4
                    first = True
                    for j in range(jd0 + 3, -1, -1):
                        blocks.append(dict(h=h, hs=h % 2, gq=gq, j=j, m=j - jd0, first=first, last=(j == 0), oi=(h * NGO + gq) % 2))
                        first = False
            NB = len(blocks)

            def load_head(h):
                hs = h % 2
                S.dma("sp", lambda e: e.dma_start(out=KTh[hs][:], in_=KT_d[h]), writes=[r[f"KTh{hs}"]])
                S.dma("sp", lambda e: e.dma_start(out=Vh[hs][:], in_=V_d[h]), writes=[r[f"Vh{hs}"]])
                S.dma("sp", lambda e: e.dma_start(out=QTh[hs][:], in_=QT_d[h]), writes=[r[f"QTh{hs}"]])

            def stage1(k):
                B_ = blocks[k]
                b2, hs, j, gq, m = k % 2, B_["hs"], B_["j"], B_["gq"], B_["m"]
                S.op("pe", lambda e: e.matmul(out=bZ[b2][:], lhsT=KTh[hs][:, j * 128:(j + 1) * 128], rhs=QTh[hs][:, gq * 512:(gq + 1) * 512], start=True, stop=True),
                     reads=[r[f"KTh{hs}"], r[f"QTh{hs}"]], writes=[r[f"bZ{b2}"]])
                S.op("act", lambda e: e.activation(out=Es[b2][:], in_=bZ[b2][:], func=AF.Exp), reads=[r[f"bZ{b2}"]], writes=[r[f"Es{b2}"]])
                if m >= 0:
                    S.op("dve", lambda e: e.tensor_tensor(out=Es[b2][:], in0=Es[b2][:], in1=maskf[:, m, :], op=ALU.mult),
                         reads=[r[f"Es{b2}"], r["mask"]], writes=[r[f"Es{b2}"]])

            def stage1b(k):
                b2 = k % 2
                S.op("act", lambda e: e.activation(out=Ls[b2][:], in_=Es[b2][:], func=AF.Ln, bias=1.0), reads=[r[f"Es{b2}"]], writes=[r[f"Ls{b2}"]])

            def stage2(k):
                B_ = blocks[k]
                b2, pc, first, last = k % 2, (k - 1) % 2, B_["first"], B_["last"]
                S.op("pe", lambda e: e.matmul(out=bRC[b2][:], lhsT=trilb[:], rhs=Ls[b2][:], start=True, stop=first),
                     reads=[r[f"Ls{b2}"]], writes=[r[f"bRC{b2}"]])
                if not first:
                    S.op("pe", lambda e: e.matmul(out=bRC[b2][:], lhsT=onesb[:], rhs=CSs[pc][:], start=False, stop=True),
                         reads=[r[f"CSs{pc}"]], writes=[r[f"bRC{b2}"]])
                S.op("act", lambda e: e.activation(out=Aes[b2][:], in_=bRC[b2][:], func=AF.Exp, scale=-1.0), reads=[r[f"bRC{b2}"]], writes=[r[f"Aes{b2}"]])
                S.op("dve", lambda e: e.tensor_tensor(out=As[b2][:], in0=Es[b2][:], in1=Aes[b2][:], op=ALU.mult),
                     reads=[r[f"Es{b2}"], r[f"Aes{b2}"]], writes=[r[f"As{b2}"]])
                if not last:
                    if first:
                        S.op("dve", lambda e: e.tensor_copy(out=CSs[b2][:], in_=Ls[b2][:]), reads=[r[f"Ls{b2}"]], writes=[r[f"CSs{b2}"]])
                    else:
                        S.op("dve", lambda e: e.tensor_tensor(out=CSs[b2][:], in0=CSs[pc][:], in1=Ls[b2][:], op=ALU.add),
                             reads=[r[f"Ls{b2}"], r[f"CSs{pc}"]], writes=[r[f"CSs{b2}"]])

            def stage3(k):
                B_ = blocks[k]
                b2, hs, j, gq, h, oi, first, last = k % 2, B_["hs"], B_["j"], B_["gq"], B_["h"], B_["oi"], B_["first"], B_["last"]
                S.op("pe", lambda e: e.matmul(out=bOT[oi][0:64, :], lhsT=Vh[hs][:, j, :], rhs=As[b2][:], start=first, stop=last),
                     reads=[r[f"Vh{hs}"], r[f"As{b2}"]], writes=[r[f"bOT{oi}"]])
                if last:
                    S.op("dve", lambda e: e.tensor_copy(out=ost[oi][:], in_=bOT[oi][0:64, :]), reads=[r[f"bOT{oi}"]], writes=[r[f"ost{oi}"]])
                    S.dma("sp", lambda e: e.dma_start(out=mixTs_d[4 * gq:4 * gq + 4, :, h, :].rearrange("j d t -> d j t"),
                                                     in_=ost[oi][:].rearrange("d (j t) -> d j t", j=4)),
                          reads=[r[f"ost{oi}"]], writes=[r["mixTsd"]], sem_res=r[f"ost{oi}"])

            load_head(0)
            for step in range(NB + 2):
                for h in range(7):
                    if step == head_start[h] + 3:
                        load_head(h + 1)
                if step < NB:
                    stage1(step)
                if 0 <= step - 1 < NB:
                    stage2(step - 1)
                if step < NB:
                    stage1b(step)
                if 0 <= step - 2 < NB:
                    stage3(step - 2)
            S.barrier()
            S.release(list(r.values()))

        def phase_C():
          NPASS = 2
          NTH = NTO // NPASS
          TG = min(4, NTH)
          with ExitStack() as ec:
            gv = sbt(ec, "gv", [128, 4, D], F32)
            brt = sbt(ec, "brt", [128, 36], F32)
            wrt = sbt(ec, "wrt", [128, 8, 36], F32)
            acc = sbt(ec, "acc", [128, NTH, D], F32)
            cTb = sbt(ec, "cTb", [128, 8, NTH * 128], BF16)
            wfull = sbt(ec, "wfull", [128, NTH, 32], F32)
            junk = sbt(ec, "junkc", [128, D], BF16)
            ssq = sbt(ec, "ssqc", [128, 2], F32)
            r0 = {k: R(k) for k in ["gv", "brt", "wrt", "acc", "cTb", "wfull", "xc0", "xc1", "junk", "ssq", "outd"]}
            S.dma("sp", lambda e: e.dma_start(out=gv[:], in_=gvec[1:5, :].partition_broadcast(128)), writes=[r0["gv"]])
            S.dma("sp", lambda e: e.dma_start(out=brt[:], in_=br.partition_broadcast(128)), writes=[r0["brt"]])
            S.dma("sp", lambda e: e.dma_start(out=wrt[:], in_=wr.rearrange("(c p) n -> p c n", p=128)), writes=[r0["wrt"]])

            for ps_i in range(NPASS):
                t0 = ps_i * NTH
                with ExitStack() as e1:
                    xts = [sbt(e1, f"xc{i}", [128, D], F32) for i in range(2)]
                    woutm = sbt(e1, "woutm", [128, 4, D], BF16)
                    wouts = sbt(e1, "wouts", [64, 8, D], BF16)
                    mTm = [sbt(e1, f"mTm{i}", [128, 4, 128], BF16) for i in range(2)]
                    mTs = [sbt(e1, f"mTs{i}", [64, 8, 128], BF16) for i in range(2)]
                    c32 = sbt(e1, "c32", [128, D], F32)
                    cT32 = sbt(e1, "cT32", [128, 8, 128], F32)
                    lg = sbt(e1, "lg", [128, 36], F32)
                    gmax = sbt(e1, "gmax", [128, 8], F32)
                    ohg = sbt(e1, "ohg", [128, 4], F32)
                    eg = sbt(e1, "eg", [128, 4], F32)
                    esel = sbt(e1, "esel", [128, 4, 8], F32)
                    es8 = sbt(e1, "es8", [128, 8], F32)
                    mk1 = sbt(e1, "mk1", [128, 8], F32)
                    mk2 = sbt(e1, "mk2", [128, 8], F32)
                    e2 = sbt(e1, "e2", [128, 8], F32)
                    wsel = sbt(e1, "wsel", [128, 8], F32)
                    bH = [pst(e1, f"bH{i}", [128, 512], F32) for i in range(2)]
                    bTf = [pst(e1, f"bTf{i}", [128, 512], F32) for i in range(2)]
                    bL = pst(e1, "bL", [128, 512], F32)
                    r = {k: R(k) for k in ["woutm", "wouts", "mTm0", "mTm1", "mTs0", "mTs1", "c32", "cT32", "lg", "rt", "bH0", "bH1", "bTf0", "bTf1", "bL"]}
                    S.dma("pool", lambda e: e.dma_start(out=woutm[:], in_=wout[0:512, :].rearrange("(h p) n -> p h n", p=128)), writes=[r["woutm"]])
                    S.dma("pool", lambda e: e.dma_start(out=wouts[:], in_=wout[512:1024, :].rearrange("(h p) n -> p h n", p=64)), writes=[r["wouts"]])
                    for tl in range(NTH):
                        t = t0 + tl
                        xi = tl % 2
                        xt = xts[xi]
                        rx = r0[f"xc{xi}"]
                        S.dma("sp", lambda e, xt=xt, t=t: e.dma_start(out=xt[:], in_=xo[t * 128:(t + 1) * 128, :]), writes=[rx])
                        S.dma("sp", lambda e, xi=xi, t=t: e.dma_start(out=mTm[xi][:], in_=mixTm_d[t]), writes=[r[f"mTm{xi}"]])
                        S.dma("sp", lambda e, xi=xi, t=t: e.dma_start(out=mTs[xi][:], in_=mixTs_d[t]), writes=[r[f"mTs{xi}"]])
                        for dh in range(2):
                            for h in range(4):
                                S.op("pe", lambda e, h=h, dh=dh, xi=xi: e.matmul(out=bH[dh][:], lhsT=mTm[xi][:, h, :], rhs=woutm[:, h, dh * 512:(dh + 1) * 512], start=(h == 0), stop=False),
                                     reads=[r[f"mTm{xi}"], r["woutm"]], writes=[r[f"bH{dh}"]])
                            for h in range(8):
                                S.op("pe", lambda e, h=h, dh=dh, xi=xi: e.matmul(out=bH[dh][:], lhsT=mTs[xi][:, h, :], rhs=wouts[:, h, dh * 512:(dh + 1) * 512], start=False, stop=(h == 7)),
                                     reads=[r[f"mTs{xi}"], r["wouts"]], writes=[r[f"bH{dh}"]])
                            S.op("dve", lambda e, dh=dh, tl=tl, xt=xt: e.tensor_tensor(out=acc[:, tl, dh * 512:(dh + 1) * 512], in0=bH[dh][:], in1=xt[:, dh * 512:(dh + 1) * 512], op=ALU.add),
                                 reads=[r[f"bH{dh}"], rx], writes=[r0["acc"]])
                        rmsnorm_stats(acc[:, tl, :], junk[:], ssq[:, 0:1], ssq[:, 1:2], r0["acc"], r0["junk"], r0["ssq"], D)
                        S.op("dve", lambda e, tl=tl: e.scalar_tensor_tensor(out=c32[:], in0=acc[:, tl, :], scalar=ssq[:, 1:2], in1=gv[:, 0, :], op0=ALU.mult, op1=ALU.mult),
                             reads=[r0["acc"], r0["ssq"], r0["gv"]], writes=[r["c32"]])
                        for c in range(8):
                            S.op("pe", lambda e, c=c: e.transpose(out=bTf[c // 4][:, (c % 4) * 128:(c % 4 + 1) * 128], in_=c32[:, c * 128:(c + 1) * 128], identity=identf[:]),
                                 reads=[r["c32"]], writes=[r[f"bTf{c // 4}"]])
                        for hh in range(2):
                            S.op("act", lambda e, hh=hh: e.copy(out=cT32[:, hh * 4:(hh + 1) * 4, :], in_=bTf[hh][:].rearrange("p (c t) -> p c t", c=4)),
                                 reads=[r[f"bTf{hh}"]], writes=[r["cT32"]])
                            S.op("dve", lambda e, hh=hh, tl=tl: e.tensor_copy(out=cTb[:, hh * 4:(hh + 1) * 4, tl * 128:(tl + 1) * 128], in_=cT32[:, hh * 4:(hh + 1) * 4, :]),
                                 reads=[r["cT32"]], writes=[r0["cTb"]])
                        for c in range(8):
                            S.op("pe", lambda e, c=c: e.matmul(out=bL[:, 0:36], lhsT=cT32[:, c, :], rhs=wrt[:, c, :], start=(c == 0), stop=(c == 7)),
                                 reads=[r["cT32"], r0["wrt"]], writes=[r["bL"]])
                        rt = r["rt"]
                        S.op("dve", lambda e: e.tensor_tensor(out=lg[:], in0=bL[:, 0:36], in1=brt[:], op=ALU.add), reads=[r["bL"], r0["brt"]], writes=[rt])
                        S.op("dve", lambda e: e.tensor_reduce(out=gmax[:, 0:1], in_=lg[:, 0:4], axis=AX.X, op=ALU.max), reads=[rt], writes=[rt])
                        S.op("dve", lambda e: e.tensor_scalar(out=ohg[:], in0=lg[:, 0:4], scalar1=gmax[:, 0:1], scalar2=None, op0=ALU.is_equal), reads=[rt], writes=[rt])
                        S.op("dve", lambda e: e.tensor_scalar(out=eg[:], in0=lg[:, 0:4], scalar1=gmax[:, 0:1], scalar2=None, op0=ALU.subtract), reads=[rt], writes=[rt])
                        S.op("pool", lambda e: e.memset(gmax[:, 1:2], 0.0), reads=[rt], writes=[rt])
                        S.op("act", lambda e: e.activation(out=eg[:], in_=eg[:], func=AF.Exp, accum_out=gmax[:, 1:2]), reads=[rt], writes=[rt])
                        S.op("dve", lambda e: e.reciprocal(out=gmax[:, 2:3], in_=gmax[:, 1:2]), reads=[rt], writes=[rt])
                        S.op("dve", lambda e: e.tensor_tensor(out=esel[:], in0=lg[:, 4:36].rearrange("p (g j) -> p g j", g=4), in1=ohg[:].unsqueeze(2).to_broadcast([128, 4, 8]), op=ALU.mult),
                             reads=[rt], writes=[rt])
                        S.op("dve", lambda e: e.tensor_reduce(out=es8[:], in_=esel[:].rearrange("p g j -> p j g"), axis=AX.X, op=ALU.add), reads=[rt], writes=[rt])
                        S.op("dve", lambda e: e.tensor_reduce(out=gmax[:, 3:4], in_=es8[:], axis=AX.X, op=ALU.max), reads=[rt], writes=[rt])
                        S.op("dve", lambda e: e.tensor_scalar(out=mk1[:], in0=es8[:], scalar1=gmax[:, 3:4], scalar2=None, op0=ALU.is_equal), reads=[rt], writes=[rt])
                        S.op("dve", lambda e: e.scalar_tensor_tensor(out=e2[:], in0=mk1[:], scalar=-1e30, in1=es8[:], op0=ALU.mult, op1=ALU.add), reads=[rt], writes=[rt])
                        S.op("dve", lambda e: e.tensor_reduce(out=gmax[:, 4:5], in_=e2[:], axis=AX.X, op=ALU.max), reads=[rt], writes=[rt])
                        S.op("dve", lambda e: e.tensor_scalar(out=mk2[:], in0=e2[:], scalar1=gmax[:, 4:5], scalar2=None, op0=ALU.is_equal), reads=[rt], writes=[rt])
                        S.op("dve", lambda e: e.tensor_tensor(out=gmax[:, 5:6], in0=gmax[:, 4:5], in1=gmax[:, 3:4], op=ALU.subtract), reads=[rt], writes=[rt])
                        S.op("act", lambda e: e.activation(out=gmax[:, 5:6], in_=gmax[:, 5:6], func=AF.Exp), reads=[rt], writes=[rt])
                        S.op("dve", lambda e: e.tensor_scalar(out=gmax[:, 6:7], in0=gmax[:, 5:6], scalar1=1.0, scalar2=None, op0=ALU.add), reads=[rt], writes=[rt])
                        S.op("dve", lambda e: e.reciprocal(out=gmax[:, 6:7], in_=gmax[:, 6:7]), reads=[rt], writes=[rt])
                        S.op("dve", lambda e: e.tensor_tensor(out=gmax[:, 6:7], in0=gmax[:, 6:7], in1=gmax[:, 2:3], op=ALU.mult), reads=[rt], writes=[rt])
                        S.op("dve", lambda e: e.tensor_tensor(out=gmax[:, 7:8], in0=gmax[:, 6:7], in1=gmax[:, 5:6], op=ALU.mult), reads=[rt], writes=[rt])
                        S.op("dve", lambda e: e.tensor_scalar(out=wsel[:], in0=mk1[:], scalar1=gmax[:, 6:7], scalar2=None, op0=ALU.mult), reads=[rt], writes=[rt])
                        S.op("dve", lambda e: e.scalar_tensor_tensor(out=wsel[:], in0=mk2[:], scalar=gmax[:, 7:8], in1=wsel[:], op0=ALU.mult, op1=ALU.add), reads=[rt], writes=[rt])
                        for g in range(4):
                            S.op("dve", lambda e, g=g, tl=tl: e.tensor_scalar(out=wfull[:, tl, g * 8:(g + 1) * 8], in0=wsel[:], scalar1=ohg[:, g:g + 1], scalar2=None, op0=ALU.mult),
                                 reads=[rt], writes=[r0["wfull"]])
                    S.barrier()
                    S.release(list(r.values()))

                with ExitStack() as e2s:
                    wgt = [sbt(e2s, f"wgt{i}", [128, 8, 512], BF16) for i in range(2)]
                    wut = [sbt(e2s, f"wut{i}", [128, 8, 512], BF16) for i in range(2)]
                    wdt = [sbt(e2s, f"wdt{i}", [128, 4, D], BF16) for i in range(2)]
                    sgs = [sbt(e2s, f"sgs{i}", [128, TG * 128], F32) for i in range(2)]
                    hid = [sbt(e2s, f"hid{i}", [128, 4, TG * 128], BF16) for i in range(2)]
                    bGt = [pst(e2s, f"bGt{i}", [128, 512], F32) for i in range(2)]
                    bUt = [pst(e2s, f"bUt{i}", [128, 512], F32) for i in range(2)]
                    bY = [pst(e2s, f"bY{i}", [128, 512], F32) for i in range(2)]
                    r = {k: R(k) for k in ["wgt0", "wgt1", "wut0", "wut1", "wdt0", "wdt1", "sgs0", "sgs1", "hid0", "hid1", "bGt0", "bGt1", "bUt0", "bUt1", "bY0", "bY1"]}
                    NW = TG * 128
                    cnt = 0
                    ycnt = 0

                    def load_exp(ex):
                        s_ = ex % 2
                        S.dma("pool", lambda e: e.dma_start(out=wgt[s_][:], in_=wg[ex].rearrange("(c p) n -> p c n", p=128)), writes=[r[f"wgt{s_}"]])
                        S.dma("pool", lambda e: e.dma_start(out=wut[s_][:], in_=wu[ex].rearrange("(c p) n -> p c n", p=128)), writes=[r[f"wut{s_}"]])
                        S.dma("pool", lambda e: e.dma_start(out=wdt[s_][:], in_=wd[ex].rearrange("(c p) n -> p c n", p=128)), writes=[r[f"wdt{s_}"]])

                    load_exp(0)
                    for ex in range(NEXP):
                        s_ = ex % 2
                        if ex + 1 < NEXP:
                            load_exp(ex + 1)
                        for tg in range(NTH // TG):
                            hb = (ex * (NTH // TG) + tg) % 2
                            for fc in range(4):
                                b2 = cnt % 2
                                cnt += 1
                                for c in range(8):
                                    S.op("pe", lambda e, c=c, fc=fc, b2=b2, s_=s_, tg=tg: e.matmul(out=bGt[b2][:, 0:NW], lhsT=wgt[s_][:, c, fc * 128:(fc + 1) * 128],
                                                                                                  rhs=cTb[:, c, tg * NW:(tg + 1) * NW], start=(c == 0), stop=(c == 7)),
                                         reads=[r[f"wgt{s_}"], r0["cTb"]], writes=[r[f"bGt{b2}"]])
                                for c in range(8):
                                    S.op("pe", lambda e, c=c, fc=fc, b2=b2, s_=s_, tg=tg: e.matmul(out=bUt[b2][:, 0:NW], lhsT=wut[s_][:, c, fc * 128:(fc + 1) * 128],
                                                                                                  rhs=cTb[:, c, tg * NW:(tg + 1) * NW], start=(c == 0), stop=(c == 7)),
                                         reads=[r[f"wut{s_}"], r0["cTb"]], writes=[r[f"bUt{b2}"]])
                                S.op("act", lambda e, b2=b2: e.activation(out=sgs[b2][:], in_=bGt[b2][:, 0:NW], func=AF.Silu), reads=[r[f"bGt{b2}"]], writes=[r[f"sgs{b2}"]])
                                S.op("dve", lambda e, b2=b2, hb=hb, fc=fc: e.tensor_tensor(out=hid[hb][:, fc, :], in0=sgs[b2][:], in1=bUt[b2][:, 0:NW], op=ALU.mult),
                                     reads=[r[f"sgs{b2}"], r[f"bUt{b2}"]], writes=[r[f"hid{hb}"]])
                            for tt in range(TG):
                                tl = tg * TG + tt
                                for dh in range(2):
                                    yb = ycnt % 2
                                    ycnt += 1
                                    for fc in range(4):
                                        S.op("pe", lambda e, fc=fc, hb=hb, tt=tt, dh=dh, yb=yb, s_=s_: e.matmul(out=bY[yb][:], lhsT=hid[hb][:, fc, tt * 128:(tt + 1) * 128],
                                                                                                               rhs=wdt[s_][:, fc, dh * 512:(dh + 1) * 512], start=(fc == 0), stop=(fc == 3)),
                                             reads=[r[f"hid{hb}"], r[f"wdt{s_}"]], writes=[r[f"bY{yb}"]])
                                    S.op("dve", lambda e, yb=yb, tl=tl, dh=dh, ex=ex: e.scalar_tensor_tensor(out=acc[:, tl, dh * 512:(dh + 1) * 512], in0=bY[yb][:], scalar=wfull[:, tl, ex:ex + 1],
                                                                                                           in1=acc[:, tl, dh * 512:(dh + 1) * 512], op0=ALU.mult, op1=ALU.add),
                                         reads=[r[f"bY{yb}"], r0["wfull"], r0["acc"]], writes=[r0["acc"]])
                    S.barrier()
                    S.release(list(r.values()))

                with ExitStack() as e3:
                    wpgt = sbt(e3, "wpgt", [128, 8, D], BF16)
                    wppt = sbt(e3, "wppt", [128, 2, D], BF16)
                    n_bf = sbt(e3, "n_bf", [128, D], BF16)
                    nT = sbt(e3, "nT", [128, 8, 128], BF16)
                    gate = sbt(e3, "gate", [128, D], F32)
                    pts = [sbt(e3, f"pt{i}", [128, 256], F32) for i in range(2)]
                    p_bf = sbt(e3, "p_bf", [128, 256], BF16)
                    pT = sbt(e3, "pT", [128, 2, 128], BF16)
                    ple = sbt(e3, "ple", [128, D], F32)
                    ss2 = sbt(e3, "ss2", [128, 4], F32)
                    h3 = sbt(e3, "h3", [128, D], F32)
                    ots = [sbt(e3, f"ot{i}", [128, D], F32) for i in range(2)]
                    bT2 = pst(e3, "bT2", [128, 1024], BF16)
                    bGa = [pst(e3, f"bGa{i}", [128, 512], F32) for i in range(2)]
                    bP = [pst(e3, f"bP{i}", [128, 512], F32) for i in range(2)]
                    r = {k: R(k) for k in ["wpgt", "wppt", "n_bf", "nT", "gate", "pt0", "pt1", "p_bf", "pT", "ple", "ss2", "h3", "ot0", "ot1", "bT2", "bGa0", "bGa1", "bP0", "bP1", "junk2"]}
                    S.dma("pool", lambda e: e.dma_start(out=wpgt[:], in_=wpg.rearrange("(c p) n -> p c n", p=128)), writes=[r["wpgt"]])
                    S.dma("pool", lambda e: e.dma_start(out=wppt[:], in_=wpp.rearrange("(c p) n -> p c n", p=128)), writes=[r["wppt"]])
                    for tl in range(NTH):
                        t = t0 + tl
                        pi = tl % 2
                        S.dma("sp", lambda e, pi=pi, t=t: e.dma_start(out=pts[pi][:], in_=po[t * 128:(t + 1) * 128, :]), writes=[r[f"pt{pi}"]])
                        rmsnorm_stats(acc[:, tl, :], junk[:], ssq[:, 0:1], ssq[:, 1:2], r0["acc"], r0["junk"], r0["ssq"], D)
                        S.op("dve", lambda e, tl=tl: e.scalar_tensor_tensor(out=n_bf[:], in0=acc[:, tl, :], scalar=ssq[:, 1:2], in1=gv[:, 1, :], op0=ALU.mult, op1=ALU.mult),
                             reads=[r0["acc"], r0["ssq"], r0["gv"]], writes=[r["n_bf"]])
                        for c in range(8):
                            S.op("pe", lambda e, c=c: e.transpose(out=bT2[:, c * 128:(c + 1) * 128], in_=n_bf[:, c * 128:(c + 1) * 128], identity=identb[:]),
                                 reads=[r["n_bf"]], writes=[r["bT2"]])
                        S.op("act", lambda e: e.copy(out=nT[:], in_=bT2[:].rearrange("p (c t) -> p c t", c=8)), reads=[r["bT2"]], writes=[r["nT"]])
                        for dh in range(2):
                            for c in range(8):
                                S.op("pe", lambda e, c=c, dh=dh: e.matmul(out=bGa[dh][:], lhsT=nT[:, c, :], rhs=wpgt[:, c, dh * 512:(dh + 1) * 512], start=(c == 0), stop=(c == 7)),
                                     reads=[r["nT"], r["wpgt"]], writes=[r[f"bGa{dh}"]])
                            S.op("act", lambda e, dh=dh: e.activation(out=gate[:, dh * 512:(dh + 1) * 512], in_=bGa[dh][:], func=AF.Sigmoid), reads=[r[f"bGa{dh}"]], writes=[r["gate"]])
                        S.op("dve", lambda e, pi=pi: e.tensor_copy(out=p_bf[:], in_=pts[pi][:]), reads=[r[f"pt{pi}"]], writes=[r["p_bf"]])
                        for c in range(2):
                            S.op("pe", lambda e, c=c: e.transpose(out=bT2[:, c * 128:(c + 1) * 128], in_=p_bf[:, c * 128:(c + 1) * 128], identity=identb[:]),
                                 reads=[r["p_bf"]], writes=[r["bT2"]])
                        S.op("act", lambda e: e.copy(out=pT[:], in_=bT2[:, 0:256].rearrange("p (c t) -> p c t", c=2)), reads=[r["bT2"]], writes=[r["pT"]])
                        for dh in range(2):
                            for c in range(2):
                                S.op("pe", lambda e, c=c, dh=dh: e.matmul(out=bP[dh][:], lhsT=pT[:, c, :], rhs=wppt[:, c, dh * 512:(dh + 1) * 512], start=(c == 0), stop=(c == 1)),
                                     reads=[r["pT"], r["wppt"]], writes=[r[f"bP{dh}"]])
                            S.op("act", lambda e, dh=dh: e.copy(out=ple[:, dh * 512:(dh + 1) * 512], in_=bP[dh][:]), reads=[r[f"bP{dh}"]], writes=[r["ple"]])
                        rmsnorm_stats(ple[:], junk[:], ss2[:, 0:1], ss2[:, 1:2], r["ple"], r0["junk"], r["ss2"], D)
                        S.op("dve", lambda e: e.scalar_tensor_tensor(out=ple[:], in0=ple[:], scalar=ss2[:, 1:2], in1=gv[:, 2, :], op0=ALU.mult, op1=ALU.mult),
                             reads=[r["ple"], r["ss2"], r0["gv"]], writes=[r["ple"]])
                        S.op("dve", lambda e: e.tensor_tensor(out=ple[:], in0=ple[:], in1=gate[:], op=ALU.mult), reads=[r["ple"], r["gate"]], writes=[r["ple"]])
                        S.op("dve", lambda e, tl=tl: e.tensor_tensor(out=h3[:], in0=ple[:], in1=acc[:, tl, :], op=ALU.add), reads=[r["ple"], r0["acc"]], writes=[r["h3"]])
                        rmsnorm_stats(h3[:], junk[:], ss2[:, 2:3], ss2[:, 3:4], r["h3"], r0["junk"], r["ss2"], D)
                        S.op("dve", lambda e, pi=pi: e.scalar_tensor_tensor(out=ots[pi][:], in0=h3[:], scalar=ss2[:, 3:4], in1=gv[:, 3, :], op0=ALU.mult, op1=ALU.mult),
                             reads=[r["h3"], r["ss2"], r0["gv"]], writes=[r[f"ot{pi}"]])
                        S.dma("sp", lambda e, pi=pi, t=t: e.dma_start(out=out[t * 128:(t + 1) * 128, :], in_=ots[pi][:]), reads=[r[f"ot{pi}"]], writes=[r0["outd"]], sem_res=r[f"ot{pi}"])
                    S.barrier()
                    S.release(list(r.values()))
        if 'A' in phases:
            phase_A()
        if 'B' in phases:
            phase_B()
        if 'C' in phases:
            phase_C()
        S.barrier()
        build_nc.last_log = S.log
        S.emit(block)
    return nc


def prep_core_inputs(inp, b, half, SP, SO):
    x = inp["x"]
    f = np.float32
    if half == 0:
        xp = np.zeros((SP, D), f)
    else:
        xp = np.ascontiguousarray(x[b, 0:SP])
    xo = np.ascontiguousarray(x[b, half * SP: half * SP + SO]) if half == 1 else np.ascontiguousarray(x[b, 0:SO])
    p = inp["p"][0, b]
    po = np.ascontiguousarray(p[half * SP: half * SP + SO]) if half == 1 else np.ascontiguousarray(p[0:SO])
    cq = np.ascontiguousarray(inp["conv_q"][0].T.reshape(4, 128, 4).transpose(1, 0, 2).reshape(128, 16))
    ck = np.ascontiguousarray(inp["conv_k"][0].T.reshape(4, 128, 4).transpose(1, 0, 2).reshape(128, 16))
    gvec = np.stack([inp["g_mix"][0], inp["g_ffn"][0], inp["g_ple"][0], inp["g_ple_post"][0], inp["g_final"]]).astype(f)
    wr = np.concatenate([inp["w_router_group"][0], inp["w_router_expert"][0]], axis=1).astype(f)
    br = np.concatenate([inp["b_router_group"][0], inp["b_router_expert"][0]])[None, :].astype(f)
    return {
        "xp": xp, "xo": xo, "po": po,
        "win": np.ascontiguousarray(inp["w_in"][0]),
        "gvec": np.ascontiguousarray(gvec),
        "gmh": np.ascontiguousarray(inp["g_mhead"]),
        "bg": np.ascontiguousarray(inp["b_gates"]),
        "cq": cq, "ck": ck,
        "wout": np.ascontiguousarray(inp["w_out"][0]),
        "wr": np.ascontiguousarray(wr), "br": np.ascontiguousarray(br),
        "wg": np.ascontiguousarray(inp["w_exp_gate"][0]),
        "wu": np.ascontiguousarray(inp["w_exp_up"][0]),
        "wd": np.ascontiguousarray(inp["w_exp_down"][0]),
        "wpg": np.ascontiguousarray(inp["w_ple_gate"][0]),
        "wpp": np.ascontiguousarray(inp["w_ple_proj"][0]),
    }


def kernel(**inputs):
    inp = {k: np.asarray(v) for k, v in inputs.items()}
    x = inp["x"]
    B, SEQ, _ = x.shape
    SH = SEQ // 2
    import os
    nc = build_nc(SH, SH, phases=os.environ.get("KPHASES", "ABC"))
    in_maps = []
    for c in range(8):
        b, half = c // 2, c % 2
        in_maps.append(prep_core_inputs(inp, b, half, SH, SH))
    res = run_bass_kernel_spmd(nc, in_maps, core_ids=list(range(8)))
    out = np.empty((B, SEQ, D), np.float32)
    for c in range(8):
        b, half = c // 2, c % 2
        out[b, half * SH:(half + 1) * SH] = res.results[c]["out"]
    return out
```
